# Optimizing a Trainium2 kernel written in Bass

```python
import jax, jax.numpy as jnp
from jax import lax
import numpy as np

D_MODEL = 1024
BATCH = 2
SEQ = 16384
DEPTH = 2

D_FF = 2816
EPS = 1e-6
NSA_HEADS = 8
NSA_KV_GROUPS = 2
HEAD_DIM = 64
NSA_WIDTH = NSA_HEADS * HEAD_DIM
KV_WIDTH = NSA_KV_GROUPS * HEAD_DIM
CMP_BLOCK = 32
CMP_STRIDE = 16
CMP_HIDDEN = 128
SLC_BLOCK = 64
RATIO = SLC_BLOCK // CMP_STRIDE
SLC_TOP_N = 16
WINDOW = 512
Q_BLOCK = 128
FORCE_BONUS = 1e4
CONV_WIDTH = 512
CONV_K = 3
IN_SIZES = (NSA_WIDTH, 3 * NSA_HEADS, KV_WIDTH, KV_WIDTH, KV_WIDTH, KV_WIDTH,
            KV_WIDTH, KV_WIDTH, CONV_WIDTH, CONV_WIDTH, CONV_WIDTH)
IN_COLS = sum(IN_SIZES)
IN_SPLITS = tuple(int(s) for s in np.cumsum(IN_SIZES)[:-1])
MIX_OUT = NSA_WIDTH + CONV_WIDTH
RWKV_HEAD = 64
RWKV_HEADS = D_MODEL // RWKV_HEAD
DECAY_LORA = 64
AAA_LORA = 64
GATE_LORA = 128
LNX_EPS = 64e-5

kernel_name = "hybrid_nsa_shortconv_rwkv7_macaron"


def rmsnorm(x, g):
    xf = x.astype(jnp.float32)
    y = xf * lax.rsqrt(jnp.mean(xf * xf, -1, keepdims=True) + EPS)
    return (y * g.astype(jnp.float32)).astype(x.dtype)


def macaron_half_ffn(x, pre_g, post_g, w_gate, w_up, w_down):
    h = rmsnorm(x, pre_g)
    h = (jax.nn.silu(h @ w_gate) * (h @ w_up)) @ w_down
    return x + 0.5 * rmsnorm(h, post_g)


def alibi_slopes(n):
    return 2.0 ** (-8.0 * jnp.arange(1, n + 1, dtype=jnp.float32) / n)


def masked_softmax(s, mask):
    s = jnp.where(mask, s.astype(jnp.float32), -jnp.inf)
    m = jnp.max(s, -1, keepdims=True)
    m = jnp.where(jnp.isfinite(m), m, 0.0)
    p = jnp.exp(s - m)
    return p / jnp.maximum(jnp.sum(p, -1, keepdims=True), 1e-30)


def compress_blocks(kv, pe, w1, w2):
    B, S, G, Dh = kv.shape
    halves = kv.reshape(B, S // CMP_STRIDE, CMP_STRIDE, G, Dh)
    blocks = jnp.concatenate([halves[:, :-1], halves[:, 1:]], axis=2)
    blocks = blocks + pe[None, None, :, None, :]
    nc = blocks.shape[1]
    flat = blocks.transpose(0, 1, 3, 2, 4).reshape(B, nc, G, CMP_BLOCK * Dh)
    return jax.nn.gelu(flat @ w1) @ w2


def nsa_attention(q, gates, kc, vc, k_slc, v_slc, k_win, v_win):
    B, S, H, Dh = q.shape
    G = kc.shape[2]
    R = H // G
    n_cmp = kc.shape[1]
    n_slc = S // SLC_BLOCK
    top_n = min(SLC_TOP_N, n_slc)
    slopes = alibi_slopes(H).reshape(G, R)
    cmp_end = jnp.arange(n_cmp) * CMP_STRIDE + (CMP_BLOCK - 1)
    qg = q.reshape(B, S, G, R, Dh) * (Dh ** -0.5)
    gg = gates.reshape(B, S, G, R, 3)
    kb = k_slc.reshape(B, n_slc, SLC_BLOCK, G, Dh).transpose(0, 3, 1, 2, 4)
    vb = v_slc.reshape(B, n_slc, SLC_BLOCK, G, Dh).transpose(0, 3, 1, 2, 4)
    gather = jax.vmap(jax.vmap(lambda blocks, ix: blocks[ix]))
    kw = jnp.pad(k_win, ((0, 0), (WINDOW, 0), (0, 0), (0, 0)))
    vw = jnp.pad(v_win, ((0, 0), (WINDOW, 0), (0, 0), (0, 0)))
    j = jnp.arange(n_slc)
    offs = jnp.arange(SLC_BLOCK)
    woffs = jnp.arange(Q_BLOCK + WINDOW) - WINDOW

    def query_block(qi):
        q0 = qi * Q_BLOCK
        t = q0 + jnp.arange(Q_BLOCK)
        qb = lax.dynamic_slice_in_dim(qg, q0, Q_BLOCK, axis=1)
        gb = lax.dynamic_slice_in_dim(gg, q0, Q_BLOCK, axis=1)
        d_cmp = t[:, None] - cmp_end[None, :]
        s = jnp.einsum("btgrd,bngd->bgrtn", qb, kc) - slopes[:, :, None, None] * d_cmp
        p_cmp = masked_softmax(s, d_cmp >= 0)
        o_cmp = jnp.einsum("bgrtn,bngd->btgrd", p_cmp.astype(vc.dtype), vc)
        imp = jnp.pad(p_cmp.sum(2), ((0, 0), (0, 0), (0, 0), (0, 1)))
        imp = imp.reshape(B, G, Q_BLOCK, n_slc, RATIO)
        last = imp[..., RATIO - 1]
        prev = jnp.pad(last, ((0, 0), (0, 0), (0, 0), (1, 0)))[..., :-1]
        imp = imp.sum(-1) - 0.5 * last + 0.5 * prev
        blk_t = (t // SLC_BLOCK)[:, None]
        forced = (j == 0) | (j == blk_t) | (j == blk_t - 1)
        imp = jnp.where(forced, imp + FORCE_BONUS, imp)
        imp = jnp.where(j <= blk_t, imp, -jnp.inf)
        _, idx = lax.top_k(imp, top_n)
        ks = gather(kb, idx)
        vs = gather(vb, idx)
        d_slc = t[:, None, None] - (idx[..., None] * SLC_BLOCK + offs)
        s = jnp.einsum("btgrd,bgtnld->bgrtnl", qb, ks) - slopes[:, :, None, None, None] * d_slc[:, :, None]
        mask = jnp.broadcast_to((d_slc >= 0)[:, :, None], s.shape)
        p = masked_softmax(s.reshape(B, G, R, Q_BLOCK, -1), mask.reshape(B, G, R, Q_BLOCK, -1))
        o_slc = jnp.einsum("bgrtm,bgtmd->btgrd", p.astype(vs.dtype),
                           vs.reshape(B, G, Q_BLOCK, top_n * SLC_BLOCK, Dh))
        kwb = lax.dynamic_slice_in_dim(kw, q0, Q_BLOCK + WINDOW, axis=1)
        vwb = lax.dynamic_slice_in_dim(vw, q0, Q_BLOCK + WINDOW, axis=1)
        kpos = q0 + woffs
        d_win = t[:, None] - kpos[None, :]
        mask = (d_win >= 0) & (d_win < WINDOW) & (kpos[None, :] >= 0)
        s = jnp.einsum("btgrd,bsgd->bgrts", qb, kwb) - slopes[:, :, None, None] * d_win
        p = masked_softmax(s, mask)
        o_win = jnp.einsum("bgrts,bsgd->btgrd", p.astype(vwb.dtype), vwb)
        return gb[..., 0:1] * o_cmp + gb[..., 1:2] * o_slc + gb[..., 2:3] * o_win

    out = lax.map(query_block, jnp.arange(S // Q_BLOCK))
    return out.transpose(1, 0, 2, 3, 4, 5).reshape(B, S, H * Dh)


def short_conv(b_gate, c_gate, u, conv_w, conv_b):
    z = c_gate * u
    y = lax.conv_general_dilated(z, conv_w[:, None, :].astype(z.dtype), window_strides=(1,),
                                 padding=[(CONV_K - 1, 0)],
                                 dimension_numbers=("NWC", "WIO", "NWC"),
                                 feature_group_count=z.shape[-1])
    return b_gate * (y + conv_b)


def nsa_shortconv_mixer(h, w_in, cmp_pe_k, cmp_w1_k, cmp_w2_k, cmp_pe_v, cmp_w1_v, cmp_w2_v,
                        conv_w, conv_b, w_out):
    B, S, _ = h.shape
    q, g, kc, vc, ks, vs, kw, vw, cb, cc, cu = jnp.split(h @ w_in, IN_SPLITS, axis=-1)
    kv = lambda t: t.reshape(B, S, NSA_KV_GROUPS, HEAD_DIM)
    kc = compress_blocks(kv(kc), cmp_pe_k, cmp_w1_k, cmp_w2_k)
    vc = compress_blocks(kv(vc), cmp_pe_v, cmp_w1_v, cmp_w2_v)
    o_nsa = nsa_attention(q.reshape(B, S, NSA_HEADS, HEAD_DIM),
                          jax.nn.sigmoid(g).reshape(B, S, NSA_HEADS, 3),
                          kc, vc, kv(ks), kv(vs), kv(kw), kv(vw))
    o_conv = short_conv(cb, cc, cu, conv_w, conv_b)
    return jnp.concatenate([o_nsa.astype(h.dtype), o_conv], axis=-1) @ w_out


def rwkv7_mixer(h, mu, w_r, w_k, w_v, w_o, w0, w_dec1, w_dec2, a0, w_a1, w_a2,
                w_g1, w_g2, k_k, k_a, r_k, lnx_g, lnx_b):
    B, S, D = h.shape
    H, N = RWKV_HEADS, RWKV_HEAD
    xx = jnp.pad(h, ((0, 0), (1, 0), (0, 0)))[:, :-1] - h
    xr, xw, xk, xv, xa, xg = [h + xx * mu[i] for i in range(6)]
    r = xr @ w_r
    k = xk @ w_k
    v = xv @ w_v
    w_log = -jax.nn.softplus(-(w0 + jnp.tanh(xw @ w_dec1) @ w_dec2)) - 0.5
    a = jax.nn.sigmoid(a0 + (xa @ w_a1) @ w_a2)
    g = jax.nn.sigmoid(xg @ w_g1) @ w_g2
    kk = (k * k_k).reshape(B, S, H, N).astype(jnp.float32)
    kk = kk * lax.rsqrt(jnp.maximum(jnp.sum(kk * kk, -1, keepdims=True), 1e-12))
    k = k * (1.0 + (a - 1.0) * k_a)
    decay = jnp.exp(-jnp.exp(w_log.astype(jnp.float32)))
    heads = lambda t: t.reshape(B, S, H, N).astype(jnp.float32).transpose(1, 0, 2, 3)
    xs = (heads(r), heads(decay), heads(k), heads(v), kk.transpose(1, 0, 2, 3), heads(a))

    def step(state, inp):
        r_t, w_t, k_t, v_t, kk_t, a_t = inp
        sa = jnp.einsum("bhij,bhj->bhi", state, -kk_t)
        state = (state * w_t[:, :, None, :] + sa[..., None] * (kk_t * a_t)[:, :, None, :]
                 + v_t[..., None] * k_t[:, :, None, :])
        return state, jnp.einsum("bhij,bhj->bhi", state, r_t)

    state0 = jnp.zeros((B, H, N, N), jnp.float32)
    _, y = lax.scan(step, state0, xs)
    y = y.transpose(1, 0, 2, 3)
    mean = jnp.mean(y, -1, keepdims=True)
    var = jnp.mean(jnp.square(y - mean), -1, keepdims=True)
    y = ((y - mean) * lax.rsqrt(var + LNX_EPS)).reshape(B, S, D) * lnx_g + lnx_b
    rh, kh, vh = (t.reshape(B, S, H, N).astype(jnp.float32) for t in (r, k, v))
    bonus = (jnp.sum(rh * kh * r_k, -1, keepdims=True) * vh).reshape(B, S, D)
    return ((y + bonus).astype(h.dtype) * g) @ w_o


def setup_inputs(seed: int = 0) -> dict:
    key = jax.random.key(seed)
    keys = iter(jax.random.split(key, 80))

    def nrm(shape, scale):
        return jax.random.normal(next(keys), shape, jnp.float32) * scale

    def unif(shape, lo, hi):
        return jax.random.uniform(next(keys), shape, jnp.float32, lo, hi)

    def gain(n=D_MODEL):
        return 1.0 + nrm((n,), 0.05)

    inp = {"x": nrm((BATCH, SEQ, D_MODEL), 1.0)}

    def add_ffn(p):
        inp[p + "_pre_g"] = gain()
        inp[p + "_post_g"] = gain()
        inp[p + "_w_gate"] = nrm((D_MODEL, D_FF), D_MODEL ** -0.5)
        inp[p + "_w_up"] = nrm((D_MODEL, D_FF), D_MODEL ** -0.5)
        inp[p + "_w_down"] = nrm((D_FF, D_MODEL), D_FF ** -0.5)

    add_ffn("l0_ffn1")
    inp["l0_mix_pre_g"] = gain()
    inp["l0_mix_post_g"] = gain()
    inp["l0_w_in"] = nrm((D_MODEL, IN_COLS), D_MODEL ** -0.5)
    for kvn in ("k", "v"):
        inp["l0_cmp_pe_" + kvn] = nrm((CMP_BLOCK, HEAD_DIM), 0.1)
        inp["l0_cmp_w1_" + kvn] = nrm((CMP_BLOCK * HEAD_DIM, CMP_HIDDEN), (CMP_BLOCK * HEAD_DIM) ** -0.5)
        inp["l0_cmp_w2_" + kvn] = nrm((CMP_HIDDEN, HEAD_DIM), CMP_HIDDEN ** -0.5)
    inp["l0_conv_w"] = nrm((CONV_K, CONV_WIDTH), 0.5)
    inp["l0_conv_b"] = nrm((CONV_WIDTH,), 0.02)
    inp["l0_w_out"] = nrm((MIX_OUT, D_MODEL), MIX_OUT ** -0.5)
    add_ffn("l0_ffn2")
    add_ffn("l1_ffn1")
    inp["l1_mix_pre_g"] = gain()
    inp["l1_mix_post_g"] = gain()
    inp["l1_mu"] = unif((6, D_MODEL), 0.0, 1.0)
    inp["l1_w_r"] = nrm((D_MODEL, D_MODEL), D_MODEL ** -0.5)
    inp["l1_w_k"] = nrm((D_MODEL, D_MODEL), D_MODEL ** -0.5)
    inp["l1_w_v"] = nrm((D_MODEL, D_MODEL), D_MODEL ** -0.5)
    inp["l1_w_o"] = nrm((D_MODEL, D_MODEL), D_MODEL ** -0.5)
    inp["l1_w0"] = unif((D_MODEL,), -6.0, 1.0)
    inp["l1_w_dec1"] = nrm((D_MODEL, DECAY_LORA), D_MODEL ** -0.5)
    inp["l1_w_dec2"] = nrm((DECAY_LORA, D_MODEL), 0.1 * DECAY_LORA ** -0.5)
    inp["l1_a0"] = nrm((D_MODEL,), 0.5)
    inp["l1_w_a1"] = nrm((D_MODEL, AAA_LORA), D_MODEL ** -0.5)
    inp["l1_w_a2"] = nrm((AAA_LORA, D_MODEL), 0.5 * AAA_LORA ** -0.5)
    inp["l1_w_g1"] = nrm((D_MODEL, GATE_LORA), D_MODEL ** -0.5)
    inp["l1_w_g2"] = nrm((GATE_LORA, D_MODEL), GATE_LORA ** -0.5)
    inp["l1_k_k"] = 0.85 + nrm((D_MODEL,), 0.05)
    inp["l1_k_a"] = 1.0 + nrm((D_MODEL,), 0.05)
    inp["l1_r_k"] = nrm((RWKV_HEADS, RWKV_HEAD), 0.1)
    inp["l1_lnx_g"] = gain()
    inp["l1_lnx_b"] = nrm((D_MODEL,), 0.02)
    add_ffn("l1_ffn2")
    return inp


def reference(x,
              l0_ffn1_pre_g, l0_ffn1_post_g, l0_ffn1_w_gate, l0_ffn1_w_up, l0_ffn1_w_down,
              l0_mix_pre_g, l0_mix_post_g, l0_w_in,
              l0_cmp_pe_k, l0_cmp_w1_k, l0_cmp_w2_k, l0_cmp_pe_v, l0_cmp_w1_v, l0_cmp_w2_v,
              l0_conv_w, l0_conv_b, l0_w_out,
              l0_ffn2_pre_g, l0_ffn2_post_g, l0_ffn2_w_gate, l0_ffn2_w_up, l0_ffn2_w_down,
              l1_ffn1_pre_g, l1_ffn1_post_g, l1_ffn1_w_gate, l1_ffn1_w_up, l1_ffn1_w_down,
              l1_mix_pre_g, l1_mix_post_g, l1_mu, l1_w_r, l1_w_k, l1_w_v, l1_w_o,
              l1_w0, l1_w_dec1, l1_w_dec2, l1_a0, l1_w_a1, l1_w_a2, l1_w_g1, l1_w_g2,
              l1_k_k, l1_k_a, l1_r_k, l1_lnx_g, l1_lnx_b,
              l1_ffn2_pre_g, l1_ffn2_post_g, l1_ffn2_w_gate, l1_ffn2_w_up, l1_ffn2_w_down):
    layers = [
        dict(ffn1=(l0_ffn1_pre_g, l0_ffn1_post_g, l0_ffn1_w_gate, l0_ffn1_w_up, l0_ffn1_w_down),
             norms=(l0_mix_pre_g, l0_mix_post_g),
             mixer=nsa_shortconv_mixer,
             mix=(l0_w_in, l0_cmp_pe_k, l0_cmp_w1_k, l0_cmp_w2_k, l0_cmp_pe_v, l0_cmp_w1_v,
                  l0_cmp_w2_v, l0_conv_w, l0_conv_b, l0_w_out),
             ffn2=(l0_ffn2_pre_g, l0_ffn2_post_g, l0_ffn2_w_gate, l0_ffn2_w_up, l0_ffn2_w_down)),
        dict(ffn1=(l1_ffn1_pre_g, l1_ffn1_post_g, l1_ffn1_w_gate, l1_ffn1_w_up, l1_ffn1_w_down),
             norms=(l1_mix_pre_g, l1_mix_post_g),
             mixer=rwkv7_mixer,
             mix=(l1_mu, l1_w_r, l1_w_k, l1_w_v, l1_w_o, l1_w0, l1_w_dec1, l1_w_dec2, l1_a0,
                  l1_w_a1, l1_w_a2, l1_w_g1, l1_w_g2, l1_k_k, l1_k_a, l1_r_k, l1_lnx_g, l1_lnx_b),
             ffn2=(l1_ffn2_pre_g, l1_ffn2_post_g, l1_ffn2_w_gate, l1_ffn2_w_up, l1_ffn2_w_down)),
    ]
    for i in range(DEPTH):
        p = layers[i]
        x = macaron_half_ffn(x, *p["ffn1"])
        pre_g, post_g = p["norms"]
        x = x + rmsnorm(p["mixer"](rmsnorm(x, pre_g), *p["mix"]), post_g)
        x = macaron_half_ffn(x, *p["ffn2"])
    return x
```

```python
import math
import numpy as np
from contextlib import ExitStack
import concourse.bass as bass
import concourse.mybir as mybir
from concourse.bass_utils import run_bass_kernel_spmd


F32 = mybir.dt.float32
BF16 = mybir.dt.bfloat16
ALU = mybir.AluOpType
AF = mybir.ActivationFunctionType
AX = mybir.AxisListType
EP = 30000
NSLOT = 14
ENGS = ('pe', 'act', 'dve', 'pool', 'sp')
ENGATTR = {'pe': 'tensor', 'act': 'scalar', 'dve': 'vector', 'pool': 'gpsimd', 'sp': 'sync'}


class Dep:
    __slots__ = ('w', 'r', 'excl')

    def __init__(self, excl=False):
        self.w = {}
        self.r = {}
        self.excl = excl


class _Rec:
    def __init__(self):
        self.calls = []

    def __getattr__(self, name):
        def m(*a, **k):
            self.calls.append((name, a, k))
        return m


class Prog:
    def __init__(self):
        self.nc = bass.Bass("TRN2", target_bir_lowering=False)
        self.stack = ExitStack()
        self.ops = {e: [] for e in ENGS}
        self.n = {e: 0 for e in ENGS}
        self.seen = {e: {} for e in ENGS}
        self.slot_cnt = [0] * NSLOT
        self.slot_next = 0
        self.keys = set()
        self._uid = 0

    def uid(self, p):
        self._uid += 1
        return "%s_%d" % (p, self._uid)

    def sbuf(self, shape, dt, name=None):
        return self.stack.enter_context(self.nc.sbuf_tensor("sb_" + (name or self.uid("t")), list(shape), dt))

    def psum(self, shape, dt, name=None):
        return self.stack.enter_context(self.nc.psum_tensor("ps_" + (name or self.uid("t")), list(shape), dt))

    def dram(self, name, shape, dt, kind="Internal"):
        return self.nc.dram_tensor(name, list(shape), dt, kind=kind).ap()

    def _waits(self, eng, R, W):
        need = {}

        def add(d):
            for k, v in d.items():
                if eng == 'pe' and k[0] == 'pe':
                    continue
                if self.seen[eng].get(k, 0) < v:
                    if need.get(k, 0) < v:
                        need[k] = v

        for d in R:
            add(d.w)
        for d in W:
            add(d.w)
            add(d.r)
        for k, v in need.items():
            self.seen[eng][k] = v
        return list(need.items())

    def op(self, eng, fn, R=(), W=()):
        rec = _Rec()
        fn(rec)
        assert len(rec.calls) == 1
        name_, a_, k_ = rec.calls[0]
        fn = (lambda e, name_=name_, a_=a_, k_=k_: getattr(e, name_)(*a_, **k_))
        W = list(W) + [d for d in R if d.excl]
        R = [d for d in R if not d.excl]
        waits = self._waits(eng, R, W)
        self.n[eng] += 1
        n = self.n[eng]
        key = (eng, (n - 1) // EP)
        val = (n - 1) % EP + 1
        self.keys.add(key)
        self.ops[eng].append((waits, fn, key, 1))
        for d in R:
            if d.r.get(key, 0) < val:
                d.r[key] = val
        for d in W:
            d.w = {key: val}
            d.r = {}

    def dma(self, q, out, in_, R=(), W=(), **kw):
        waits = self._waits(q, R, W)
        s = self.slot_next
        self.slot_next = (s + 1) % NSLOT
        key = ('dma', s)
        self.keys.add(key)
        prev = 16 * self.slot_cnt[s]
        if prev > 0 and self.seen[q].get(key, 0) < prev:
            waits.append((key, prev))
            self.seen[q][key] = prev
        self.slot_cnt[s] += 1
        val = 16 * self.slot_cnt[s]
        assert val < 60000
        self.ops[q].append((waits, (lambda e: e.dma_start(out=out, in_=in_, **kw)), key, 16))
        for d in R:
            d.r[key] = val
        for d in W:
            d.w = {key: val}
            d.r = {}

    def mm(self, out, lhsT, rhs, start, stop, R=(), W=(), **kw):
        self.op('pe', lambda e: e.matmul(out, lhsT, rhs, start=start, stop=stop, **kw), R, W)

    def tr(self, out, in_, ident, R=(), W=()):
        self.op('pe', lambda e: e.transpose(out, in_, ident), R, W)

    def act(self, out, in_, func, R=(), W=(), eng='act', **kw):
        self.op('act', lambda e: e.activation(out=out, in_=in_, func=func, **kw), R, W)

    def build(self):
        nc = self.nc
        sems = {}
        for k in sorted(self.keys, key=str):
            sems[k] = self.stack.enter_context(nc.semaphore("s_%s_%d" % (k[0], k[1])))
        with nc.Block() as block:
            for e in ENGS:
                oplist = self.ops[e]
                final = []
                if e == 'sp':
                    final = [(('dma', s), 16 * c) for s, c in enumerate(self.slot_cnt) if c > 0]

                def body(eng, oplist=oplist, final=final):
                    for waits, fn, key, amt in oplist:
                        ws = list(waits)
                        att = None
                        if key[0] != 'dma' and ws:
                            att = ws.pop()
                        for k, v in ws:
                            eng.wait_ge(sems[k], v)
                        ins = fn(eng)
                        if att is not None:
                            ins._wait_ge(sems[att[0]], att[1])
                        ins.then_inc(sems[key], amt)
                    for k, v in final:
                        eng.wait_ge(sems[k], v)

                if oplist or final:
                    getattr(block, ENGATTR[e])(body)
        self.stack.close()
        return nc

    def stats(self):
        return {e: len(self.ops[e]) for e in ENGS}


D = 1024
DFF = 2816
NFC = DFF // 128
EPS = 1e-6


class DT:
    def __init__(self, ap, rows):
        self.ap = ap
        self.deps = [Dep() for _ in range((rows + 127) // 128)]


class Consts:
    def __init__(self, P, cdram):
        self.ident_d = Dep()
        self.ident = P.sbuf([128, 128], BF16, "ident_sb")
        self.ident_f = P.sbuf([128, 128], F32, "ident_f")
        P.dma('sp', self.ident_f[:], cdram['ident'][:, :], W=[self.ident_d])
        P.op('dve', lambda e: e.tensor_copy(self.ident[:], self.ident_f[:]), R=[self.ident_d], W=[self.ident_d])
        self.eps1 = P.sbuf([128, 4], F32, "eps1")
        self.eps_d = Dep()
        P.op('dve', lambda e: e.memset(self.eps1[:, 0:1], EPS), W=[self.eps_d])
        P.op('dve', lambda e: e.memset(self.eps1[:, 1:2], 4 * EPS), W=[self.eps_d])


class FFNBufs:
    def __init__(self, P):
        self.wg = P.sbuf([128, 8, 2840], BF16, "wg")
        self.wu = P.sbuf([128, 8, DFF], BF16, "wu")
        self.wd = P.sbuf([128, NFC, D], BF16, "wd")
        self.wg_d = [Dep() for _ in range(8)]
        self.wu_d = [Dep() for _ in range(8)]
        self.wd_d = [Dep() for _ in range(NFC // 2)]
        self.stage = [P.sbuf([128, 2048], F32, "wstage%d" % i) for i in range(2)]
        self.stage_d = [Dep() for _ in range(2)]
        self.stage_i = 0
        self.gcol = P.sbuf([128, 8], F32, "gcol")
        self.gcol_d = Dep()
        self.gpost = P.sbuf([128, D], F32, "gpost")
        self.gpost_d = Dep()
        NX = 4
        self.NX = NX
        self.xt = [P.sbuf([128, D], F32, "xt%d" % i) for i in range(NX)]
        self.xt_d = [Dep() for _ in range(NX)]
        self.xn = [P.sbuf([128, D], BF16, "xn%d" % i) for i in range(2)]
        self.xn_d = [Dep() for _ in range(2)]
        self.junk = P.sbuf([128, D], BF16, "junk")
        self.junk_d = Dep()
        self.st = [P.sbuf([128, 8], F32, "st%d" % i) for i in range(NX)]
        self.st_d = [Dep() for _ in range(NX)]
        self.hT = [P.sbuf([128, 8, 256], BF16, "hT%d" % i) for i in range(2)]
        self.hT_d = [Dep() for _ in range(2)]
        self.sg = [P.sbuf([128, 256], F32, "sg%d" % i) for i in range(2)]
        self.sg_d = [Dep() for _ in range(2)]
        self.a = [P.sbuf([128, 256], BF16, "a%d" % i) for i in range(3)]
        self.a_d = [Dep() for _ in range(3)]
        self.t = [P.sbuf([128, D], F32, "t0")] * 2
        self.t_d = [Dep()] * 2
        self.yt = [P.sbuf([128, D], F32, "yt%d" % i) for i in range(2)]
        self.yt_d = [Dep() for _ in range(2)]
        self.psT = P.psum([128, 1024], BF16, "psT")
        self.psT_d = Dep(True)
        self.gu = [P.psum([128, 512], F32, "gu%d" % i) for i in range(2)]
        self.gu_d = [Dep(True) for _ in range(2)]
        self.o = [P.psum([128, 1024], F32, "ops%d" % i) for i in range(2)]
        self.o_d = [Dep(True) for _ in range(2)]
        self.cnt = 0
        self.xrot = 0


def ffn_load_weights(P, B, wg, wu, wd, pre_g, post_g):
    P.dma('sp', B.gcol[:], pre_g[:, :], R=[], W=[B.gcol_d])
    P.dma('sp', B.gpost[:], post_g.partition_broadcast(128), W=[B.gpost_d])
    for wsrc, wdst, wdeps in ((wg, B.wg, B.wg_d), (wu, B.wu, B.wu_d)):
        for dc in range(8):
            for hf in range(2):
                si = B.stage_i
                B.stage_i ^= 1
                st, sd = B.stage[si], B.stage_d[si]
                P.dma('sp', st[:, 0:1408], wsrc[dc * 128:(dc + 1) * 128, hf * 1408:(hf + 1) * 1408], W=[sd])
                P.act(wdst[:, dc, hf * 1408:(hf + 1) * 1408], st[:, 0:1408], AF.Copy,
                      R=[sd, B.gcol_d], W=[wdeps[dc]], scale=B.gcol[:, dc:dc + 1])
    for j in range(NFC // 2):
        si = B.stage_i
        B.stage_i ^= 1
        st, sd = B.stage[si], B.stage_d[si]
        P.dma('sp', st[:, 0:2048].rearrange("p (c n) -> p c n", c=2),
              wd[j * 256:(j + 1) * 256, :].rearrange("(c p) n -> p c n", p=128), W=[sd])
        P.op('pool', lambda e, st=st, j=j: e.tensor_copy(
            B.wd[:, 2 * j:2 * j + 2, :], st[:, 0:2048].rearrange("p (c n) -> p c n", c=2)),
            R=[sd], W=[B.wd_d[j]])


def ffn_stage(P, B, C, X, Y, T, load_q='sp', store_q='act'):
    nblk = T // 256
    wdeps_gu = B.wg_d + B.wu_d

    def prep(b):
        hb = b % 2
        for tt in range(2):
            tile = b * 2 + tt
            xi = tile % B.NX
            xt, xd = B.xt[xi], B.xt_d[xi]
            st, sd = B.st[xi], B.st_d[xi]
            P.dma(load_q, xt[:], X.ap[tile * 128:(tile + 1) * 128, :], R=[X.deps[tile]], W=[xd])
            P.act(B.junk[:], xt[:], AF.Square, R=[xd], W=[B.junk_d, sd], accum_out=st[:, 0:1])
            P.act(st[:, 1:2], st[:, 0:1], AF.Sqrt, R=[sd, C.eps_d], W=[sd], scale=1.0 / D, bias=C.eps1[:, 0:1])
            P.op('dve', lambda e, st=st: e.reciprocal(st[:, 2:3], st[:, 1:2]), R=[sd], W=[sd])
            xn, xnd = B.xn[tt], B.xn_d[tt]
            P.act(xn[:], xt[:], AF.Copy, R=[xd, sd], W=[xnd], scale=st[:, 2:3])
            for dc in range(8):
                P.tr(B.psT[:, dc * 128:(dc + 1) * 128], xn[:, dc * 128:(dc + 1) * 128], C.ident[:],
                     R=[xnd, C.ident_d], W=[B.psT_d])
            P.op('dve', lambda e, hb=hb, tt=tt: e.tensor_copy(
                B.hT[hb][:, :, tt * 128:(tt + 1) * 128], B.psT[:, :].rearrange("p (c n) -> p c n", c=8)),
                R=[B.psT_d], W=[B.hT_d[hb]])

    def down(b, fc):
        ai = B.cnt_a[(b, fc)]
        for tt in range(2):
            for half in range(2):
                P.mm(B.o[tt][:, half * 512:(half + 1) * 512], B.a[ai][:, tt * 128:(tt + 1) * 128],
                     B.wd[:, fc, half * 512:(half + 1) * 512], start=(fc == 0), stop=(fc == NFC - 1),
                     R=[B.a_d[ai], B.wd_d[fc // 2]], W=[B.o_d[tt]])

    def post(b):
        for tt in range(2):
            tile = b * 2 + tt
            xi = tile % B.NX
            xt, xd = B.xt[xi], B.xt_d[xi]
            st, sd = B.st[xi], B.st_d[xi]
            P.act(B.junk[:], B.o[tt][:], AF.Square, R=[B.o_d[tt]], W=[B.junk_d, sd], accum_out=st[:, 4:5])
            P.act(st[:, 5:6], st[:, 4:5], AF.Sqrt, R=[sd, C.eps_d], W=[sd], scale=4.0 / D, bias=C.eps1[:, 1:2])
            P.op('dve', lambda e, st=st: e.reciprocal(st[:, 6:7], st[:, 5:6]), R=[sd], W=[sd])
            t, td = B.t[tt], B.t_d[tt]
            P.op('dve', lambda e, t=t, tt=tt: e.tensor_tensor(t[:], B.o[tt][:], B.gpost[:], ALU.mult),
                 R=[B.o_d[tt], B.gpost_d], W=[td])
            yt, yd = B.yt[tt], B.yt_d[tt]
            P.op('dve', lambda e, t=t, yt=yt, st=st, xt=xt: e.scalar_tensor_tensor(
                yt[:], t[:], st[:, 6:7], xt[:], ALU.mult, ALU.add),
                R=[td, sd, xd], W=[yd])
            P.dma(store_q, Y.ap[tile * 128:(tile + 1) * 128, :], yt[:], R=[yd], W=[Y.deps[tile]])

    B.cnt_a = {}
    prep(0)
    for b in range(nblk):
        hb = b % 2
        if b + 1 < nblk:
            prep(b + 1)
        for fc in range(NFC):
            gi = B.cnt % 2
            B.cnt += 1
            gu, gud = B.gu[gi], B.gu_d[gi]
            for which, wsb, wdp in ((0, B.wg, B.wg_d), (1, B.wu, B.wu_d)):
                for dc in range(8):
                    P.mm(gu[:, which * 256:(which + 1) * 256], wsb[:, dc, fc * 128:(fc + 1) * 128],
                         B.hT[hb][:, dc, :], start=(dc == 0), stop=(dc == 7),
                         R=[wdp[dc], B.hT_d[hb]], W=[gud])
            sg, sgd = B.sg[gi], B.sg_d[gi]
            P.act(sg[:], gu[:, 0:256], AF.Silu, R=[gud], W=[sgd])
            ai = (b * NFC + fc) % 3
            B.cnt_a[(b, fc)] = ai
            P.op('dve', lambda e, ai=ai, sg=sg, gu=gu: e.tensor_tensor(B.a[ai][:], sg[:], gu[:, 256:512], ALU.mult),
                 R=[sgd, gud], W=[B.a_d[ai]])
            if fc >= 2:
                down(b, fc - 2)
        down(b, NFC - 2)
        down(b, NFC - 1)
        post(b)


def norm_transpose(P, B, C, src_ap, src_deps, tile, hT_ap, hT_d, do_norm=True):
    xi = B.xrot % B.NX
    B.xrot += 1
    xt, xd = B.xt[xi], B.xt_d[xi]
    st, sd = B.st[xi], B.st_d[xi]
    xn, xnd = B.xn[xi % 2], B.xn_d[xi % 2]
    P.dma('sp', xt[:], src_ap[tile * 128:(tile + 1) * 128, :], R=src_deps, W=[xd])
    if do_norm:
        P.act(B.junk[:], xt[:], AF.Square, R=[xd], W=[B.junk_d, sd], accum_out=st[:, 0:1])
        P.act(st[:, 1:2], st[:, 0:1], AF.Sqrt, R=[sd, C.eps_d], W=[sd], scale=1.0 / D, bias=C.eps1[:, 0:1])
        P.op('dve', lambda e: e.reciprocal(st[:, 2:3], st[:, 1:2]), R=[sd], W=[sd])
        P.act(xn[:], xt[:], AF.Copy, R=[xd, sd], W=[xnd], scale=st[:, 2:3])
    else:
        P.act(xn[:], xt[:], AF.Copy, R=[xd], W=[xnd])
    for dc in range(8):
        P.tr(B.psT[:, dc * 128:(dc + 1) * 128], xn[:, dc * 128:(dc + 1) * 128], C.ident[:], R=[xnd, C.ident_d], W=[B.psT_d])
    P.op('dve', lambda e: e.tensor_copy(hT_ap, B.psT[:, :].rearrange("p (c n) -> p c n", c=8)), R=[B.psT_d], W=[hT_d])
    return xt, xd, st, sd


def inproj_stage(P, B, C, X, T, w_in, gcol_in, proj_out):
    P.dma('sp', B.gcol[:], gcol_in[:, :], W=[B.gcol_d])
    for dc in range(8):
        for hf in range(2):
            si = B.stage_i
            B.stage_i ^= 1
            st, sd = B.stage[si], B.stage_d[si]
            P.dma('sp', st[:, 0:1420], w_in[dc * 128:(dc + 1) * 128, hf * 1420:(hf + 1) * 1420], W=[sd])
            P.act(B.wg[:, dc, hf * 1420:(hf + 1) * 1420], st[:, 0:1420], AF.Copy, R=[sd, B.gcol_d], W=[B.wg_d[dc]],
                  scale=B.gcol[:, dc:dc + 1])
    banks = [(B.gu[0][:, :], B.gu_d[0]), (B.gu[1][:, :], B.gu_d[1]), (B.o[0][:, 0:512], B.o_d[0]), (B.o[1][:, 0:512], B.o_d[1])]
    groups = [(0, 512), (512, 1024), (1024, 1536), (1536, 2048), (2048, 2560), (2560, 2840)]
    k = 0
    for tile in range(T // 128):
        hb = tile % 2
        hT_ap = B.hT[hb][:, :, 0:128]
        norm_transpose(P, B, C, X.ap, [X.deps[tile]], tile, hT_ap, B.hT_d[hb])
        for (c0, c1) in groups:
            bk, bkd = banks[k % 4]
            pr, prd = B.yt[k % 2], B.yt_d[k % 2]
            k += 1
            n = c1 - c0
            for dc in range(8):
                P.mm(bk[:, 0:n], B.hT[hb][:, dc, 0:128], B.wg[:, dc, c0:c1], dc == 0, dc == 7, R=[B.hT_d[hb], B.wg_d[dc]], W=[bkd])
            if k % 2:
                P.act(pr[:, 0:n], bk[:, 0:n], AF.Copy, R=[bkd], W=[prd])
            else:
                P.op('dve', lambda e: e.tensor_copy(pr[:, 0:n], bk[:, 0:n]), R=[bkd], W=[prd])
            P.dma('act', proj_out[tile * 128:(tile + 1) * 128, c0:c1], pr[:, 0:n], R=[prd])


def projres_stage(P, B, C, T, a_src, W, post_g, Xres, Y, conv=None):
    for j in range(4):
        si = B.stage_i
        B.stage_i ^= 1
        st, sd = B.stage[si], B.stage_d[si]
        P.dma('sp', st[:, 0:2048].rearrange("p (c n) -> p c n", c=2), W[j * 256:(j + 1) * 256, :].rearrange("(c p) n -> p c n", p=128), W=[sd])
        P.op('pool', lambda e: e.tensor_copy(B.wd[:, 2 * j:2 * j + 2, :], st[:, 0:2048].rearrange("p (c n) -> p c n", c=2)),
             R=[sd], W=[B.wd_d[j]])
    P.dma('sp', B.gpost[:], post_g.partition_broadcast(128), W=[B.gpost_d])
    if conv is not None:
        cwk = [B.wu[:, kk, 0:1024].bitcast(F32) for kk in range(4)]
        cwk_d = [B.wu_d[kk] for kk in range(4)]
        for kk in range(3):
            P.dma('sp', cwk[kk][:, :], conv['w'][kk, :].partition_broadcast(128), W=[cwk_d[kk]])
        P.dma('sp', cwk[3][:, :], conv['b'].partition_broadcast(128), W=[cwk_d[3]])
        cv = [B.wu[:, 4 + i, 0:2048].bitcast(F32) for i in range(3)]
        cv_d = [B.wu_d[4 + i] for i in range(3)]
        cbt = B.wu[:, 7, 0:1024].bitcast(F32)
        cbt_d = B.wu_d[7]
    for tile in range(T // 128):
        ai = B.xrot % B.NX
        B.xrot += 1
        at, ad = B.xt[ai], B.xt_d[ai]
        an, and_ = B.xn[ai % 2], B.xn_d[ai % 2]
        if conv is None:
            P.dma('sp', at[:], a_src[tile * 128:(tile + 1) * 128, :], W=[ad])
        else:
            P.dma('sp', at[:, 0:512], a_src[tile * 128:(tile + 1) * 128, :], W=[ad])
            r0 = tile * 128
            for sh in range(3):
                P.dma('sp', cv[sh][:, :], conv['cbcu'][r0 + sh:r0 + sh + 128, 512:1536], W=[cv_d[sh]])
            P.dma('sp', cbt[:, :], conv['cbcu'][r0 + 2:r0 + 130, 0:512], W=[cbt_d])
            for sh in range(3):
                P.op('dve', lambda e: e.tensor_tensor(cv[sh][:, 0:512], cv[sh][:, 0:512], cv[sh][:, 512:1024], ALU.mult),
                     R=[cv_d[sh]], W=[cv_d[sh]])
                P.op('dve', lambda e: e.tensor_tensor(cv[sh][:, 0:512], cv[sh][:, 0:512], cwk[sh][:, :], ALU.mult),
                     R=[cv_d[sh], cwk_d[sh]], W=[cv_d[sh]])
            P.op('dve', lambda e: e.tensor_tensor(cv[0][:, 0:512], cv[0][:, 0:512], cv[1][:, 0:512], ALU.add), R=[cv_d[1]], W=[cv_d[0]])
            P.op('dve', lambda e: e.tensor_tensor(cv[0][:, 0:512], cv[0][:, 0:512], cv[2][:, 0:512], ALU.add), R=[cv_d[2]], W=[cv_d[0]])
            P.op('dve', lambda e: e.tensor_tensor(cv[0][:, 0:512], cv[0][:, 0:512], cwk[3][:, :], ALU.add), R=[cwk_d[3]], W=[cv_d[0]])
            P.op('dve', lambda e: e.tensor_tensor(at[:, 512:1024], cv[0][:, 0:512], cbt[:, :], ALU.mult), R=[cv_d[0], cbt_d], W=[ad])
        P.act(an[:], at[:], AF.Copy, R=[ad], W=[and_])
        hb = tile % 2
        for dc in range(8):
            P.tr(B.psT[:, dc * 128:(dc + 1) * 128], an[:, dc * 128:(dc + 1) * 128], C.ident[:], R=[and_, C.ident_d], W=[B.psT_d])
        P.op('dve', lambda e: e.tensor_copy(B.hT[hb][:, :, 0:128], B.psT[:, :].rearrange("p (c n) -> p c n", c=8)),
             R=[B.psT_d], W=[B.hT_d[hb]])
        tt = tile % 2
        for half in range(2):
            for fc in range(8):
                P.mm(B.o[tt][:, half * 512:(half + 1) * 512], B.hT[hb][:, fc, 0:128], B.wd[:, fc, half * 512:(half + 1) * 512],
                     fc == 0, fc == 7, R=[B.hT_d[hb], B.wd_d[fc // 2]], W=[B.o_d[tt]])
        xi = B.xrot % B.NX
        B.xrot += 1
        xt, xd = B.xt[xi], B.xt_d[xi]
        st, sd = B.st[xi], B.st_d[xi]
        P.dma('sp', xt[:], Xres.ap[tile * 128:(tile + 1) * 128, :], R=[Xres.deps[tile]], W=[xd])
        P.act(B.junk[:], B.o[tt][:], AF.Square, R=[B.o_d[tt]], W=[B.junk_d, sd], accum_out=st[:, 4:5])
        P.act(st[:, 5:6], st[:, 4:5], AF.Sqrt, R=[sd, C.eps_d], W=[sd], scale=1.0 / D, bias=C.eps1[:, 0:1])
        P.op('dve', lambda e: e.reciprocal(st[:, 6:7], st[:, 5:6]), R=[sd], W=[sd])
        t, td = B.t[tt], B.t_d[tt]
        P.op('dve', lambda e: e.tensor_tensor(t[:], B.o[tt][:], B.gpost[:], ALU.mult), R=[B.o_d[tt], B.gpost_d], W=[td])
        yt, yd = B.yt[tt], B.yt_d[tt]
        P.op('dve', lambda e: e.scalar_tensor_tensor(yt[:], t[:], st[:, 6:7], xt[:], ALU.mult, ALU.add), R=[td, sd, xd], W=[yd])
        P.dma('act', Y.ap[tile * 128:(tile + 1) * 128, :], yt[:], R=[yd], W=[Y.deps[tile]])


D = 1024
EPS = 1e-6
LNX_EPS = 64e-5
CW = math.exp(-0.5)


class Tl:
    def __init__(self, P, shape, dt, name):
        self.t = P.sbuf(shape, dt, name)
        self.d = Dep()


class Banks:
    def __init__(self, P, n=7):
        self.b = [P.psum([128, 512], F32, "bank%d" % i) for i in range(n)]
        self.d = [Dep(True) for _ in range(n)]
        self.i = 0
        self.n = n

    def get(self):
        i = self.i
        self.i = (i + 1) % self.n
        return self.b[i], self.d[i]


def rwkv_consts_np():
    p = np.arange(128)[:, None]
    f = np.arange(128)[None, :]
    c = {}
    c['ident'] = np.eye(128, dtype=np.float32)
    c['negSU'] = -(p < f).astype(np.float32)
    c['negSL'] = -(f < p).astype(np.float32)
    c['SU'] = (p < f).astype(np.float32)
    c['U'] = (p <= f).astype(np.float32)
    c['TriS'] = np.ones((128, 128), np.float32)
    c['OnesS'] = np.ones((128, 128), np.float32)
    return np.stack([c[k] for k in ('ident', 'negSU', 'negSL', 'SU', 'U', 'TriS', 'OnesS')], 0)


def rwkv_stage(P, S, xpad, zout, w, cmat):
    NT = S // 128
    BK = Banks(P, 7)
    psT = P.psum([128, 1024], BF16, "psT")
    psT_d = Dep(True)
    cf = Tl(P, [128, 7, 128], F32, "cf")
    P.dma('sp', cf.t[:], cmat.rearrange("k p f -> p k f"), W=[cf.d])
    cb = Tl(P, [128, 6, 128], BF16, "cb")
    P.op('dve', lambda e: e.tensor_copy(cb.t[:], cf.t[:, 0:6, :]), R=[cf.d], W=[cb.d])
    ident = cb.t[:, 0, :]

    def bc4(k):
        return cb.t[:, k, :].unsqueeze(1).broadcast_to([128, 4, 128])
    TriS = cf.t[:, 5, :]
    OnesS = cf.t[:, 6, :]
    onescol = cf.t[:, 6, 0:1]
    epsc = Tl(P, [128, 2], F32, "epsc")
    P.op('dve', lambda e: e.memset(epsc.t[:, 0:1], EPS), W=[epsc.d])
    P.op('dve', lambda e: e.memset(epsc.t[:, 1:2], LNX_EPS), W=[epsc.d])
    Wall = Tl(P, [128, 8, 2, 1024], BF16, "Wall")
    gcol = Tl(P, [128, 8], F32, "gcol")
    mucol = Tl(P, [128, 6, 8], F32, "mucol")
    P.dma('sp', gcol.t[:], w['gmu'][:, 0, :], W=[gcol.d])
    P.dma('sp', mucol.t[:], w['gmu'][:, 1:7, :], W=[mucol.d])
    mucol_all = Dep()
    sc = Tl(P, [128, 6, 2, 8], F32, "sc")
    for m in range(6):
        P.op('dve', lambda e, m=m: e.tensor_tensor(sc.t[:, m, 1, :], mucol.t[:, m, :], gcol.t[:, :], ALU.mult),
             R=[gcol.d, mucol.d], W=[sc.d])
        P.op('dve', lambda e, m=m: e.tensor_tensor(sc.t[:, m, 0, :], gcol.t[:, :], sc.t[:, m, 1, :], ALU.subtract),
             R=[gcol.d], W=[sc.d])
    stg = [Tl(P, [128, 256], F32, "stg%d" % i) for i in range(2)]
    si = 0
    for name, m, c0, n in (('w_r', 0, 0, 256), ('w_k', 2, 256, 256), ('w_v', 3, 512, 256),
                           ('w_dec1', 1, 768, 64), ('w_a1', 4, 832, 64), ('w_g1', 5, 896, 128)):
        for dc in range(8):
            st = stg[si]
            si ^= 1
            P.dma('sp', st.t[:, 0:n], w[name][dc * 128:(dc + 1) * 128, :], W=[st.d])
            for cp in range(2):
                P.act(Wall.t[:, dc, cp, c0:c0 + n], st.t[:, 0:n], AF.Copy, R=[st.d, sc.d], W=[Wall.d],
                      scale=sc.t[:, m, cp, dc:dc + 1])
    W2A = Tl(P, [128, 256], BF16, "W2A")
    W2G = Tl(P, [128, 256], BF16, "W2G")
    st = stg[si]; si ^= 1
    P.dma('sp', st.t[0:64, :], w['w_dec2'][:, :], W=[st.d])
    st2 = stg[si]; si ^= 1
    P.dma('sp', st2.t[64:128, :], w['w_a2'][:, :], W=[st2.d])
    P.op('dve', lambda e, st=st: e.tensor_copy(W2A.t[0:64, :], st.t[0:64, :]), R=[st.d], W=[W2A.d])
    P.op('dve', lambda e, st2=st2: e.tensor_copy(W2A.t[64:128, :], st2.t[64:128, :]), R=[st2.d], W=[W2A.d])
    st = stg[si]; si ^= 1
    P.dma('sp', st.t[:, :], w['w_g2'][:, :], W=[st.d])
    P.op('dve', lambda e, st=st: e.tensor_copy(W2G.t[:, :], st.t[:, :]), R=[st.d], W=[W2G.d])
    bcn = ('w0', 'a0', 'k_k', 'k_a', 'r_k', 'lnx_g', 'lnx_b')
    bct = Tl(P, [128, 7, 256], F32, "bct")
    bcd = [Dep() for _ in bcn]
    for i, nme in enumerate(bcn):
        P.dma('sp', bct.t[:, i, :], w[nme].partition_broadcast(128), W=[bcd[i]])
    BCI = {n: i for i, n in enumerate(bcn)}

    def bc(nme):
        return bct.t[:, BCI[nme], :], bcd[BCI[nme]]

    H = [Tl(P, [128, 64], F32, "H%d" % i) for i in range(2)]
    Hb = [Tl(P, [128, 64], BF16, "Hb%d" % i) for i in range(2)]
    for i in range(2):
        P.op('dve', lambda e, i=i: e.memset(H[i].t[:], 0.0), W=[H[i].d])
        P.op('dve', lambda e, i=i: e.memset(Hb[i].t[:], 0.0), W=[Hb[i].d])
    hT = [Tl(P, [128, 8, 129], BF16, "hT%d" % i) for i in range(2)]
    P.op('dve', lambda e: e.memset(hT[0].t[:, :, 0:1], 0.0), W=[hT[0].d])

    def mk(shape, dt, name, n=2):
        return [Tl(P, shape, dt, "%s_%d" % (name, i)) for i in range(n)]
    xt = mk([128, D], F32, "xt")
    xn = mk([128, D], BF16, "xn")
    junk = Tl(P, [128, D], BF16, "junk")
    st8 = mk([128, 8], F32, "st8")
    TA = mk([128, 128], BF16, "TA")
    TB = mk([128, 128], BF16, "TB")
    names_f = ['r_sb', 'k_sb', 'v_sb', 'wpre', 'sg', 'apre', 'a_sb', 'g_sb', 'kkr', 'sq', 'kkn', 't1', 'k2', 'ka',
               'cum', 'E1', 'E2', 'E3', 'E4', 'd4', 'd2', 'y_sb', 'yc', 'ysq', 'bon', 'zt']
    LONG = ('r_sb', 'k2', 'v_sb', 'g_sb')
    F = {n: mk([128, 256], F32, n, 4 if n in LONG else 2) for n in names_f}
    s4 = mk([128, 16], F32, "s4f")
    s4b = mk([128, 16], F32, "s4b")
    gC = mk([128, 2], F32, "gC", 4)
    vb = mk([128, 256], BF16, "vb", 4)
    QT = mk([128, 4, 256], BF16, "QT")
    bhat = mk([128, 256], BF16, "bhat", 4)
    khat = mk([128, 256], BF16, "khat", 4)
    FT = mk([128, 8, 128], BF16, "FT", 4)
    Cm = {n: mk([128, 512], BF16, n, 4) for n in ('AkT', 'RbT', 'RkT')}
    NM = {n: [mk([128, 512], BF16, '%s%d' % (n, s_), 3) for s_ in range(4 if n == 'Pm' else 2)] for n in ('N', 'M', 'Pm')}
    Wn = mk([128, 256], BF16, "Wn")
    sghi = mk([128, 256], BF16, "sghi")
    sglo = mk([128, 256], BF16, "sglo")
    Ub = mk([128, 256], BF16, "Ub")

    def v4(ap):
        return ap.rearrange("p (h j) -> p h j", h=4)

    def b4(ap4):
        return ap4.unsqueeze(2).broadcast_to([128, 4, 64])

    def g5(ap):
        return ap.rearrange("p (h n) -> p h n", h=4)

    CTX = {}

    def front(tau):
        q = tau % 2
        q4 = tau % 4
        X, XN, ST, HT = xt[q], xn[q], st8[q], hT[q]
        P.dma('sp', X.t[:], xpad[tau * 128 + 1: tau * 128 + 129, :], W=[X.d])
        P.act(junk.t[:], X.t[:], AF.Square, R=[X.d], W=[junk.d, ST.d], accum_out=ST.t[:, 0:1])
        P.act(ST.t[:, 1:2], ST.t[:, 0:1], AF.Sqrt, R=[ST.d, epsc.d], W=[ST.d], scale=1.0 / D, bias=epsc.t[:, 0:1])
        P.op('dve', lambda e, ST=ST: e.reciprocal(ST.t[:, 2:3], ST.t[:, 1:2]), R=[ST.d], W=[ST.d])
        P.act(XN.t[:], X.t[:], AF.Copy, R=[X.d, ST.d], W=[XN.d], scale=ST.t[:, 2:3])
        for dc in range(8):
            P.tr(psT[:, dc * 128:(dc + 1) * 128], XN.t[:, dc * 128:(dc + 1) * 128], ident, R=[XN.d, cb.d], W=[psT_d])
        P.op('dve', lambda e, HT=HT: e.tensor_copy(HT.t[:, :, 1:129], psT[:, :].rearrange("p (c n) -> p c n", c=8)),
             R=[psT_d], W=[HT.d])
        if tau > 0:
            HP = hT[1 - q]
            P.op('dve', lambda e, HT=HT, HP=HP: e.tensor_copy(HT.t[:, :, 0:1], HP.t[:, :, 128:129]),
                 R=[HP.d], W=[HT.d])
        yield
        bA, dA = BK.get()
        bB, dB = BK.get()
        bC, dC = BK.get()
        bD, dD = BK.get()

        def proj(out, c0, c1, dep):
            k = 0
            for cp in range(2):
                for dc in range(8):
                    P.mm(out, HT.t[:, dc, 1 - cp:129 - cp], Wall.t[:, dc, cp, c0:c1], k == 0, k == 15,
                         R=[HT.d, Wall.d], W=[dep])
                    k += 1
        proj(bA[:, 0:512], 0, 512, dA)
        proj(bB[:, 0:256], 512, 768, dB)
        for (o0, c0) in ((0, 768), (128, 896)):
            k = 0
            for cp in range(2):
                for dc in range(8):
                    P.mm(bC[:, o0:o0 + 128], Wall.t[:, dc, cp, c0:c0 + 128], HT.t[:, dc, 1 - cp:129 - cp], k == 0, k == 15,
                         R=[HT.d, Wall.d], W=[dC])
                    k += 1
        ta, tb = TA[q], TB[q]
        P.act(ta.t[0:64, :], bC[0:64, 0:128], AF.Tanh, R=[dC], W=[ta.d])
        P.act(ta.t[64:128, :], bC[64:128, 0:128], AF.Copy, R=[dC], W=[ta.d])
        P.act(tb.t[:, :], bC[:, 128:256], AF.Sigmoid, R=[dC], W=[tb.d])
        P.mm(bB[:, 256:512], ta.t[0:64, :], W2A.t[0:64, :], True, True, R=[ta.d, W2A.d], W=[dB])
        P.mm(bD[:, 0:256], ta.t[64:128, :], W2A.t[64:128, :], True, True, R=[ta.d, W2A.d], W=[dD])
        P.mm(bD[:, 256:512], tb.t[:, :], W2G.t[:, :], True, True, R=[tb.d, W2G.d], W=[dD])
        f = {n: F[n][q4 if n in LONG else q] for n in names_f}

        def A_(out, in_, func, R, **kw):
            P.act(out.t[:] if isinstance(out, Tl) else out, in_, func, R=R, W=[out.d] if isinstance(out, Tl) else [], **kw)

        def TT(out, a, b, op, R):
            P.op('dve', lambda e: e.tensor_tensor(out.t[:], a, b, op), R=R, W=[out.d])

        def STT(out, a, s, b, op0, op1, R):
            P.op('dve', lambda e: e.scalar_tensor_tensor(out.t[:], a, s, b, op0, op1), R=R, W=[out.d])
        A_(f['r_sb'], bA[:, 0:256], AF.Copy, [dA])
        A_(f['k_sb'], bA[:, 256:512], AF.Copy, [dA])
        A_(f['v_sb'], bB[:, 0:256], AF.Copy, [dB])
        VB = vb[q4]
        P.op('dve', lambda e, VB=VB: e.tensor_copy(VB.t[:], f['v_sb'].t[:]), R=[f['v_sb'].d], W=[VB.d])
        t_, d_ = bc('w0')
        TT(f['wpre'], bB[:, 256:512], t_, ALU.add, [dB, d_])
        A_(f['sg'], f['wpre'].t[:], AF.Sigmoid, [f['wpre'].d])
        t_, d_ = bc('a0')
        TT(f['apre'], bD[:, 0:256], t_, ALU.add, [dD, d_])
        A_(f['a_sb'], f['apre'].t[:], AF.Sigmoid, [f['apre'].d])
        A_(f['g_sb'], bD[:, 256:512], AF.Copy, [dD])
        yield
        t_, d_ = bc('k_k')
        TT(f['kkr'], f['k_sb'].t[:], t_, ALU.mult, [f['k_sb'].d, d_])
        TT(f['sq'], f['kkr'].t[:], f['kkr'].t[:], ALU.mult, [f['kkr'].d])
        S4 = s4[q]
        P.op('dve', lambda e, S4=S4: e.tensor_reduce(S4.t[:, 0:4], v4(f['sq'].t[:, :]), AX.X, ALU.add),
             R=[f['sq'].d], W=[S4.d])
        P.op('dve', lambda e, S4=S4: e.tensor_scalar(S4.t[:, 0:4], S4.t[:, 0:4], 1e-12, None, ALU.max), R=[S4.d], W=[S4.d])
        P.act(S4.t[:, 4:8], S4.t[:, 0:4], AF.Sqrt, R=[S4.d], W=[S4.d])
        P.op('dve', lambda e, S4=S4: e.reciprocal(S4.t[:, 8:12], S4.t[:, 4:8]), R=[S4.d], W=[S4.d])
        P.op('dve', lambda e, S4=S4: e.tensor_tensor(v4(f['kkn'].t[:, :]), v4(f['kkr'].t[:, :]), b4(S4.t[:, 8:12]), ALU.mult),
             R=[f['kkr'].d, S4.d], W=[f['kkn'].d])
        yield
        t_, d_ = bc('k_a')
        STT(f['t1'], f['a_sb'].t[:], -1.0, t_, ALU.add, ALU.mult, [f['a_sb'].d, d_])
        STT(f['k2'], f['t1'].t[:], 1.0, f['k_sb'].t[:], ALU.add, ALU.mult, [f['t1'].d, f['k_sb'].d])
        TT(f['ka'], f['kkn'].t[:], f['a_sb'].t[:], ALU.mult, [f['kkn'].d, f['a_sb'].d])
        yield
        SH, SL_ = sghi[q], sglo[q]
        P.op('dve', lambda e: e.tensor_copy(SH.t[:], f['sg'].t[:]), R=[f['sg'].d], W=[SH.d])
        P.op('dve', lambda e: e.tensor_tensor(SL_.t[:], f['sg'].t[:], SH.t[:], ALU.subtract), R=[f['sg'].d, SH.d], W=[SL_.d])
        bE, dE = BK.get()
        for i_, S_ in enumerate((SH, SL_)):
            P.mm(bE[:, 0:256], cb.t[:, 4, :], S_.t[:], i_ == 0, i_ == 1, R=[cb.d, S_.d], W=[dE])
        for i_, S_ in enumerate((SH, SL_)):
            P.mm(bE[:, 256:512], cb.t[:, 5, :], S_.t[:], i_ == 0, i_ == 1, R=[cb.d, S_.d], W=[dE])
        bF, dF = BK.get()
        for pr in range(2):
            for i_, S_ in enumerate((SH, SL_)):
                P.mm(bF[:, pr * 128:(pr + 1) * 128], S_.t[:, pr * 128:(pr + 1) * 128], cb.t[:, 5, :], i_ == 0, i_ == 1,
                     R=[cb.d, S_.d], W=[dF])
        A_(f['cum'], bE[:, 0:256], AF.Copy, [dE], scale=-CW)
        STT(f['d4'], bE[:, 256:512], -CW, f['cum'].t[:], ALU.mult, ALU.subtract, [dE, f['cum'].d])
        STT(f['d2'], f['sg'].t[:], CW, f['cum'].t[:], ALU.mult, ALU.add, [f['sg'].d, f['cum'].d])
        A_(f['E1'], f['cum'].t[:], AF.Exp, [f['cum'].d])
        A_(f['E3'], f['cum'].t[:], AF.Exp, [f['cum'].d], scale=-1.0)
        A_(f['E4'], f['d4'].t[:], AF.Exp, [f['d4'].d])
        A_(f['E2'], f['d2'].t[:], AF.Exp, [f['d2'].d])
        GC = gC[q4]
        P.act(GC.t[:, 0:1], bF[:, 0:1], AF.Exp, R=[dF], W=[GC.d], scale=-CW)
        P.act(GC.t[:, 1:2], bF[:, 128:129], AF.Exp, R=[dF], W=[GC.d], scale=-CW)
        yield
        QTq = QT[q]
        for ty, (a, b) in enumerate((('r_sb', 'E1'), ('kkn', 'E2'), ('ka', 'E3'), ('k2', 'E3'))):
            P.op('dve', lambda e, ty=ty, a=a, b=b, QTq=QTq: e.tensor_tensor(QTq.t[:, ty, :], f[a].t[:], f[b].t[:], ALU.mult),
                 R=[f[a].d, f[b].d], W=[QTq.d])
        BH, KH = bhat[q4], khat[q4]
        P.op('dve', lambda e, BH=BH: e.tensor_tensor(BH.t[:], f['ka'].t[:], f['E4'].t[:], ALU.mult),
             R=[f['ka'].d, f['E4'].d], W=[BH.d])
        P.op('dve', lambda e, KH=KH: e.tensor_tensor(KH.t[:], f['k2'].t[:], f['E4'].t[:], ALU.mult),
             R=[f['k2'].d, f['E4'].d], W=[KH.d])
        yield
        for ty in range(4):
            for pair in range(2):
                idx = ty * 2 + pair
                P.tr(psT[:, idx * 128:(idx + 1) * 128], QTq.t[:, ty, pair * 128:(pair + 1) * 128], ident,
                     R=[QTq.d, cb.d], W=[psT_d])
        FTq = FT[q4]
        P.op('dve', lambda e, FTq=FTq: e.tensor_copy(FTq.t[:, :, :], psT[:, :].rearrange("p (c n) -> p c n", c=8)),
             R=[psT_d], W=[FTq.d])

        def fm(h, ty):
            pair, hh = h // 2, h % 2
            return FTq.t[hh * 64:(hh + 1) * 64, ty * 2 + pair, :]
        yield
        Nk, Mk, Pk = NM['N'][q][0], NM['M'][q][0], NM['Pm'][q4][0]
        AkT, RbT, RkT = Cm['AkT'][q4], Cm['RbT'][q4], Cm['RkT'][q4]

        def hv(ap, hh):
            return ap.rearrange("p (a b n) -> p a b n", a=2, b=2)[:, :, hh, :]

        def bc2(k):
            return cb.t[:, k, :].unsqueeze(1).broadcast_to([128, 2, 128])
        for T_, (la, ra), mi in ((Nk, (2, 1), 1), (Mk, (1, 2), 2), (AkT, (3, 1), 3), (RbT, (2, 0), 4), (RkT, (3, 0), 4)):
            for hh in range(2):
                bk, dk = BK.get()
                for pair in range(2):
                    h = pair * 2 + hh
                    P.mm(bk[:, pair * 128:(pair + 1) * 128], fm(h, la), fm(h, ra), True, True, R=[FTq.d], W=[dk])
                P.op('dve', lambda e: e.tensor_tensor(hv(T_.t[:, :], hh), bk[:, 0:256].rearrange("p (a n) -> p a n", a=2),
                                                      bc2(mi), ALU.mult), R=[dk, cb.d], W=[T_.d])
            yield
        P.op('dve', lambda e: e.tensor_tensor(g5(Pk.t[:, :]), g5(Nk.t[:, :]), bc4(0), ALU.add),
             R=[Nk.d, cb.d], W=[Pk.d])
        yield
        cur = 0
        for lvl in range(6):
            nxt = (cur + 1) % 3
            Nn, Mn = NM['N'][q][nxt], NM['M'][q][nxt]
            bm, dm = BK.get()
            for h in range(4):
                blk = slice(h * 128, (h + 1) * 128)
                P.mm(bm[:, blk], Nk.t[:, blk], Mk.t[:, blk], True, True, R=[Nk.d, Mk.d], W=[dm])
            if lvl < 5:
                bn, dn = BK.get()
                for h in range(4):
                    blk = slice(h * 128, (h + 1) * 128)
                    P.mm(bn[:, blk], Mk.t[:, blk], Nk.t[:, blk], True, True, R=[Nk.d, Mk.d], W=[dn])
            if lvl >= 1:
                Pn = NM['Pm'][q4][lvl % 3]
                bp, dp = BK.get()
                for h in range(4):
                    blk = slice(h * 128, (h + 1) * 128)
                    P.mm(bp[:, blk], Mk.t[:, blk], Pk.t[:, blk], True, True, R=[Mk.d, Pk.d], W=[dp])
            if lvl < 5:
                P.act(Nn.t[:, :], bn[:, :], AF.Copy, R=[dn], W=[Nn.d])
            P.op('dve', lambda e: e.tensor_copy(Mn.t[:, :], bm[:, :]), R=[dm], W=[Mn.d])
            if lvl >= 1:
                P.op('dve', lambda e: e.tensor_tensor(Pn.t[:, :], bp[:, :], Pk.t[:, :], ALU.add), R=[dp, Pk.d], W=[Pn.d])
                Pk = Pn
            Nk, Mk = Nn, Mn
            cur = nxt
            yield
        Pn = NM['Pm'][q4][0]
        bp, dp = BK.get()
        for h in range(4):
            blk = slice(h * 128, (h + 1) * 128)
            P.mm(bp[:, blk], Mk.t[:, blk], Pk.t[:, blk], True, True, R=[Mk.d, Pk.d], W=[dp])
        P.op('dve', lambda e: e.tensor_tensor(Pn.t[:, :], bp[:, :], Pk.t[:, :], ALU.add), R=[dp, Pk.d], W=[Pn.d])
        Pk = Pn

        CTX[tau] = dict(f=f, FTq=FTq, AkT=AkT, RbT=RbT, RkT=RkT, Pk=Pk, VB=VB, BH=BH, KH=KH, GC=GC, fm=fm)


    def back(tau):
        q = tau % 2
        c_ = CTX.pop(tau)
        f, FTq, AkT, RbT, RkT, Pk, VB, BH, KH, GC, fm = (c_[k] for k in ('f', 'FTq', 'AkT', 'RbT', 'RkT', 'Pk', 'VB', 'BH', 'KH', 'GC', 'fm'))
        S4 = s4b[q]
        f = dict(f)
        for n_ in ('y_sb', 'yc', 'ysq', 'bon', 'zt'):
            f[n_] = F[n_][q]

        def A_(out, in_, func, R, **kw):
            P.act(out.t[:] if isinstance(out, Tl) else out, in_, func, R=R, W=[out.d] if isinstance(out, Tl) else [], **kw)

        def TT(out, a, b, op, R):
            P.op('dve', lambda e: e.tensor_tensor(out.t[:], a, b, op), R=R, W=[out.d])

        def STT(out, a, s, b, op0, op1, R):
            P.op('dve', lambda e: e.scalar_tensor_tensor(out.t[:], a, s, b, op0, op1), R=R, W=[out.d])
        WN = Wn[q]
        for hh in range(2):
            bw, dw = BK.get()
            for pair in range(2):
                h = pair * 2 + hh
                c = slice(h * 64, (h + 1) * 64)
                blk = slice(h * 128, (h + 1) * 128)
                P.mm(bw[:, c], fm(h, 1), Hb[pair].t[hh * 64:(hh + 1) * 64, :], True, False, R=[FTq.d, Hb[pair].d], W=[dw])
                P.mm(bw[:, c], AkT.t[:, blk], VB.t[:, c], False, True, R=[AkT.d, VB.d], W=[dw])
            for pair in range(2):
                h = pair * 2 + hh
                c = slice(h * 64, (h + 1) * 64)
                P.act(WN.t[:, c], bw[:, c], AF.Copy, R=[dw], W=[WN.d], scale=-1.0)
        yield
        bu, du = BK.get()
        for h in range(4):
            c = slice(h * 64, (h + 1) * 64)
            blk = slice(h * 128, (h + 1) * 128)
            P.mm(bu[:, c], Pk.t[:, blk], WN.t[:, c], True, True, R=[Pk.d, WN.d], W=[du])
        UB = Ub[q]
        P.op('dve', lambda e, UB=UB, bu=bu: e.tensor_copy(UB.t[:, :], bu[:, 0:256]), R=[du], W=[UB.d])
        yield
        for hh in range(2):
            by, dy = BK.get()
            for pair in range(2):
                h = pair * 2 + hh
                c = slice(h * 64, (h + 1) * 64)
                blk = slice(h * 128, (h + 1) * 128)
                P.mm(by[:, c], fm(h, 0), Hb[pair].t[hh * 64:(hh + 1) * 64, :], True, False, R=[FTq.d, Hb[pair].d], W=[dy])
                P.mm(by[:, c], RbT.t[:, blk], UB.t[:, c], False, False, R=[RbT.d, UB.d], W=[dy])
                P.mm(by[:, c], RkT.t[:, blk], VB.t[:, c], False, True, R=[RkT.d, VB.d], W=[dy])
            for pair in range(2):
                h = pair * 2 + hh
                c = slice(h * 64, (h + 1) * 64)
                P.act(f['y_sb'].t[:, c], by[:, c], AF.Copy, R=[dy], W=[f['y_sb'].d])
        yield
        bh, dh = BK.get()
        for pair in range(2):
            pc = slice(pair * 128, (pair + 1) * 128)
            P.mm(bh[:, pc], BH.t[:, pc], UB.t[:, pc], True, False, R=[BH.d, UB.d], W=[dh])
            P.mm(bh[:, pc], KH.t[:, pc], VB.t[:, pc], False, True, R=[KH.d, VB.d], W=[dh])
        for pair in range(2):
            for hh in range(2):
                rows = slice(hh * 64, (hh + 1) * 64)
                P.op('dve', lambda e, pair=pair, hh=hh, rows=rows, bh=bh, GC=GC: e.scalar_tensor_tensor(
                    H[pair].t[rows, :], H[pair].t[rows, :], GC.t[rows, pair:pair + 1],
                    bh[rows, pair * 128 + hh * 64: pair * 128 + hh * 64 + 64], ALU.mult, ALU.add),
                    R=[GC.d, dh, Hb[pair].d], W=[H[pair].d])
            P.act(Hb[pair].t[:, :], H[pair].t[:, :], AF.Copy, R=[H[pair].d], W=[Hb[pair].d])
        yield
        y = f['y_sb']
        P.op('dve', lambda e, S4=S4: e.tensor_reduce(S4.t[:, 12:16], v4(y.t[:, :]), AX.X, ALU.add), R=[y.d], W=[S4.d])
        P.op('dve', lambda e, S4=S4: e.tensor_scalar(S4.t[:, 12:16], S4.t[:, 12:16], -1.0 / 64, None, ALU.mult), R=[S4.d], W=[S4.d])
        P.op('dve', lambda e, S4=S4: e.tensor_tensor(v4(f['yc'].t[:, :]), v4(y.t[:, :]), b4(S4.t[:, 12:16]), ALU.add),
             R=[y.d, S4.d], W=[f['yc'].d])
        TT(f['ysq'], f['yc'].t[:], f['yc'].t[:], ALU.mult, [f['yc'].d])
        P.op('dve', lambda e, S4=S4: e.tensor_reduce(S4.t[:, 0:4], v4(f['ysq'].t[:, :]), AX.X, ALU.add), R=[f['ysq'].d], W=[S4.d])
        P.act(S4.t[:, 4:8], S4.t[:, 0:4], AF.Sqrt, R=[S4.d, epsc.d], W=[S4.d], scale=1.0 / 64, bias=epsc.t[:, 1:2])
        P.op('dve', lambda e, S4=S4: e.reciprocal(S4.t[:, 8:12], S4.t[:, 4:8]), R=[S4.d], W=[S4.d])
        P.op('dve', lambda e, S4=S4: e.tensor_tensor(v4(f['ysq'].t[:, :]), v4(f['yc'].t[:, :]), b4(S4.t[:, 8:12]), ALU.mult),
             R=[f['yc'].d, S4.d], W=[f['ysq'].d])
        t_, d_ = bc('lnx_g')
        TT(f['yc'], f['ysq'].t[:], t_, ALU.mult, [f['ysq'].d, d_])
        t_, d_ = bc('lnx_b')
        TT(f['ysq'], f['yc'].t[:], t_, ALU.add, [f['yc'].d, d_])
        yield
        t_, d_ = bc('r_k')
        TT(f['bon'], f['r_sb'].t[:], t_, ALU.mult, [f['r_sb'].d, d_])
        TT(f['yc'], f['bon'].t[:], f['k2'].t[:], ALU.mult, [f['bon'].d, f['k2'].d])
        P.op('dve', lambda e, S4=S4: e.tensor_reduce(S4.t[:, 12:16], v4(f['yc'].t[:, :]), AX.X, ALU.add), R=[f['yc'].d], W=[S4.d])
        P.op('dve', lambda e, S4=S4: e.tensor_tensor(v4(f['bon'].t[:, :]), v4(f['v_sb'].t[:, :]), b4(S4.t[:, 12:16]), ALU.mult),
             R=[f['v_sb'].d, S4.d], W=[f['bon'].d])
        TT(f['yc'], f['ysq'].t[:], f['bon'].t[:], ALU.add, [f['ysq'].d, f['bon'].d])
        TT(f['zt'], f['yc'].t[:], f['g_sb'].t[:], ALU.mult, [f['yc'].d, f['g_sb'].d])
        P.dma('act', zout[tau * 128:(tau + 1) * 128, :], f['zt'].t[:], R=[f['zt'].d])


    def backs(a, b):
        yield from back(a)
        yield from back(b)

    def drive(gens):
        gens = list(gens)
        while gens:
            for g_ in list(gens):
                try:
                    next(g_)
                except StopIteration:
                    gens.remove(g_)

    assert NT % 2 == 0
    drive([front(0), front(1)])
    for p_ in range(NT // 2):
        gl_ = []
        if 2 * p_ + 2 < NT:
            gl_ = [front(2 * p_ + 2), front(2 * p_ + 3)]
        drive(gl_ + [backs(2 * p_, 2 * p_ + 1)])


NEG = 32768.0
SLOPES = [2.0 ** (-(h + 1)) for h in range(8)]
GC = 1.5957691216057308


class _AT:
    pass


class Arena:
    def __init__(self, P, nbytes):
        self.cap = nbytes
        self.t = P.sbuf([128, nbytes // 2], BF16, "arena")
        self.off = 0
        self.deps = []
        self.inh_w = {}
        self.inh_r = {}

    def alloc(self, shape, dt, name=None):
        n = 1
        for s_ in shape[1:]:
            n *= s_
        nb = n * (4 if dt == F32 else 2)
        nb = (nb + 3) // 4 * 4
        assert self.off + nb <= self.cap, ("arena overflow", name, self.off, nb)
        ap = self.t[0:shape[0], self.off // 2:(self.off + nb) // 2]
        if dt == F32:
            ap = ap.bitcast(F32)
        ap = ap[:, 0:n]
        if len(shape) == 3:
            ap = ap.rearrange("p (a b) -> p a b", a=shape[1])
        elif len(shape) == 4:
            ap = ap.rearrange("p (a b c) -> p a b c", a=shape[1], b=shape[2])
        self.off += nb
        o = _AT()
        o.t = ap
        o.d = Dep()
        o.d.w = dict(self.inh_w)
        o.d.r = dict(self.inh_r)
        self.deps.append(o.d)
        return o

    def reset(self):
        for d in self.deps:
            for src in (d.w, d.r):
                for k, v in src.items():
                    if self.inh_w.get(k, 0) < v:
                        self.inh_w[k] = v
        self.inh_r = dict(self.inh_w)
        self.deps = []
        self.off = 0


def nsa_host_consts(S, m):
    NJ = S // 512
    NKT = S // 128
    NCT = (S // 16 - 1 + 127) // 128
    iq = np.arange(128)
    sl = np.array(SLOPES, np.float32)
    c = {}
    qaug = np.zeros((NJ, 128, 8, 5), np.float32)
    for j in range(NJ):
        qi = 4 * j + m
        qaug[j, :, :, 0] = -sl[None, :] * iq[:, None]
        qaug[j, :, :, 1] = sl[None, :]
        qaug[j, :, :, 2] = -sl[None, :] * 128 * qi
        qaug[j, :, :, 3] = sl[None, :] * 128
        qaug[j, :, :, 4] = 31 * sl[None, :]
    c['qaug'] = qaug.reshape(NJ * 128, 40)
    ka = np.zeros((128, NKT, 5), np.float32)
    ka[:, :, 0] = 1
    ka[:, :, 1] = iq[:, None]
    ka[:, :, 2] = 1
    ka[:, :, 3] = np.arange(NKT)[None, :]
    c['kaug_slc'] = ka.reshape(128, NKT * 5)
    kw = np.zeros((128, 5, 5), np.float32)
    kw[:, :, 0] = 1
    kw[:, :, 1] = iq[:, None]
    kw[:, :, 3] = (np.arange(5) - 4)[None, :]
    c['kaug_win'] = kw.reshape(128, 25)
    n = np.arange(NCT * 128)
    c['kaug_cmp'] = np.stack([np.ones_like(n), 16 * (n % 128), np.ones_like(n), 16 * (n // 128), np.ones_like(n)], 0).astype(np.float32)
    pw = np.zeros((NCT * 128, 256), np.float32)
    for nn in range(NCT * 128):
        jj = nn // 4
        if jj < 256:
            pw[nn, jj] += 1.0 if nn % 4 < 3 else 0.5
        if nn % 4 == 3 and jj + 1 < 256:
            pw[nn, jj + 1] += 0.5
    c['poolw'] = pw.reshape(NCT, 128, 256).transpose(1, 0, 2).reshape(128, NCT * 256)
    ex = np.zeros((128, 64, 128), np.float32)
    key = np.arange(128)
    for cc in range(64):
        ex[2 * cc + (key >= 64), cc, key] = 1.0
    c['ex'] = ex.reshape(128, 64 * 128)
    cm = np.zeros((NJ, 2, 128, 128), np.float32)
    for j in range(NJ):
        qi = 4 * j + m
        NT = j // 4
        for ab, nt in enumerate((NT, NT - 1)):
            nn = 128 * nt + iq[:, None]
            cm[j, ab] = (16 * nn + 31 <= 128 * qi + iq[None, :])
    c['cmask'] = ((cm - 1.0) * NEG).reshape(NJ * 2 * 128, 128)
    sm = np.zeros((4, 128, 128), np.float32)
    for s in range(4):
        sm[s] = 1.0 if s < m else ((iq[:, None] <= iq[None, :]) if s == m else 0.0)
    c['smask'] = ((sm - 1.0) * NEG).transpose(1, 0, 2).reshape(128, 4 * 128)
    wm = np.zeros((2, 5, 128, 128), np.float32)
    for st, qi in enumerate((m, m + 4)):
        for slot in range(5):
            kt = qi - 4 + slot
            d = 128 * (qi - kt) + iq[None, :] - iq[:, None]
            wm[st, slot] = ((d >= 0) & (d < 512)) if kt >= 0 else 0.0
    c['wmask'] = ((wm - 1.0) * NEG).transpose(2, 0, 1, 3).reshape(128, 10 * 128)
    sb = np.zeros((NJ, 128, 256), np.float32)
    jj = np.arange(256)[None, :]
    for j in range(NJ):
        qi = 4 * j + m
        blk = (128 * qi + iq[:, None]) // 64
        forced = (jj == 0) | (jj == blk) | (jj == blk - 1)
        sb[j] = np.where(forced, 1e4, 0.0)
        sb[j] = np.where(jj <= blk, sb[j], -1e30)
    c['selbias'] = sb.reshape(NJ * 128, 256)
    c['ident'] = np.eye(128, dtype=np.float32)
    return c


def nsa_host_data(proj_b, m, S):
    NJ = S // 512
    d = {}
    tiles = [4 * j + m for j in range(NJ)]
    d['qg'] = np.concatenate([proj_b[t * 128:(t + 1) * 128, 0:536] for t in tiles], 0)
    kv = proj_b[:, 536:1304]
    d['kv'] = np.concatenate([kv, np.zeros((128, 768), np.float32)], 0)
    kwv = np.concatenate([np.zeros((512, 256), np.float32), proj_b[:, 1048:1304]], 0)
    d['kwin'] = np.concatenate([kwv[(t - 4) * 128 + 512:(t + 1) * 128 + 512] for t in tiles], 0)
    return {k: np.ascontiguousarray(v, dtype=np.float32) for k, v in d.items()}


def nsa_stage(P, S, dr, w):
    NJ = S // 512
    NKT = S // 128
    NCT = (S // 16 - 1 + 127) // 128
    bank = [P.psum([128, 512], F32, "bank%d" % i) for i in range(7)]
    bd = [Dep(True) for _ in range(7)]
    psT = P.psum([128, 1024], BF16, "psT")
    psT_d = Dep(True)
    rot = [0]

    def rbank(lo=0, hi=7):
        i = lo + rot[0] % (hi - lo)
        rot[0] += 1
        return bank[i], bd[i]
    AR = Arena(P, 54 * 1024)

    def AT(P_, shape, dt, name):
        return AR.alloc(shape, dt, name)
    ident_f = Tl(P, [128, 128], F32, "ident_f")
    ident = Tl(P, [128, 128], BF16, "identb")
    P.dma('sp', ident_f.t[:], dr['ident'][:, :], W=[ident_f.d])
    P.op('dve', lambda e: e.tensor_copy(ident.t[:], ident_f.t[:]), R=[ident_f.d], W=[ident.d])
    stage = AT(P, [128, 2048], F32, "cstage")
    exb = Tl(P, [128, 64, 128], BF16, "exb")
    for pc in range(4):
        P.dma('sp', stage.t[:, :], dr['ex'][:, pc * 2048:(pc + 1) * 2048], W=[stage.d])
        P.op('dve', lambda e: e.tensor_copy(exb.t[:, pc * 16:(pc + 1) * 16, :], stage.t[:, :].rearrange("p (c k) -> p c k", c=16)),
             R=[stage.d], W=[exb.d])
    kaug_slc = Tl(P, [128, NKT, 5], F32, "kaug_slc")
    P.dma('sp', kaug_slc.t[:], dr['kaug_slc'].rearrange("p (k c) -> p k c", c=5), W=[kaug_slc.d])
    kaug_win = Tl(P, [128, 5, 5], F32, "kaug_win")
    P.dma('sp', kaug_win.t[:], dr['kaug_win'].rearrange("p (k c) -> p k c", c=5), W=[kaug_win.d])
    smaskb = Tl(P, [128, 4, 128], BF16, "smaskb")
    wmaskb = Tl(P, [128, 10, 128], BF16, "wmaskb")
    P.dma('sp', stage.t[:, 0:512], dr['smask'][:, :], W=[stage.d])
    P.op('dve', lambda e: e.tensor_copy(smaskb.t[:], stage.t[:, 0:512].rearrange("p (k c) -> p k c", c=128)), R=[stage.d], W=[smaskb.d])
    P.dma('sp', stage.t[:, 0:1280], dr['wmask'][:, :], W=[stage.d])
    P.op('dve', lambda e: e.tensor_copy(wmaskb.t[:], stage.t[:, 0:1280].rearrange("p (k c) -> p k c", c=128)), R=[stage.d], W=[wmaskb.d])
    KTs = [Tl(P, [69, S], BF16, "KTs%d" % g) for g in range(2)]
    Vs = Tl(P, [128, NKT, 2, 65], BF16, "Vs")
    KTc = [Tl(P, [69, NCT * 128], BF16, "KTc%d" % g) for g in range(2)]
    Vc = Tl(P, [128, NCT, 2, 321], BF16, "Vc")
    P.op('dve', lambda e: e.memset(Vs.t[:, :, :, 64:65], 1.0), W=[Vs.d])
    P.op('dve', lambda e: e.memset(Vc.t[:, :, :, 64:65], 1.0), W=[Vc.d])
    P.dma('sp', stage.t[:, 0:NCT * 256], dr['poolw'][:, :], R=[], W=[stage.d])
    for g in range(2):
        P.op('dve', lambda e: e.tensor_copy(Vc.t[:, :, g, 65:321], stage.t[:, 0:NCT * 256].rearrange("p (t c) -> p t c", c=256)),
             R=[stage.d], W=[Vc.d])
    P.dma('sp', stage.t[64:69, 0:NCT * 128], dr['kaug_cmp'][:, :], W=[stage.d])
    for g in range(2):
        P.op('dve', lambda e: e.tensor_copy(KTc[g].t[64:69, :], stage.t[64:69, 0:NCT * 128]), R=[stage.d], W=[KTc[g].d])
    kmx = Tl(P, [128, 4], F32, "kmx")
    P.op('dve', lambda e: e.memset(kmx.t[:, :], 0.0), W=[kmx.d])
    kvt = [AT(P, [128, 256], F32, "kvt%d" % i) for i in range(2)]
    Ktm = [AT(P, [128, 2, 69], BF16, "Ktm%d" % i) for i in range(2)]
    sq = AT(P, [128, 512], F32, "sq")
    ss = [AT(P, [128, 8], F32, "ss%d" % i) for i in range(2)]
    def kprepA(kt):
        kv_, km, s_ = kvt[kt % 2], Ktm[kt % 2], ss[kt % 2]
        P.dma('sp', kv_.t[:, :], dr['kv'][kt * 128:(kt + 1) * 128, 256:512], W=[kv_.d])
        P.act(km.t[:, :, 0:64], kv_.t[:, 0:128].rearrange("p (g d) -> p g d", g=2), AF.Copy, R=[kv_.d], W=[km.d])
        for g in range(2):
            P.op('dve', lambda e: e.tensor_copy(km.t[:, g, 64:69], kaug_slc.t[:, kt, :]), R=[kaug_slc.d], W=[km.d])
        P.op('dve', lambda e: e.tensor_tensor(sq.t[:, 0:128], kv_.t[:, 0:128], kv_.t[:, 0:128], ALU.mult), R=[kv_.d], W=[sq.d])
        P.op('dve', lambda e: e.tensor_reduce(s_.t[:, 0:2], sq.t[:, 0:128].rearrange("p (g d) -> p g d", g=2), AX.X, ALU.add),
             R=[sq.d], W=[s_.d])
        P.op('dve', lambda e: e.tensor_tensor(kmx.t[:, 0:2], kmx.t[:, 0:2], s_.t[:, 0:2], ALU.max), R=[s_.d], W=[kmx.d])
        P.op('dve', lambda e: e.tensor_copy(Vs.t[:, kt, :, 0:64], kv_.t[:, 128:256].rearrange("p (g d) -> p g d", g=2)),
             R=[kv_.d], W=[Vs.d])

    def kprepB(kt):
        km = Ktm[kt % 2]
        for g in range(2):
            P.tr(psT[0:69, g * 128:(g + 1) * 128], km.t[:, g, :], ident.t[:], R=[km.d, ident.d], W=[psT_d])
        for g in range(2):
            P.act(KTs[g].t[0:69, kt * 128:(kt + 1) * 128], psT[0:69, g * 128:(g + 1) * 128], AF.Copy, R=[psT_d], W=[KTs[g].d])

    kprepA(0)
    for kt in range(NKT):
        if kt + 1 < NKT:
            kprepA(kt + 1)
        kprepB(kt)
    W1 = [AT(P, [128, 32, 128], BF16, "W1%s" % n) for n in "kv"]
    W2 = [AT(P, [128, 64], BF16, "W2%s" % n) for n in "kv"]
    peT = [AT(P, [128, 32, 2], BF16, "peT%s" % n) for n in "kv"]
    b1 = [AT(P, [128, 2], F32, "b1%s" % n) for n in "kv"]
    for i, n in enumerate("kv"):
        for half in range(2):
            for pc in range(2):
                P.dma('sp', stage.t[half * 64:(half + 1) * 64, 0:2048].rearrange("p (a h) -> p a h", a=16),
                      w['w1_' + n].rearrange("(a d) h -> d a h", d=64)[:, pc * 16:(pc + 1) * 16, :], W=[stage.d])
                P.op('dve', lambda e: e.tensor_copy(W1[i].t[half * 64:(half + 1) * 64, pc * 16:(pc + 1) * 16, :],
                                                    stage.t[half * 64:(half + 1) * 64, 0:2048].rearrange("p (a h) -> p a h", a=16)),
                     R=[stage.d], W=[W1[i].d])
        P.dma('sp', stage.t[:, 0:64], w['w2_' + n][:, :], W=[stage.d])
        P.op('dve', lambda e: e.tensor_copy(W2[i].t[:, :], stage.t[:, 0:64]), R=[stage.d], W=[W2[i].d])
        P.dma('sp', stage.t[0:32, 0:64], w['pe_' + n][:, :], W=[stage.d])
        petm = AT(P, [32, 64], BF16, "petm%s" % n)
        P.op('dve', lambda e: e.tensor_copy(petm.t[:, :], stage.t[0:32, 0:64]), R=[stage.d], W=[petm.d])
        P.tr(psT[0:64, 0:32], petm.t[:, :], ident.t[0:32, 0:32], R=[petm.d, ident.d], W=[psT_d])
        for dup in range(2):
            P.op('dve', lambda e: e.tensor_copy(peT[i].t[0:64, :, dup], psT[0:64, 0:32]), R=[psT_d], W=[peT[i].d])
        bk, bkd = rbank()
        for p_ in range(32):
            P.mm(bk[:, 0:2], W1[i].t[0:64, p_, :], peT[i].t[0:64, p_, :], p_ == 0, p_ == 31, R=[W1[i].d, peT[i].d], W=[bkd])
        P.act(b1[i].t[:, 0:2], bk[:, 0:2], AF.Copy, R=[bkd], W=[b1[i].d])
    kct = AT(P, [128, 2, 2304], BF16, "kct")
    P.op('dve', lambda e: e.memset(kct.t[:, :, :], 0.0), W=[kct.d])
    ctm = [AT(P, [128, 256], F32, "ctm%d" % i) for i in range(2)]
    ctb = [AT(P, [128, 256], BF16, "ctb%d" % i) for i in range(2)]
    gl = {n: [AT(P, [128, 128], F32, "gl_%s%d" % (n, i)) for i in range(2)] for n in ('xb', 'x2', 'in1', 'sg')}
    a1 = [AT(P, [128, 128], BF16, "a1_%d" % i) for i in range(2)]
    cnt = 0
    for nt in range(NCT):
        ntile = min(17, (S + 128 - nt * 2048) // 128)
        def cA(tt):
            c_, cb_ = ctm[tt % 2], ctb[tt % 2]
            r0 = nt * 2048 + tt * 128
            P.dma('sp', c_.t[:, :], dr['kv'][r0:r0 + 128, 0:256], W=[c_.d])
            P.op('dve', lambda e: e.tensor_copy(cb_.t[:, :], c_.t[:, :]), R=[c_.d], W=[cb_.d])

        def cB(tt):
            cb_ = ctb[tt % 2]
            for kvi in range(2):
                P.tr(psT[:, kvi * 128:(kvi + 1) * 128], cb_.t[:, kvi * 128:(kvi + 1) * 128], ident.t[:], R=[cb_.d, ident.d], W=[psT_d])
            P.act(kct.t[:, :, tt * 128:(tt + 1) * 128], psT[:, 0:256].rearrange("p (k n) -> p k n", k=2), AF.Copy, R=[psT_d], W=[kct.d])
        cA(0)
        for tt in range(ntile):
            if tt + 1 < ntile:
                cA(tt + 1)
            cB(tt)
        for kvi in range(2):
            for g in range(2):
                bk, bkd = rbank()
                rows = slice(g * 64, (g + 1) * 64)
                for p_ in range(32):
                    P.mm(bk[:, 0:128], W1[kvi].t[rows, p_, :], kct.t[rows, kvi, p_:p_ + 2033:16], p_ == 0, p_ == 31,
                         R=[W1[kvi].d, kct.d], W=[bkd])
                i2 = cnt % 2
                cnt += 1
                xb, x2, in1, sg, A1 = gl['xb'][i2], gl['x2'][i2], gl['in1'][i2], gl['sg'][i2], a1[i2]
                P.act(xb.t[:, :], bk[:, 0:128], AF.Identity, R=[bkd, b1[kvi].d], W=[xb.d], bias=b1[kvi].t[:, 0:1])
                P.op('dve', lambda e: e.tensor_tensor(x2.t[:, :], xb.t[:, :], xb.t[:, :], ALU.mult), R=[xb.d], W=[x2.d])
                P.op('dve', lambda e: e.tensor_scalar(in1.t[:, :], x2.t[:, :], 0.044715, 1.0, ALU.mult, ALU.add), R=[x2.d], W=[in1.d])
                P.op('dve', lambda e: e.tensor_tensor(x2.t[:, :], in1.t[:, :], xb.t[:, :], ALU.mult), R=[in1.d, xb.d], W=[x2.d])
                P.act(sg.t[:, :], x2.t[:, :], AF.Sigmoid, R=[x2.d], W=[sg.d], scale=GC)
                P.op('dve', lambda e: e.tensor_tensor(A1.t[:, :], xb.t[:, :], sg.t[:, :], ALU.mult), R=[xb.d, sg.d], W=[A1.d])
                bo, bod = rbank()
                if kvi == 0:
                    P.mm(bo[0:64, 0:128], W2[0].t[:, :], A1.t[:, :], True, True, R=[W2[0].d, A1.d], W=[bod])
                    P.act(KTc[g].t[0:64, nt * 128:(nt + 1) * 128], bo[0:64, 0:128], AF.Copy, R=[bod], W=[KTc[g].d])
                else:
                    P.mm(bo[:, 0:64], A1.t[:, :], W2[1].t[:, :], True, True, R=[W2[1].d, A1.d], W=[bod])
                    P.act(Vc.t[:, nt, g, 0:64], bo[:, 0:64], AF.Copy, R=[bod], W=[Vc.d])
    onesb = AT(P, [128, 2], BF16, "onesb")
    P.op('dve', lambda e: e.memset(onesb.t[:, :], 1.0), W=[onesb.d])
    kc2 = AT(P, [64, NCT * 128], BF16, "kc2")
    for g in range(2):
        P.op('dve', lambda e: e.tensor_tensor(kc2.t[:, :], KTc[g].t[0:64, :], KTc[g].t[0:64, :], ALU.mult), R=[KTc[g].d], W=[kc2.d])
        for nt in range(NCT):
            bk, bkd = rbank()
            P.mm(bk[:, 0:2], kc2.t[:, nt * 128:(nt + 1) * 128], onesb.t[0:64, :], True, True, R=[kc2.d, onesb.d], W=[bkd])
            P.op('dve', lambda e: e.tensor_scalar(kmx.t[:, 2:3], bk[:, 0:1], 1.02, None, ALU.mult), R=[bkd], W=[kmx.d])
            P.op('dve', lambda e: e.tensor_tensor(kmx.t[:, 0:1], kmx.t[:, 0:1], kmx.t[:, 2:3], ALU.max), R=[kmx.d], W=[kmx.d])
    P.op('dve', lambda e: e.tensor_tensor(kmx.t[:, 0:1], kmx.t[:, 0:1], kmx.t[:, 1:2], ALU.max), R=[kmx.d], W=[kmx.d])
    kmb = AT(P, [128, 128], F32, "kmb")
    P.op('dve', lambda e: e.tensor_copy(kmb.t[:, :], kmx.t[:, 0:1].broadcast_to([128, 128])), R=[kmx.d], W=[kmb.d])
    bk, bkd = rbank()
    P.op('pe', lambda e: e.transpose(bk[:, 0:128], kmb.t[:, :], ident_f.t[:, :]), R=[kmb.d, ident_f.d], W=[bkd])
    Kmax2 = Tl(P, [128, 1], F32, "Kmax2")
    P.op('dve', lambda e: e.tensor_reduce(Kmax2.t[:, 0:1], bk[:, 0:128], AX.X, ALU.max), R=[bkd], W=[Kmax2.d])
    AR.reset()
    sq = AT(P, [128, 512], F32, "sq2")
    qg = [AT(P, [128, 536], F32, "qg%d" % i) for i in range(2)]
    qa = [AT(P, [128, 8, 5], F32, "qa%d" % i) for i in range(2)]
    gates = [AT(P, [128, 24], F32, "gates%d" % i) for i in range(2)]
    Qtm = [AT(P, [128, 8, 69], BF16, "Qtm%d" % i) for i in range(2)]
    Qaug = [AT(P, [69, 8, 128], BF16, "Qaug%d" % i) for i in range(2)]
    msq = [AT(P, [128, 16], F32, "msq%d" % i) for i in range(2)]
    kwt = [AT(P, [128, 5, 256], F32, "kwt0")] * 2
    Kwm = [AT(P, [128, 5, 2, 69], BF16, "Kwm%d" % i) for i in range(2)]
    KTw = [AT(P, [69, 10, 128], BF16, "KTw%d" % i) for i in range(2)]
    Vw = [AT(P, [128, 5, 2, 65], BF16, "Vw%d" % i) for i in range(2)]
    for i in range(2):
        P.op('dve', lambda e: e.memset(Vw[i].t[:, :, :, 64:65], 1.0), W=[Vw[i].d])
    cmk = [AT(P, [128, 2, 128], F32, "cmk%d" % i) for i in range(2)]
    cmkb = [AT(P, [128, 2, 128], BF16, "cmkb%d" % i) for i in range(2)]
    sbias = [AT(P, [128, 256], F32, "sbias%d" % i) for i in range(2)]
    E = [AT(P, [128, 512], BF16, "E%d" % i) for i in range(3)]
    ecnt = [0]
    imp = AT(P, [128, 256], F32, "imp")
    imp2 = AT(P, [128, 256], F32, "imp2")
    m8 = AT(P, [128, 16], F32, "m8")
    negsel = AT(P, [128, 256], BF16, "negsel")
    NST = [AT(P, [128, 2, 128], BF16, "NST%d" % i) for i in range(2)]
    wts = AT(P, [128, 3, 4], F32, "wts")
    den = AT(P, [128, 3, 4], F32, "den")
    onsa = [AT(P, [128, 512], F32, "onsa%d" % i) for i in range(2)]

    def attn_tile(KT_ap, KT_d, Qg, V_ap, V_d, Ob, Obd, ncolsV, first, last, sel=None, mask=None, per_r_banks=None):
        sb_, sbd = rbank(0, 3)
        sv = sb_[:, :].rearrange("p (r q) -> p r q", r=4)
        extra = []
        if sel is not None:
            ex_ap, nst_ap, nst_d = sel
            extra.append((ex_ap, nst_ap, [exb.d, nst_d]))
        if mask is not None:
            mk_ap, mk_d = mask
            extra.append((ident.t[:, :], mk_ap.unsqueeze(1).broadcast_to([128, 4, 128]), [ident.d, mk_d]))
        P.mm(sv, KT_ap, Qg, True, len(extra) == 0, R=[KT_d, Qaug[jq].d], W=[sbd])
        for i_, (l_, r_, d_) in enumerate(extra):
            P.mm(sv, l_, r_, False, i_ == len(extra) - 1, R=d_, W=[sbd])
        e_ = E[ecnt[0] % 3]
        ecnt[0] += 1
        P.act(e_.t[:, :], sb_[:, :], AF.Exp, R=[sbd], W=[e_.d])
        def pv():
            for r in range(4):
                if per_r_banks is not None:
                    ob, obd = per_r_banks[r]
                    P.mm(ob[:, 0:ncolsV], e_.t[:, r * 128:(r + 1) * 128], V_ap, first, last, R=[e_.d, V_d], W=[obd])
                else:
                    P.mm(Ob[:, r * ncolsV:(r + 1) * ncolsV], e_.t[:, r * 128:(r + 1) * 128], V_ap, first and r == 0, last, R=[e_.d, V_d], W=[Obd],
                         skip_group_check=True)
        pending.append(pv)
        while len(pending) > 2:
            pending.pop(0)()

    pending = []

    def flush():
        while pending:
            pending.pop(0)()

    for j in range(NJ):
        jq = j % 2
        Q_, QA, GT, QT_, QG_, MS = qg[jq], qa[jq], gates[jq], Qtm[jq], Qaug[jq], msq[jq]
        P.dma('sp', Q_.t[:, :], dr['qg'][j * 128:(j + 1) * 128, :], W=[Q_.d])
        P.dma('sp', QA.t[:, :, :], dr['qaug'][j * 128:(j + 1) * 128, :].rearrange("p (h c) -> p h c", c=5), W=[QA.d])
        P.act(GT.t[:, :], Q_.t[:, 512:536], AF.Sigmoid, R=[Q_.d], W=[GT.d])
        P.act(QT_.t[:, :, 0:64], Q_.t[:, 0:512].rearrange("p (h d) -> p h d", h=8), AF.Copy, R=[Q_.d], W=[QT_.d], scale=0.125)
        P.op('dve', lambda e: e.tensor_tensor(sq.t[:, :], Q_.t[:, 0:512], Q_.t[:, 0:512], ALU.mult), R=[Q_.d], W=[sq.d])
        P.op('dve', lambda e: e.tensor_reduce(MS.t[:, 0:8], sq.t[:, :].rearrange("p (h d) -> p h d", h=8), AX.X, ALU.add), R=[sq.d], W=[MS.d])
        P.act(MS.t[:, 8:16], MS.t[:, 0:8], AF.Sqrt, R=[MS.d, Kmax2.d], W=[MS.d], scale=Kmax2.t[:, 0:1])
        P.op('dve', lambda e: e.scalar_tensor_tensor(QT_.t[:, :, 64], MS.t[:, 8:16], -0.125, QA.t[:, :, 0], ALU.mult, ALU.add),
             R=[MS.d, QA.d], W=[QT_.d])
        P.op('dve', lambda e: e.tensor_copy(QT_.t[:, :, 65:69], QA.t[:, :, 1:5]), R=[QA.d], W=[QT_.d])
        for h in range(8):
            P.tr(psT[0:69, h * 128:(h + 1) * 128], QT_.t[:, h, :], ident.t[:], R=[QT_.d, ident.d], W=[psT_d])
        P.act(QG_.t[0:69, :, :], psT[0:69, :].rearrange("p (h q) -> p h q", h=8), AF.Copy, R=[psT_d], W=[QG_.d])
        KW, KM, KTW, VW = kwt[jq], Kwm[jq], KTw[jq], Vw[jq]
        P.dma('sp', KW.t[:, :, :], dr['kwin'][j * 640:(j + 1) * 640, :].rearrange("(s p) c -> p s c", p=128), W=[KW.d])
        P.act(KM.t[:, :, :, 0:64], KW.t[:, :, 0:128].rearrange("p s (g d) -> p s g d", g=2), AF.Copy, R=[KW.d], W=[KM.d])
        for g in range(2):
            P.op('dve', lambda e: e.tensor_copy(KM.t[:, :, g, 64:69], kaug_win.t[:, :, :]), R=[kaug_win.d], W=[KM.d])
        P.op('dve', lambda e: e.tensor_copy(VW.t[:, :, :, 0:64], KW.t[:, :, 128:256].rearrange("p s (g d) -> p s g d", g=2)),
             R=[KW.d], W=[VW.d])
        for half in range(2):
            idxs = list(range(half * 5, half * 5 + 5))
            for ii, sg_ in enumerate(idxs):
                s_, g_ = sg_ // 2, sg_ % 2
                P.tr(psT[0:69, ii * 128:(ii + 1) * 128], KM.t[:, s_, g_, :], ident.t[:], R=[KM.d, ident.d], W=[psT_d])
            P.act(KTW.t[0:69, half * 5:half * 5 + 5, :], psT[0:69, 0:640].rearrange("p (a q) -> p a q", a=5), AF.Copy, R=[psT_d], W=[KTW.d])
        CM, SB_ = cmk[jq], sbias[jq]
        P.dma('sp', CM.t[:, :, :], dr['cmask'][j * 256:(j + 1) * 256, :].rearrange("(a p) c -> p a c", p=128), W=[CM.d])
        P.dma('sp', SB_.t[:, :], dr['selbias'][j * 128:(j + 1) * 128, :], W=[SB_.d])
        CMB = cmkb[jq]
        P.op('dve', lambda e: e.tensor_copy(CMB.t[:], CM.t[:]), R=[CM.d], W=[CMB.d])
        ON = onsa[jq]
        for g in range(2):
            Qg = QG_.t[0:69, 4 * g:4 * g + 4, :]
            NT = j // 4
            crb = [(bank[3 + r], bd[3 + r]) for r in range(4)]
            for nt in range(NT + 1):
                mk = None
                if nt == NT:
                    mk = (CMB.t[:, 0, :], CMB.d)
                elif nt == NT - 1:
                    mk = (CMB.t[:, 1, :], CMB.d)
                attn_tile(KTc[g].t[0:69, nt * 128:(nt + 1) * 128], KTc[g].d, Qg, Vc.t[:, nt, g, :], Vc.d, None, None, 321,
                          nt == 0, nt == NT, mask=mk, per_r_banks=crb)
            flush()
            for r in range(4):
                ob, obd = crb[r]
                P.op('dve', lambda e: e.tensor_scalar(den.t[:, 0, r:r + 1], ob[:, 64:65], 1e-30, None, ALU.max), R=[obd], W=[den.d])
            P.op('dve', lambda e: e.reciprocal(den.t[:, 0, :], den.t[:, 0, :]), R=[den.d], W=[den.d])
            P.op('dve', lambda e: e.tensor_tensor(wts.t[:, 0, :], den.t[:, 0, :], GT.t[:, :].rearrange("p (h x) -> p h x", x=3)[:, 4 * g:4 * g + 4, 0],
                                                  ALU.mult), R=[den.d, GT.d], W=[wts.d])
            for r in range(4):
                ob, obd = crb[r]
                col = (4 * g + r) * 64
                P.op('dve', lambda e: e.tensor_scalar(ON.t[:, col:col + 64], ob[:, 0:64], wts.t[:, 0, r:r + 1], None, ALU.mult),
                     R=[obd, wts.d], W=[ON.d])
                if r == 0:
                    P.op('dve', lambda e: e.tensor_scalar(imp.t[:, :], ob[:, 65:321], den.t[:, 0, r:r + 1], None, ALU.mult),
                         R=[obd, den.d], W=[imp.d])
                else:
                    P.op('dve', lambda e: e.scalar_tensor_tensor(imp.t[:, :], ob[:, 65:321], den.t[:, 0, r:r + 1], imp.t[:, :], ALU.mult, ALU.add),
                         R=[obd, den.d, imp.d], W=[imp.d])
            wb, wbd = bank[4], bd[4]
            wset = 0 if j == 0 else 1
            for slot in range(5):
                mk = None
                if j == 0 or slot in (0, 4):
                    mk = (wmaskb.t[:, wset * 5 + slot, :], wmaskb.d)
                attn_tile(KTW.t[0:69, slot * 2 + g, :], KTW.d, Qg, VW.t[:, slot, g, :], VW.d, wb, wbd, 65, slot == 0, slot == 4, mask=mk)
            P.op('dve', lambda e: e.tensor_tensor(imp2.t[:, :], imp.t[:, :], SB_.t[:, :], ALU.add), R=[imp.d, SB_.d], W=[imp2.d])
            P.op('dve', lambda e: e.max(m8.t[:, 0:8], imp2.t[:, :]), R=[imp2.d], W=[m8.d])
            P.op('dve', lambda e: e.match_replace(imp.t[:, :], m8.t[:, 0:8], imp2.t[:, :], -3.0e38), R=[imp2.d, m8.d], W=[imp.d])
            P.op('dve', lambda e: e.max(m8.t[:, 8:16], imp.t[:, :]), R=[imp.d], W=[m8.d])
            P.op('dve', lambda e: e.tensor_scalar(imp.t[:, :], imp2.t[:, :], m8.t[:, 15:16], None, ALU.is_ge), R=[imp2.d, m8.d], W=[imp.d])
            P.op('dve', lambda e: e.tensor_scalar(negsel.t[:, :], imp.t[:, :], -1.0, NEG, ALU.add, ALU.mult), R=[imp.d], W=[negsel.d])
            NS = NST[g]
            for jt in range(2):
                P.tr(psT[:, jt * 128:(jt + 1) * 128], negsel.t[:, jt * 128:(jt + 1) * 128], ident.t[:], R=[negsel.d, ident.d], W=[psT_d])
            P.op('dve', lambda e: e.tensor_copy(NS.t[:, :, :], psT[:, 0:256].rearrange("p (a q) -> p a q", a=2)), R=[psT_d], W=[NS.d])
            sbk, sbkd = bank[3], bd[3]
            nk = 4 * j + 4
            for kt in range(nk):
                mk = None
                if kt >= 4 * j:
                    mk = (smaskb.t[:, kt - 4 * j, :], smaskb.d)
                attn_tile(KTs[g].t[0:69, kt * 128:(kt + 1) * 128], KTs[g].d, Qg, Vs.t[:, kt, g, :], Vs.d, sbk, sbkd, 65,
                          kt == 0, kt == nk - 1, sel=(exb.t[:, kt % 64, :], NS.t[:, kt // 64, :].unsqueeze(1).broadcast_to([128, 4, 128]), NS.d), mask=mk)
            flush()
            for xi, (ob, obd) in ((1, (sbk, sbkd)), (2, (wb, wbd))):
                P.op('dve', lambda e: e.tensor_scalar(den.t[:, xi, :], ob[:, 0:260].rearrange("p (r c) -> p r c", r=4)[:, :, 64], 1e-30, None, ALU.max),
                     R=[obd], W=[den.d])
                P.op('dve', lambda e: e.reciprocal(den.t[:, xi, :], den.t[:, xi, :]), R=[den.d], W=[den.d])
                P.op('dve', lambda e: e.tensor_tensor(wts.t[:, xi, :], den.t[:, xi, :],
                                                      GT.t[:, :].rearrange("p (h x) -> p h x", x=3)[:, 4 * g:4 * g + 4, xi], ALU.mult),
                     R=[den.d, GT.d], W=[wts.d])
                for r in range(4):
                    col = (4 * g + r) * 64
                    P.op('dve', lambda e: e.scalar_tensor_tensor(ON.t[:, col:col + 64], ob[:, r * 65:r * 65 + 64], wts.t[:, xi, r:r + 1],
                                                                 ON.t[:, col:col + 64], ALU.mult, ALU.add), R=[obd, wts.d, ON.d], W=[ON.d])
        P.dma('act', dr['onsa'][j * 128:(j + 1) * 128, :], ON.t[:, :], R=[ON.d])


_PROGS = {}
T_CORE = 4096
SEQ = 16384


def _gcol(v):
    return np.ascontiguousarray(np.asarray(v, np.float32).reshape(8, 128).T)


def _f32(a):
    return np.ascontiguousarray(np.asarray(a, dtype=np.float32))


def _run(nc, in_maps):
    res = run_bass_kernel_spmd(nc, in_maps, core_ids=list(range(8)))
    return res.results


FFN_W = (("wg", [D, DFF]), ("wu", [D, DFF]), ("wd", [DFF, D]), ("pre", [128, 8]), ("post", [D]))


def _ffn_drams(P, tag):
    return {k: P.dram("%s_%s" % (k, tag), shp, F32, "ExternalInput") for k, shp in FFN_W}


def _ffn_inputs(inp, prefix, tag):
    return {"wg_" + tag: _f32(inp[prefix + "_w_gate"]), "wu_" + tag: _f32(inp[prefix + "_w_up"]),
            "wd_" + tag: _f32(inp[prefix + "_w_down"]), "pre_" + tag: _gcol(inp[prefix + "_pre_g"]),
            "post_" + tag: _f32(inp[prefix + "_post_g"])}


def build_L1(T):
    P = Prog()
    x = P.dram("x", [T, D], F32, "ExternalInput")
    x1 = P.dram("x1", [T, D], F32, "ExternalOutput")
    proj = P.dram("proj", [T, 2840], F32, "ExternalOutput")
    ident = P.dram("ident", [128, 128], F32, "ExternalInput")
    w_in = P.dram("w_in", [D, 2840], F32, "ExternalInput")
    gin = P.dram("gcol_in", [128, 8], F32, "ExternalInput")
    fw = _ffn_drams(P, "a")
    C = Consts(P, {'ident': ident})
    B = FFNBufs(P)
    ffn_load_weights(P, B, fw['wg'], fw['wu'], fw['wd'], fw['pre'], fw['post'])
    X = DT(x, T)
    Y = DT(x1, T)
    ffn_stage(P, B, C, X, Y, T)
    inproj_stage(P, B, C, Y, T, w_in, gin, proj)
    return P.build()


def build_PF(T, conv, n_ffn):
    P = Prog()
    ident = P.dram("ident", [128, 128], F32, "ExternalInput")
    a = P.dram("a", [T, 512 if conv else D], F32, "ExternalInput")
    xres = P.dram("xres", [T, D], F32, "ExternalInput")
    Wm = P.dram("w_mix", [D, D], F32, "ExternalInput")
    pg = P.dram("mix_post", [D], F32, "ExternalInput")
    cv = None
    if conv:
        cv = dict(cbcu=P.dram("cbcu", [T + 2, 1536], F32, "ExternalInput"), w=P.dram("conv_w", [3, 512], F32, "ExternalInput"),
                  b=P.dram("conv_b", [512], F32, "ExternalInput"))
    fws = [_ffn_drams(P, "f%d" % i) for i in range(n_ffn)]
    out = P.dram("out", [T, D], F32, "ExternalOutput")
    C = Consts(P, {'ident': ident})
    B = FFNBufs(P)
    cur = DT(P.dram("scr0", [T, D], F32, "Internal"), T)
    projres_stage(P, B, C, T, a, Wm, pg, DT(xres, T), cur, conv=cv)
    for i in range(n_ffn):
        nxt = DT(out if i == n_ffn - 1 else P.dram("scr%d" % (i + 1), [T, D], F32, "Internal"), T)
        fw = fws[i]
        ffn_load_weights(P, B, fw['wg'], fw['wu'], fw['wd'], fw['pre'], fw['post'])
        ffn_stage(P, B, C, cur, nxt, T)
        cur = nxt
    return P.build()


NSA_WSH = dict(pe_k=[32, 64], w1_k=[2048, 128], w2_k=[128, 64], pe_v=[32, 64], w1_v=[2048, 128], w2_v=[128, 64])


def build_NSA(S):
    P = Prog()
    NJ = S // 512
    c0 = nsa_host_consts(S, 0)
    dr = {k: P.dram(k, list(v.shape), F32, "ExternalInput") for k, v in c0.items()}
    for k, v in dict(qg=[NJ * 128, 536], kv=[S + 128, 768], kwin=[NJ * 640, 256]).items():
        dr[k] = P.dram(k, v, F32, "ExternalInput")
    dr['onsa'] = P.dram("onsa", [NJ * 128, 512], F32, "ExternalOutput")
    w = {k: P.dram(k, v, F32, "ExternalInput") for k, v in NSA_WSH.items()}
    nsa_stage(P, S, dr, w)
    return P.build()


RW_SH = dict(gmu=[128, 7, 8], w_r=[D, 256], w_k=[D, 256], w_v=[D, 256], w_dec1=[D, 64], w_a1=[D, 64], w_g1=[D, 128],
             w_dec2=[64, 256], w_a2=[64, 256], w_g2=[128, 256], w0=[256], a0=[256], k_k=[256], k_a=[256], r_k=[256],
             lnx_g=[256], lnx_b=[256])


def build_RWKV(S):
    P = Prog()
    xpad = P.dram("xpad", [S + 1, D], F32, "ExternalInput")
    zout = P.dram("z", [S, 256], F32, "ExternalOutput")
    cmat = P.dram("cmat", [7, 128, 128], F32, "ExternalInput")
    w = {k: P.dram(k, v, F32, "ExternalInput") for k, v in RW_SH.items()}
    rwkv_stage(P, S, xpad, zout, w, cmat)
    return P.build()


def _prog(name, fn):
    if name not in _PROGS:
        _PROGS[name] = fn()
    return _PROGS[name]


def kernel(**inp):
    x = _f32(inp["x"])
    NB, S, _ = x.shape
    T = NB * S // 8
    CPB = 8 // NB
    eye = np.eye(128, dtype=np.float32)
    xs = x.reshape(8, T, D)
    nc = _prog("L1", lambda: build_L1(T))
    shared = dict(ident=eye, w_in=_f32(inp["l0_w_in"]), gcol_in=_gcol(inp["l0_mix_pre_g"]))
    shared.update(_ffn_inputs(inp, "l0_ffn1", "a"))
    r = _run(nc, [dict(shared, x=xs[c]) for c in range(8)])
    x1 = np.stack([r[c]["x1"] for c in range(8)], 0)
    proj = np.stack([r[c]["proj"] for c in range(8)], 0).reshape(NB, S, 2840)
    nc = _prog("NSA", lambda: build_NSA(S))
    cw = dict(pe_k=_f32(inp["l0_cmp_pe_k"]), w1_k=_f32(inp["l0_cmp_w1_k"]), w2_k=_f32(inp["l0_cmp_w2_k"]),
              pe_v=_f32(inp["l0_cmp_pe_v"]), w1_v=_f32(inp["l0_cmp_w1_v"]), w2_v=_f32(inp["l0_cmp_w2_v"]))
    consts = [nsa_host_consts(S, m) for m in range(CPB)]
    maps = []
    for c in range(8):
        b, m = c // CPB, c % CPB
        d = nsa_host_data(proj[b], m, S)
        d.update(consts[m])
        d.update(cw)
        maps.append({k: _f32(v) for k, v in d.items()})
    r = _run(nc, maps)
    onsa = np.zeros((NB, S, 512), np.float32)
    NJ = S // 512
    for c in range(8):
        b, m = c // CPB, c % CPB
        o = r[c]["onsa"]
        for j in range(NJ):
            qi = 4 * j + m
            onsa[b, qi * 128:(qi + 1) * 128] = o[j * 128:(j + 1) * 128]
    onsa = onsa.reshape(8, T, 512)
    nc = _prog("PF2", lambda: build_PF(T, True, 2))
    cbcu_full = np.concatenate([np.zeros((NB, 2, 1536), np.float32), proj[:, :, 1304:2840]], 1)
    shared = dict(ident=eye, w_mix=_f32(inp["l0_w_out"]), mix_post=_f32(inp["l0_mix_post_g"]),
                  conv_w=_f32(inp["l0_conv_w"]), conv_b=_f32(inp["l0_conv_b"]))
    shared.update(_ffn_inputs(inp, "l0_ffn2", "f0"))
    shared.update(_ffn_inputs(inp, "l1_ffn1", "f1"))
    maps = []
    for c in range(8):
        b, m = c // CPB, c % CPB
        t0 = m * T
        maps.append(dict(shared, a=onsa[c], xres=x1[c], cbcu=_f32(cbcu_full[b, t0:t0 + T + 2])))
    r = _run(nc, maps)
    x4 = np.stack([r[c]["out"] for c in range(8)], 0)
    nc = _prog("RWKV", lambda: build_RWKV(S))
    x4b = x4.reshape(NB, S, D)
    cm = rwkv_consts_np()
    gmu = np.concatenate([np.asarray(inp["l1_mix_pre_g"], np.float32)[None], np.asarray(inp["l1_mu"], np.float32)], 0)
    gmu = np.ascontiguousarray(gmu.reshape(7, 8, 128).transpose(2, 0, 1))
    maps = []
    for c in range(8):
        b, hg = c // CPB, c % CPB
        cs = slice(hg * 256, (hg + 1) * 256)
        d = dict(xpad=np.concatenate([np.zeros((1, D), np.float32), x4b[b]], 0), cmat=cm, gmu=gmu,
                 w_r=inp["l1_w_r"][:, cs], w_k=inp["l1_w_k"][:, cs], w_v=inp["l1_w_v"][:, cs],
                 w_dec1=inp["l1_w_dec1"], w_a1=inp["l1_w_a1"], w_g1=inp["l1_w_g1"],
                 w_dec2=inp["l1_w_dec2"][:, cs], w_a2=inp["l1_w_a2"][:, cs], w_g2=inp["l1_w_g2"][:, cs],
                 w0=inp["l1_w0"][cs], a0=inp["l1_a0"][cs], k_k=inp["l1_k_k"][cs], k_a=inp["l1_k_a"][cs],
                 r_k=np.asarray(inp["l1_r_k"]).reshape(-1)[cs], lnx_g=inp["l1_lnx_g"][cs], lnx_b=inp["l1_lnx_b"][cs])
        maps.append({k: _f32(v) for k, v in d.items()})
    r = _run(nc, maps)
    z = np.zeros((NB, S, D), np.float32)
    for c in range(8):
        b, hg = c // CPB, c % CPB
        z[b, :, hg * 256:(hg + 1) * 256] = r[c]["z"]
    z = z.reshape(8, T, D)
    nc = _prog("PF1", lambda: build_PF(T, False, 1))
    shared = dict(ident=eye, w_mix=_f32(inp["l1_w_o"]), mix_post=_f32(inp["l1_mix_post_g"]))
    shared.update(_ffn_inputs(inp, "l1_ffn2", "f0"))
    r = _run(nc, [dict(shared, a=z[c], xres=x4[c]) for c in range(8)])
    out = np.stack([r[c]["out"] for c in range(8)], 0).reshape(NB, S, D)
    return out.astype(np.float32)
```

```python
import math
import numpy as np
from contextlib import ExitStack
import concourse.bass as bass
import concourse.mybir as mybir
from concourse.bass_utils import run_bass_kernel_spmd


F32 = mybir.dt.float32
BF16 = mybir.dt.bfloat16
ALU = mybir.AluOpType
AF = mybir.ActivationFunctionType
AX = mybir.AxisListType
EP = 30000
NSLOT = 14
ENGS = ('pe', 'act', 'dve', 'pool', 'sp')
ENGATTR = {'pe': 'tensor', 'act': 'scalar', 'dve': 'vector', 'pool': 'gpsimd', 'sp': 'sync'}


class Dep:
    __slots__ = ('w', 'r', 'excl')

    def __init__(self, excl=False):
        self.w = {}
        self.r = {}
        self.excl = excl


class _Rec:
    def __init__(self):
        self.calls = []

    def __getattr__(self, name):
        def m(*a, **k):
            self.calls.append((name, a, k))
        return m


class Prog:
    def __init__(self):
        self.nc = bass.Bass("TRN2", target_bir_lowering=False)
        self.stack = ExitStack()
        self.ops = {e: [] for e in ENGS}
        self.n = {e: 0 for e in ENGS}
        self.seen = {e: {} for e in ENGS}
        self.slot_cnt = [0] * NSLOT
        self.slot_next = 0
        self.keys = set()
        self._uid = 0

    def uid(self, p):
        self._uid += 1
        return "%s_%d" % (p, self._uid)

    def sbuf(self, shape, dt, name=None):
        return self.stack.enter_context(self.nc.sbuf_tensor("sb_" + (name or self.uid("t")), list(shape), dt))

    def psum(self, shape, dt, name=None):
        return self.stack.enter_context(self.nc.psum_tensor("ps_" + (name or self.uid("t")), list(shape), dt))

    def dram(self, name, shape, dt, kind="Internal"):
        return self.nc.dram_tensor(name, list(shape), dt, kind=kind).ap()

    def _waits(self, eng, R, W):
        need = {}

        def add(d):
            for k, v in d.items():
                if eng == 'pe' and k[0] == 'pe':
                    continue
                if self.seen[eng].get(k, 0) < v:
                    if need.get(k, 0) < v:
                        need[k] = v

        for d in R:
            add(d.w)
        for d in W:
            add(d.w)
            add(d.r)
        for k, v in need.items():
            self.seen[eng][k] = v
        return list(need.items())

    def op(self, eng, fn, R=(), W=()):
        rec = _Rec()
        fn(rec)
        assert len(rec.calls) == 1
        name_, a_, k_ = rec.calls[0]
        fn = (lambda e, name_=name_, a_=a_, k_=k_: getattr(e, name_)(*a_, **k_))
        W = list(W) + [d for d in R if d.excl]
        R = [d for d in R if not d.excl]
        waits = self._waits(eng, R, W)
        self.n[eng] += 1
        n = self.n[eng]
        key = (eng, (n - 1) // EP)
        val = (n - 1) % EP + 1
        self.keys.add(key)
        self.ops[eng].append((waits, fn, key, 1))
        for d in R:
            if d.r.get(key, 0) < val:
                d.r[key] = val
        for d in W:
            d.w = {key: val}
            d.r = {}

    def dma(self, q, out, in_, R=(), W=(), **kw):
        waits = self._waits(q, R, W)
        s = self.slot_next
        self.slot_next = (s + 1) % NSLOT
        key = ('dma', s)
        self.keys.add(key)
        prev = 16 * self.slot_cnt[s]
        if prev > 0 and self.seen[q].get(key, 0) < prev:
            waits.append((key, prev))
            self.seen[q][key] = prev
        self.slot_cnt[s] += 1
        val = 16 * self.slot_cnt[s]
        assert val < 60000
        self.ops[q].append((waits, (lambda e: e.dma_start(out=out, in_=in_, **kw)), key, 16))
        for d in R:
            d.r[key] = val
        for d in W:
            d.w = {key: val}
            d.r = {}

    def mm(self, out, lhsT, rhs, start, stop, R=(), W=(), **kw):
        self.op('pe', lambda e: e.matmul(out, lhsT, rhs, start=start, stop=stop, **kw), R, W)

    def tr(self, out, in_, ident, R=(), W=()):
        self.op('pe', lambda e: e.transpose(out, in_, ident), R, W)

    def act(self, out, in_, func, R=(), W=(), eng='act', **kw):
        self.op('act', lambda e: e.activation(out=out, in_=in_, func=func, **kw), R, W)

    def build(self):
        nc = self.nc
        sems = {}
        for k in sorted(self.keys, key=str):
            sems[k] = self.stack.enter_context(nc.semaphore("s_%s_%d" % (k[0], k[1])))
        with nc.Block() as block:
            for e in ENGS:
                oplist = self.ops[e]
                final = []
                if e == 'sp':
                    final = [(('dma', s), 16 * c) for s, c in enumerate(self.slot_cnt) if c > 0]

                def body(eng, oplist=oplist, final=final):
                    for waits, fn, key, amt in oplist:
                        ws = list(waits)
                        att = None
                        if key[0] != 'dma' and ws:
                            att = ws.pop()
                        for k, v in ws:
                            eng.wait_ge(sems[k], v)
                        ins = fn(eng)
                        if att is not None:
                            ins._wait_ge(sems[att[0]], att[1])
                        ins.then_inc(sems[key], amt)
                    for k, v in final:
                        eng.wait_ge(sems[k], v)

                if oplist or final:
                    getattr(block, ENGATTR[e])(body)
        self.stack.close()
        return nc

    def stats(self):
        return {e: len(self.ops[e]) for e in ENGS}


D = 1024
DFF = 2816
NFC = DFF // 128
EPS = 1e-6


class DT:
    def __init__(self, ap, rows):
        self.ap = ap
        self.deps = [Dep() for _ in range((rows + 127) // 128)]


class Consts:
    def __init__(self, P, cdram):
        self.ident_d = Dep()
        self.ident = P.sbuf([128, 128], BF16, "ident_sb")
        self.ident_f = P.sbuf([128, 128], F32, "ident_f")
        P.dma('sp', self.ident_f[:], cdram['ident'][:, :], W=[self.ident_d])
        P.op('dve', lambda e: e.tensor_copy(self.ident[:], self.ident_f[:]), R=[self.ident_d], W=[self.ident_d])
        self.eps1 = P.sbuf([128, 4], F32, "eps1")
        self.eps_d = Dep()
        P.op('dve', lambda e: e.memset(self.eps1[:, 0:1], EPS), W=[self.eps_d])
        P.op('dve', lambda e: e.memset(self.eps1[:, 1:2], 4 * EPS), W=[self.eps_d])


class FFNBufs:
    def __init__(self, P):
        self.wg = P.sbuf([128, 8, 2840], BF16, "wg")
        self.wu = P.sbuf([128, 8, DFF], BF16, "wu")
        self.wd = P.sbuf([128, NFC, D], BF16, "wd")
        self.wg_d = [Dep() for _ in range(8)]
        self.wu_d = [Dep() for _ in range(8)]
        self.wd_d = [Dep() for _ in range(NFC // 2)]
        self.stage = [P.sbuf([128, 2048], F32, "wstage%d" % i) for i in range(2)]
        self.stage_d = [Dep() for _ in range(2)]
        self.stage_i = 0
        self.gcol = P.sbuf([128, 8], F32, "gcol")
        self.gcol_d = Dep()
        self.gpost = P.sbuf([128, D], F32, "gpost")
        self.gpost_d = Dep()
        NX = 4
        self.NX = NX
        self.xt = [P.sbuf([128, D], F32, "xt%d" % i) for i in range(NX)]
        self.xt_d = [Dep() for _ in range(NX)]
        self.xn = [P.sbuf([128, D], BF16, "xn%d" % i) for i in range(2)]
        self.xn_d = [Dep() for _ in range(2)]
        self.junk = P.sbuf([128, D], BF16, "junk")
        self.junk_d = Dep()
        self.st = [P.sbuf([128, 8], F32, "st%d" % i) for i in range(NX)]
        self.st_d = [Dep() for _ in range(NX)]
        self.hT = [P.sbuf([128, 8, 256], BF16, "hT%d" % i) for i in range(2)]
        self.hT_d = [Dep() for _ in range(2)]
        self.sg = [P.sbuf([128, 256], F32, "sg%d" % i) for i in range(2)]
        self.sg_d = [Dep() for _ in range(2)]
        self.a = [P.sbuf([128, 256], BF16, "a%d" % i) for i in range(3)]
        self.a_d = [Dep() for _ in range(3)]
        self.t = [P.sbuf([128, D], F32, "t0")] * 2
        self.t_d = [Dep()] * 2
        self.yt = [P.sbuf([128, D], F32, "yt%d" % i) for i in range(2)]
        self.yt_d = [Dep() for _ in range(2)]
        self.psT = P.psum([128, 1024], BF16, "psT")
        self.psT_d = Dep(True)
        self.gu = [P.psum([128, 512], F32, "gu%d" % i) for i in range(2)]
        self.gu_d = [Dep(True) for _ in range(2)]
        self.o = [P.psum([128, 1024], F32, "ops%d" % i) for i in range(2)]
        self.o_d = [Dep(True) for _ in range(2)]
        self.cnt = 0
        self.xrot = 0


def ffn_load_weights(P, B, wg, wu, wd, pre_g, post_g):
    P.dma('sp', B.gcol[:], pre_g[:, :], R=[], W=[B.gcol_d])
    P.dma('sp', B.gpost[:], post_g.partition_broadcast(128), W=[B.gpost_d])
    for wsrc, wdst, wdeps in ((wg, B.wg, B.wg_d), (wu, B.wu, B.wu_d)):
        for dc in range(8):
            for hf in range(2):
                si = B.stage_i
                B.stage_i ^= 1
                st, sd = B.stage[si], B.stage_d[si]
                P.dma('sp', st[:, 0:1408], wsrc[dc * 128:(dc + 1) * 128, hf * 1408:(hf + 1) * 1408], W=[sd])
                P.act(wdst[:, dc, hf * 1408:(hf + 1) * 1408], st[:, 0:1408], AF.Copy,
                      R=[sd, B.gcol_d], W=[wdeps[dc]], scale=B.gcol[:, dc:dc + 1])
    for j in range(NFC // 2):
        si = B.stage_i
        B.stage_i ^= 1
        st, sd = B.stage[si], B.stage_d[si]
        P.dma('sp', st[:, 0:2048].rearrange("p (c n) -> p c n", c=2),
              wd[j * 256:(j + 1) * 256, :].rearrange("(c p) n -> p c n", p=128), W=[sd])
        P.op('pool', lambda e, st=st, j=j: e.tensor_copy(
            B.wd[:, 2 * j:2 * j + 2, :], st[:, 0:2048].rearrange("p (c n) -> p c n", c=2)),
            R=[sd], W=[B.wd_d[j]])


def ffn_stage(P, B, C, X, Y, T, load_q='sp', store_q='act'):
    nblk = T // 256
    wdeps_gu = B.wg_d + B.wu_d

    def prep(b):
        hb = b % 2
        for tt in range(2):
            tile = b * 2 + tt
            xi = tile % B.NX
            xt, xd = B.xt[xi], B.xt_d[xi]
            st, sd = B.st[xi], B.st_d[xi]
            P.dma(load_q, xt[:], X.ap[tile * 128:(tile + 1) * 128, :], R=[X.deps[tile]], W=[xd])
            P.act(B.junk[:], xt[:], AF.Square, R=[xd], W=[B.junk_d, sd], accum_out=st[:, 0:1])
            P.act(st[:, 1:2], st[:, 0:1], AF.Sqrt, R=[sd, C.eps_d], W=[sd], scale=1.0 / D, bias=C.eps1[:, 0:1])
            P.op('dve', lambda e, st=st: e.reciprocal(st[:, 2:3], st[:, 1:2]), R=[sd], W=[sd])
            xn, xnd = B.xn[tt], B.xn_d[tt]
            P.act(xn[:], xt[:], AF.Copy, R=[xd, sd], W=[xnd], scale=st[:, 2:3])
            for dc in range(8):
                P.tr(B.psT[:, dc * 128:(dc + 1) * 128], xn[:, dc * 128:(dc + 1) * 128], C.ident[:],
                     R=[xnd, C.ident_d], W=[B.psT_d])
            P.op('dve', lambda e, hb=hb, tt=tt: e.tensor_copy(
                B.hT[hb][:, :, tt * 128:(tt + 1) * 128], B.psT[:, :].rearrange("p (c n) -> p c n", c=8)),
                R=[B.psT_d], W=[B.hT_d[hb]])

    def down(b, fc):
        ai = B.cnt_a[(b, fc)]
        for tt in range(2):
            for half in range(2):
                P.mm(B.o[tt][:, half * 512:(half + 1) * 512], B.a[ai][:, tt * 128:(tt + 1) * 128],
                     B.wd[:, fc, half * 512:(half + 1) * 512], start=(fc == 0), stop=(fc == NFC - 1),
                     R=[B.a_d[ai], B.wd_d[fc // 2]], W=[B.o_d[tt]])

    def post(b):
        for tt in range(2):
            tile = b * 2 + tt
            xi = tile % B.NX
            xt, xd = B.xt[xi], B.xt_d[xi]
            st, sd = B.st[xi], B.st_d[xi]
            P.act(B.junk[:], B.o[tt][:], AF.Square, R=[B.o_d[tt]], W=[B.junk_d, sd], accum_out=st[:, 4:5])
            P.act(st[:, 5:6], st[:, 4:5], AF.Sqrt, R=[sd, C.eps_d], W=[sd], scale=4.0 / D, bias=C.eps1[:, 1:2])
            P.op('dve', lambda e, st=st: e.reciprocal(st[:, 6:7], st[:, 5:6]), R=[sd], W=[sd])
            t, td = B.t[tt], B.t_d[tt]
            P.op('dve', lambda e, t=t, tt=tt: e.tensor_tensor(t[:], B.o[tt][:], B.gpost[:], ALU.mult),
                 R=[B.o_d[tt], B.gpost_d], W=[td])
            yt, yd = B.yt[tt], B.yt_d[tt]
            P.op('dve', lambda e, t=t, yt=yt, st=st, xt=xt: e.scalar_tensor_tensor(
                yt[:], t[:], st[:, 6:7], xt[:], ALU.mult, ALU.add),
                R=[td, sd, xd], W=[yd])
            P.dma(store_q, Y.ap[tile * 128:(tile + 1) * 128, :], yt[:], R=[yd], W=[Y.deps[tile]])

    B.cnt_a = {}
    prep(0)
    for b in range(nblk):
        hb = b % 2
        if b + 1 < nblk:
            prep(b + 1)
        for fc in range(NFC):
            gi = B.cnt % 2
            B.cnt += 1
            gu, gud = B.gu[gi], B.gu_d[gi]
            for which, wsb, wdp in ((0, B.wg, B.wg_d), (1, B.wu, B.wu_d)):
                for dc in range(8):
                    P.mm(gu[:, which * 256:(which + 1) * 256], wsb[:, dc, fc * 128:(fc + 1) * 128],
                         B.hT[hb][:, dc, :], start=(dc == 0), stop=(dc == 7),
                         R=[wdp[dc], B.hT_d[hb]], W=[gud])
            sg, sgd = B.sg[gi], B.sg_d[gi]
            P.act(sg[:], gu[:, 0:256], AF.Silu, R=[gud], W=[sgd])
            ai = (b * NFC + fc) % 3
            B.cnt_a[(b, fc)] = ai
            P.op('dve', lambda e, ai=ai, sg=sg, gu=gu: e.tensor_tensor(B.a[ai][:], sg[:], gu[:, 256:512], ALU.mult),
                 R=[sgd, gud], W=[B.a_d[ai]])
            if fc >= 2:
                down(b, fc - 2)
        down(b, NFC - 2)
        down(b, NFC - 1)
        post(b)


def norm_transpose(P, B, C, src_ap, src_deps, tile, hT_ap, hT_d, do_norm=True):
    xi = B.xrot % B.NX
    B.xrot += 1
    xt, xd = B.xt[xi], B.xt_d[xi]
    st, sd = B.st[xi], B.st_d[xi]
    xn, xnd = B.xn[xi % 2], B.xn_d[xi % 2]
    P.dma('sp', xt[:], src_ap[tile * 128:(tile + 1) * 128, :], R=src_deps, W=[xd])
    if do_norm:
        P.act(B.junk[:], xt[:], AF.Square, R=[xd], W=[B.junk_d, sd], accum_out=st[:, 0:1])
        P.act(st[:, 1:2], st[:, 0:1], AF.Sqrt, R=[sd, C.eps_d], W=[sd], scale=1.0 / D, bias=C.eps1[:, 0:1])
        P.op('dve', lambda e: e.reciprocal(st[:, 2:3], st[:, 1:2]), R=[sd], W=[sd])
        P.act(xn[:], xt[:], AF.Copy, R=[xd, sd], W=[xnd], scale=st[:, 2:3])
    else:
        P.act(xn[:], xt[:], AF.Copy, R=[xd], W=[xnd])
    for dc in range(8):
        P.tr(B.psT[:, dc * 128:(dc + 1) * 128], xn[:, dc * 128:(dc + 1) * 128], C.ident[:], R=[xnd, C.ident_d], W=[B.psT_d])
    P.op('dve', lambda e: e.tensor_copy(hT_ap, B.psT[:, :].rearrange("p (c n) -> p c n", c=8)), R=[B.psT_d], W=[hT_d])
    return xt, xd, st, sd


def inproj_stage(P, B, C, X, T, w_in, gcol_in, proj_out):
    P.dma('sp', B.gcol[:], gcol_in[:, :], W=[B.gcol_d])
    for dc in range(8):
        for hf in range(2):
            si = B.stage_i
            B.stage_i ^= 1
            st, sd = B.stage[si], B.stage_d[si]
            P.dma('sp', st[:, 0:1420], w_in[dc * 128:(dc + 1) * 128, hf * 1420:(hf + 1) * 1420], W=[sd])
            P.act(B.wg[:, dc, hf * 1420:(hf + 1) * 1420], st[:, 0:1420], AF.Copy, R=[sd, B.gcol_d], W=[B.wg_d[dc]],
                  scale=B.gcol[:, dc:dc + 1])
    banks = [(B.gu[0][:, :], B.gu_d[0]), (B.gu[1][:, :], B.gu_d[1]), (B.o[0][:, 0:512], B.o_d[0]), (B.o[1][:, 0:512], B.o_d[1])]
    groups = [(0, 512), (512, 1024), (1024, 1536), (1536, 2048), (2048, 2560), (2560, 2840)]
    k = 0
    for tile in range(T // 128):
        hb = tile % 2
        hT_ap = B.hT[hb][:, :, 0:128]
        norm_transpose(P, B, C, X.ap, [X.deps[tile]], tile, hT_ap, B.hT_d[hb])
        for (c0, c1) in groups:
            bk, bkd = banks[k % 4]
            pr, prd = B.yt[k % 2], B.yt_d[k % 2]
            k += 1
            n = c1 - c0
            for dc in range(8):
                P.mm(bk[:, 0:n], B.hT[hb][:, dc, 0:128], B.wg[:, dc, c0:c1], dc == 0, dc == 7, R=[B.hT_d[hb], B.wg_d[dc]], W=[bkd])
            if k % 2:
                P.act(pr[:, 0:n], bk[:, 0:n], AF.Copy, R=[bkd], W=[prd])
            else:
                P.op('dve', lambda e: e.tensor_copy(pr[:, 0:n], bk[:, 0:n]), R=[bkd], W=[prd])
            P.dma('act', proj_out[tile * 128:(tile + 1) * 128, c0:c1], pr[:, 0:n], R=[prd])


def projres_stage(P, B, C, T, a_src, W, post_g, Xres, Y, conv=None):
    for j in range(4):
        si = B.stage_i
        B.stage_i ^= 1
        st, sd = B.stage[si], B.stage_d[si]
        P.dma('sp', st[:, 0:2048].rearrange("p (c n) -> p c n", c=2), W[j * 256:(j + 1) * 256, :].rearrange("(c p) n -> p c n", p=128), W=[sd])
        P.op('pool', lambda e: e.tensor_copy(B.wd[:, 2 * j:2 * j + 2, :], st[:, 0:2048].rearrange("p (c n) -> p c n", c=2)),
             R=[sd], W=[B.wd_d[j]])
    P.dma('sp', B.gpost[:], post_g.partition_broadcast(128), W=[B.gpost_d])
    if conv is not None:
        cwk = [B.wu[:, kk, 0:1024].bitcast(F32) for kk in range(4)]
        cwk_d = [B.wu_d[kk] for kk in range(4)]
        for kk in range(3):
            P.dma('sp', cwk[kk][:, :], conv['w'][kk, :].partition_broadcast(128), W=[cwk_d[kk]])
        P.dma('sp', cwk[3][:, :], conv['b'].partition_broadcast(128), W=[cwk_d[3]])
        cv = [B.wu[:, 4 + i, 0:2048].bitcast(F32) for i in range(3)]
        cv_d = [B.wu_d[4 + i] for i in range(3)]
        cbt = B.wu[:, 7, 0:1024].bitcast(F32)
        cbt_d = B.wu_d[7]
    for tile in range(T // 128):
        ai = B.xrot % B.NX
        B.xrot += 1
        at, ad = B.xt[ai], B.xt_d[ai]
        an, and_ = B.xn[ai % 2], B.xn_d[ai % 2]
        if conv is None:
            P.dma('sp', at[:], a_src[tile * 128:(tile + 1) * 128, :], W=[ad])
        else:
            P.dma('sp', at[:, 0:512], a_src[tile * 128:(tile + 1) * 128, :], W=[ad])
            r0 = tile * 128
            for sh in range(3):
                P.dma('sp', cv[sh][:, :], conv['cbcu'][r0 + sh:r0 + sh + 128, 512:1536], W=[cv_d[sh]])
            P.dma('sp', cbt[:, :], conv['cbcu'][r0 + 2:r0 + 130, 0:512], W=[cbt_d])
            for sh in range(3):
                P.op('dve', lambda e: e.tensor_tensor(cv[sh][:, 0:512], cv[sh][:, 0:512], cv[sh][:, 512:1024], ALU.mult),
                     R=[cv_d[sh]], W=[cv_d[sh]])
                P.op('dve', lambda e: e.tensor_tensor(cv[sh][:, 0:512], cv[sh][:, 0:512], cwk[sh][:, :], ALU.mult),
                     R=[cv_d[sh], cwk_d[sh]], W=[cv_d[sh]])
            P.op('dve', lambda e: e.tensor_tensor(cv[0][:, 0:512], cv[0][:, 0:512], cv[1][:, 0:512], ALU.add), R=[cv_d[1]], W=[cv_d[0]])
            P.op('dve', lambda e: e.tensor_tensor(cv[0][:, 0:512], cv[0][:, 0:512], cv[2][:, 0:512], ALU.add), R=[cv_d[2]], W=[cv_d[0]])
            P.op('dve', lambda e: e.tensor_tensor(cv[0][:, 0:512], cv[0][:, 0:512], cwk[3][:, :], ALU.add), R=[cwk_d[3]], W=[cv_d[0]])
            P.op('dve', lambda e: e.tensor_tensor(at[:, 512:1024], cv[0][:, 0:512], cbt[:, :], ALU.mult), R=[cv_d[0], cbt_d], W=[ad])
        P.act(an[:], at[:], AF.Copy, R=[ad], W=[and_])
        hb = tile % 2
        for dc in range(8):
            P.tr(B.psT[:, dc * 128:(dc + 1) * 128], an[:, dc * 128:(dc + 1) * 128], C.ident[:], R=[and_, C.ident_d], W=[B.psT_d])
        P.op('dve', lambda e: e.tensor_copy(B.hT[hb][:, :, 0:128], B.psT[:, :].rearrange("p (c n) -> p c n", c=8)),
             R=[B.psT_d], W=[B.hT_d[hb]])
        tt = tile % 2
        for half in range(2):
            for fc in range(8):
                P.mm(B.o[tt][:, half * 512:(half + 1) * 512], B.hT[hb][:, fc, 0:128], B.wd[:, fc, half * 512:(half + 1) * 512],
                     fc == 0, fc == 7, R=[B.hT_d[hb], B.wd_d[fc // 2]], W=[B.o_d[tt]])
        xi = B.xrot % B.NX
        B.xrot += 1
        xt, xd = B.xt[xi], B.xt_d[xi]
        st, sd = B.st[xi], B.st_d[xi]
        P.dma('sp', xt[:], Xres.ap[tile * 128:(tile + 1) * 128, :], R=[Xres.deps[tile]], W=[xd])
        P.act(B.junk[:], B.o[tt][:], AF.Square, R=[B.o_d[tt]], W=[B.junk_d, sd], accum_out=st[:, 4:5])
        P.act(st[:, 5:6], st[:, 4:5], AF.Sqrt, R=[sd, C.eps_d], W=[sd], scale=1.0 / D, bias=C.eps1[:, 0:1])
        P.op('dve', lambda e: e.reciprocal(st[:, 6:7], st[:, 5:6]), R=[sd], W=[sd])
        t, td = B.t[tt], B.t_d[tt]
        P.op('dve', lambda e: e.tensor_tensor(t[:], B.o[tt][:], B.gpost[:], ALU.mult), R=[B.o_d[tt], B.gpost_d], W=[td])
        yt, yd = B.yt[tt], B.yt_d[tt]
        P.op('dve', lambda e: e.scalar_tensor_tensor(yt[:], t[:], st[:, 6:7], xt[:], ALU.mult, ALU.add), R=[td, sd, xd], W=[yd])
        P.dma('act', Y.ap[tile * 128:(tile + 1) * 128, :], yt[:], R=[yd], W=[Y.deps[tile]])


D = 1024
EPS = 1e-6
LNX_EPS = 64e-5
CW = math.exp(-0.5)


class Tl:
    def __init__(self, P, shape, dt, name):
        self.t = P.sbuf(shape, dt, name)
        self.d = Dep()


class Banks:
    def __init__(self, P, n=7):
        self.b = [P.psum([128, 512], F32, "bank%d" % i) for i in range(n)]
        self.d = [Dep(True) for _ in range(n)]
        self.i = 0
        self.n = n

    def get(self):
        i = self.i
        self.i = (i + 1) % self.n
        return self.b[i], self.d[i]


def rwkv_consts_np():
    p = np.arange(128)[:, None]
    f = np.arange(128)[None, :]
    c = {}
    c['ident'] = np.eye(128, dtype=np.float32)
    c['negSU'] = -(p < f).astype(np.float32)
    c['negSL'] = -(f < p).astype(np.float32)
    c['SU'] = (p < f).astype(np.float32)
    c['U'] = (p <= f).astype(np.float32)
    c['TriS'] = np.ones((128, 128), np.float32)
    c['OnesS'] = np.ones((128, 128), np.float32)
    return np.stack([c[k] for k in ('ident', 'negSU', 'negSL', 'SU', 'U', 'TriS', 'OnesS')], 0)


def rwkv_stage(P, S, xpad, zout, w, cmat):
    NT = S // 128
    BK = Banks(P, 7)
    psT = P.psum([128, 1024], BF16, "psT")
    psT_d = Dep(True)
    cf = Tl(P, [128, 7, 128], F32, "cf")
    P.dma('sp', cf.t[:], cmat.rearrange("k p f -> p k f"), W=[cf.d])
    cb = Tl(P, [128, 6, 128], BF16, "cb")
    P.op('dve', lambda e: e.tensor_copy(cb.t[:], cf.t[:, 0:6, :]), R=[cf.d], W=[cb.d])
    ident = cb.t[:, 0, :]

    def bc4(k):
        return cb.t[:, k, :].unsqueeze(1).broadcast_to([128, 4, 128])
    TriS = cf.t[:, 5, :]
    OnesS = cf.t[:, 6, :]
    onescol = cf.t[:, 6, 0:1]
    epsc = Tl(P, [128, 2], F32, "epsc")
    P.op('dve', lambda e: e.memset(epsc.t[:, 0:1], EPS), W=[epsc.d])
    P.op('dve', lambda e: e.memset(epsc.t[:, 1:2], LNX_EPS), W=[epsc.d])
    Wall = Tl(P, [128, 8, 2, 1024], BF16, "Wall")
    gcol = Tl(P, [128, 8], F32, "gcol")
    mucol = Tl(P, [128, 6, 8], F32, "mucol")
    P.dma('sp', gcol.t[:], w['gmu'][:, 0, :], W=[gcol.d])
    P.dma('sp', mucol.t[:], w['gmu'][:, 1:7, :], W=[mucol.d])
    mucol_all = Dep()
    sc = Tl(P, [128, 6, 2, 8], F32, "sc")
    for m in range(6):
        P.op('dve', lambda e, m=m: e.tensor_tensor(sc.t[:, m, 1, :], mucol.t[:, m, :], gcol.t[:, :], ALU.mult),
             R=[gcol.d, mucol.d], W=[sc.d])
        P.op('dve', lambda e, m=m: e.tensor_tensor(sc.t[:, m, 0, :], gcol.t[:, :], sc.t[:, m, 1, :], ALU.subtract),
             R=[gcol.d], W=[sc.d])
    stg = [Tl(P, [128, 256], F32, "stg%d" % i) for i in range(2)]
    si = 0
    for name, m, c0, n in (('w_r', 0, 0, 256), ('w_k', 2, 256, 256), ('w_v', 3, 512, 256),
                           ('w_dec1', 1, 768, 64), ('w_a1', 4, 832, 64), ('w_g1', 5, 896, 128)):
        for dc in range(8):
            st = stg[si]
            si ^= 1
            P.dma('sp', st.t[:, 0:n], w[name][dc * 128:(dc + 1) * 128, :], W=[st.d])
            for cp in range(2):
                P.act(Wall.t[:, dc, cp, c0:c0 + n], st.t[:, 0:n], AF.Copy, R=[st.d, sc.d], W=[Wall.d],
                      scale=sc.t[:, m, cp, dc:dc + 1])
    W2A = Tl(P, [128, 256], BF16, "W2A")
    W2G = Tl(P, [128, 256], BF16, "W2G")
    st = stg[si]; si ^= 1
    P.dma('sp', st.t[0:64, :], w['w_dec2'][:, :], W=[st.d])
    st2 = stg[si]; si ^= 1
    P.dma('sp', st2.t[64:128, :], w['w_a2'][:, :], W=[st2.d])
    P.op('dve', lambda e, st=st: e.tensor_copy(W2A.t[0:64, :], st.t[0:64, :]), R=[st.d], W=[W2A.d])
    P.op('dve', lambda e, st2=st2: e.tensor_copy(W2A.t[64:128, :], st2.t[64:128, :]), R=[st2.d], W=[W2A.d])
    st = stg[si]; si ^= 1
    P.dma('sp', st.t[:, :], w['w_g2'][:, :], W=[st.d])
    P.op('dve', lambda e, st=st: e.tensor_copy(W2G.t[:, :], st.t[:, :]), R=[st.d], W=[W2G.d])
    bcn = ('w0', 'a0', 'k_k', 'k_a', 'r_k', 'lnx_g', 'lnx_b')
    bct = Tl(P, [128, 7, 256], F32, "bct")
    bcd = [Dep() for _ in bcn]
    for i, nme in enumerate(bcn):
        P.dma('sp', bct.t[:, i, :], w[nme].partition_broadcast(128), W=[bcd[i]])
    BCI = {n: i for i, n in enumerate(bcn)}

    def bc(nme):
        return bct.t[:, BCI[nme], :], bcd[BCI[nme]]

    H = [Tl(P, [128, 64], F32, "H%d" % i) for i in range(2)]
    Hb = [Tl(P, [128, 64], BF16, "Hb%d" % i) for i in range(2)]
    for i in range(2):
        P.op('dve', lambda e, i=i: e.memset(H[i].t[:], 0.0), W=[H[i].d])
        P.op('dve', lambda e, i=i: e.memset(Hb[i].t[:], 0.0), W=[Hb[i].d])
    hT = [Tl(P, [128, 8, 129], BF16, "hT%d" % i) for i in range(2)]
    P.op('dve', lambda e: e.memset(hT[0].t[:, :, 0:1], 0.0), W=[hT[0].d])

    def mk(shape, dt, name, n=2):
        return [Tl(P, shape, dt, "%s_%d" % (name, i)) for i in range(n)]
    xt = mk([128, D], F32, "xt")
    xn = mk([128, D], BF16, "xn")
    junk = Tl(P, [128, D], BF16, "junk")
    st8 = mk([128, 8], F32, "st8")
    TA = mk([128, 128], BF16, "TA")
    TB = mk([128, 128], BF16, "TB")
    names_f = ['r_sb', 'k_sb', 'v_sb', 'wpre', 'sg', 'apre', 'a_sb', 'g_sb', 'kkr', 'sq', 'kkn', 't1', 'k2', 'ka',
               'cum', 'E1', 'E2', 'E3', 'E4', 'd4', 'd2', 'y_sb', 'yc', 'ysq', 'bon', 'zt']
    LONG = ('r_sb', 'k2', 'v_sb', 'g_sb')
    F = {n: mk([128, 256], F32, n, 4 if n in LONG else 2) for n in names_f}
    s4 = mk([128, 16], F32, "s4f")
    s4b = mk([128, 16], F32, "s4b")
    gC = mk([128, 2], F32, "gC", 4)
    vb = mk([128, 256], BF16, "vb", 4)
    QT = mk([128, 4, 256], BF16, "QT")
    bhat = mk([128, 256], BF16, "bhat", 4)
    khat = mk([128, 256], BF16, "khat", 4)
    FT = mk([128, 8, 128], BF16, "FT", 4)
    Cm = {n: mk([128, 512], BF16, n, 4) for n in ('AkT', 'RbT', 'RkT')}
    NM = {n: [mk([128, 512], BF16, '%s%d' % (n, s_), 3) for s_ in range(4 if n == 'Pm' else 2)] for n in ('N', 'M', 'Pm')}
    Wn = mk([128, 256], BF16, "Wn")
    sghi = mk([128, 256], BF16, "sghi")
    sglo = mk([128, 256], BF16, "sglo")
    Ub = mk([128, 256], BF16, "Ub")

    def v4(ap):
        return ap.rearrange("p (h j) -> p h j", h=4)

    def b4(ap4):
        return ap4.unsqueeze(2).broadcast_to([128, 4, 64])

    def g5(ap):
        return ap.rearrange("p (h n) -> p h n", h=4)

    CTX = {}

    def front(tau):
        q = tau % 2
        q4 = tau % 4
        X, XN, ST, HT = xt[q], xn[q], st8[q], hT[q]
        P.dma('sp', X.t[:], xpad[tau * 128 + 1: tau * 128 + 129, :], W=[X.d])
        P.act(junk.t[:], X.t[:], AF.Square, R=[X.d], W=[junk.d, ST.d], accum_out=ST.t[:, 0:1])
        P.act(ST.t[:, 1:2], ST.t[:, 0:1], AF.Sqrt, R=[ST.d, epsc.d], W=[ST.d], scale=1.0 / D, bias=epsc.t[:, 0:1])
        P.op('dve', lambda e, ST=ST: e.reciprocal(ST.t[:, 2:3], ST.t[:, 1:2]), R=[ST.d], W=[ST.d])
        P.act(XN.t[:], X.t[:], AF.Copy, R=[X.d, ST.d], W=[XN.d], scale=ST.t[:, 2:3])
        for dc in range(8):
            P.tr(psT[:, dc * 128:(dc + 1) * 128], XN.t[:, dc * 128:(dc + 1) * 128], ident, R=[XN.d, cb.d], W=[psT_d])
        P.op('dve', lambda e, HT=HT: e.tensor_copy(HT.t[:, :, 1:129], psT[:, :].rearrange("p (c n) -> p c n", c=8)),
             R=[psT_d], W=[HT.d])
        if tau > 0:
            HP = hT[1 - q]
            P.op('dve', lambda e, HT=HT, HP=HP: e.tensor_copy(HT.t[:, :, 0:1], HP.t[:, :, 128:129]),
                 R=[HP.d], W=[HT.d])
        yield
        bA, dA = BK.get()
        bB, dB = BK.get()
        bC, dC = BK.get()
        bD, dD = BK.get()

        def proj(out, c0, c1, dep):
            k = 0
            for cp in range(2):
                for dc in range(8):
                    P.mm(out, HT.t[:, dc, 1 - cp:129 - cp], Wall.t[:, dc, cp, c0:c1], k == 0, k == 15,
                         R=[HT.d, Wall.d], W=[dep])
                    k += 1
        proj(bA[:, 0:512], 0, 512, dA)
        proj(bB[:, 0:256], 512, 768, dB)
        for (o0, c0) in ((0, 768), (128, 896)):
            k = 0
            for cp in range(2):
                for dc in range(8):
                    P.mm(bC[:, o0:o0 + 128], Wall.t[:, dc, cp, c0:c0 + 128], HT.t[:, dc, 1 - cp:129 - cp], k == 0, k == 15,
                         R=[HT.d, Wall.d], W=[dC])
                    k += 1
        ta, tb = TA[q], TB[q]
        P.act(ta.t[0:64, :], bC[0:64, 0:128], AF.Tanh, R=[dC], W=[ta.d])
        P.act(ta.t[64:128, :], bC[64:128, 0:128], AF.Copy, R=[dC], W=[ta.d])
        P.act(tb.t[:, :], bC[:, 128:256], AF.Sigmoid, R=[dC], W=[tb.d])
        P.mm(bB[:, 256:512], ta.t[0:64, :], W2A.t[0:64, :], True, True, R=[ta.d, W2A.d], W=[dB])
        P.mm(bD[:, 0:256], ta.t[64:128, :], W2A.t[64:128, :], True, True, R=[ta.d, W2A.d], W=[dD])
        P.mm(bD[:, 256:512], tb.t[:, :], W2G.t[:, :], True, True, R=[tb.d, W2G.d], W=[dD])
        f = {n: F[n][q4 if n in LONG else q] for n in names_f}

        def A_(out, in_, func, R, **kw):
            P.act(out.t[:] if isinstance(out, Tl) else out, in_, func, R=R, W=[out.d] if isinstance(out, Tl) else [], **kw)

        def TT(out, a, b, op, R):
            P.op('dve', lambda e: e.tensor_tensor(out.t[:], a, b, op), R=R, W=[out.d])

        def STT(out, a, s, b, op0, op1, R):
            P.op('dve', lambda e: e.scalar_tensor_tensor(out.t[:], a, s, b, op0, op1), R=R, W=[out.d])
        A_(f['r_sb'], bA[:, 0:256], AF.Copy, [dA])
        A_(f['k_sb'], bA[:, 256:512], AF.Copy, [dA])
        A_(f['v_sb'], bB[:, 0:256], AF.Copy, [dB])
        VB = vb[q4]
        P.op('dve', lambda e, VB=VB: e.tensor_copy(VB.t[:], f['v_sb'].t[:]), R=[f['v_sb'].d], W=[VB.d])
        t_, d_ = bc('w0')
        TT(f['wpre'], bB[:, 256:512], t_, ALU.add, [dB, d_])
        A_(f['sg'], f['wpre'].t[:], AF.Sigmoid, [f['wpre'].d])
        t_, d_ = bc('a0')
        TT(f['apre'], bD[:, 0:256], t_, ALU.add, [dD, d_])
        A_(f['a_sb'], f['apre'].t[:], AF.Sigmoid, [f['apre'].d])
        A_(f['g_sb'], bD[:, 256:512], AF.Copy, [dD])
        yield
        t_, d_ = bc('k_k')
        TT(f['kkr'], f['k_sb'].t[:], t_, ALU.mult, [f['k_sb'].d, d_])
        TT(f['sq'], f['kkr'].t[:], f['kkr'].t[:], ALU.mult, [f['kkr'].d])
        S4 = s4[q]
        P.op('dve', lambda e, S4=S4: e.tensor_reduce(S4.t[:, 0:4], v4(f['sq'].t[:, :]), AX.X, ALU.add),
             R=[f['sq'].d], W=[S4.d])
        P.op('dve', lambda e, S4=S4: e.tensor_scalar(S4.t[:, 0:4], S4.t[:, 0:4], 1e-12, None, ALU.max), R=[S4.d], W=[S4.d])
        P.act(S4.t[:, 4:8], S4.t[:, 0:4], AF.Sqrt, R=[S4.d], W=[S4.d])
        P.op('dve', lambda e, S4=S4: e.reciprocal(S4.t[:, 8:12], S4.t[:, 4:8]), R=[S4.d], W=[S4.d])
        P.op('dve', lambda e, S4=S4: e.tensor_tensor(v4(f['kkn'].t[:, :]), v4(f['kkr'].t[:, :]), b4(S4.t[:, 8:12]), ALU.mult),
             R=[f['kkr'].d, S4.d], W=[f['kkn'].d])
        yield
        t_, d_ = bc('k_a')
        STT(f['t1'], f['a_sb'].t[:], -1.0, t_, ALU.add, ALU.mult, [f['a_sb'].d, d_])
        STT(f['k2'], f['t1'].t[:], 1.0, f['k_sb'].t[:], ALU.add, ALU.mult, [f['t1'].d, f['k_sb'].d])
        TT(f['ka'], f['kkn'].t[:], f['a_sb'].t[:], ALU.mult, [f['kkn'].d, f['a_sb'].d])
        yield
        SH, SL_ = sghi[q], sglo[q]
        P.op('dve', lambda e: e.tensor_copy(SH.t[:], f['sg'].t[:]), R=[f['sg'].d], W=[SH.d])
        P.op('dve', lambda e: e.tensor_tensor(SL_.t[:], f['sg'].t[:], SH.t[:], ALU.subtract), R=[f['sg'].d, SH.d], W=[SL_.d])
        bE, dE = BK.get()
        for i_, S_ in enumerate((SH, SL_)):
            P.mm(bE[:, 0:256], cb.t[:, 4, :], S_.t[:], i_ == 0, i_ == 1, R=[cb.d, S_.d], W=[dE])
        for i_, S_ in enumerate((SH, SL_)):
            P.mm(bE[:, 256:512], cb.t[:, 5, :], S_.t[:], i_ == 0, i_ == 1, R=[cb.d, S_.d], W=[dE])
        bF, dF = BK.get()
        for pr in range(2):
            for i_, S_ in enumerate((SH, SL_)):
                P.mm(bF[:, pr * 128:(pr + 1) * 128], S_.t[:, pr * 128:(pr + 1) * 128], cb.t[:, 5, :], i_ == 0, i_ == 1,
                     R=[cb.d, S_.d], W=[dF])
        A_(f['cum'], bE[:, 0:256], AF.Copy, [dE], scale=-CW)
        STT(f['d4'], bE[:, 256:512], -CW, f['cum'].t[:], ALU.mult, ALU.subtract, [dE, f['cum'].d])
        STT(f['d2'], f['sg'].t[:], CW, f['cum'].t[:], ALU.mult, ALU.add, [f['sg'].d, f['cum'].d])
        A_(f['E1'], f['cum'].t[:], AF.Exp, [f['cum'].d])
        A_(f['E3'], f['cum'].t[:], AF.Exp, [f['cum'].d], scale=-1.0)
        A_(f['E4'], f['d4'].t[:], AF.Exp, [f['d4'].d])
        A_(f['E2'], f['d2'].t[:], AF.Exp, [f['d2'].d])
        GC = gC[q4]
        P.act(GC.t[:, 0:1], bF[:, 0:1], AF.Exp, R=[dF], W=[GC.d], scale=-CW)
        P.act(GC.t[:, 1:2], bF[:, 128:129], AF.Exp, R=[dF], W=[GC.d], scale=-CW)
        yield
        QTq = QT[q]
        for ty, (a, b) in enumerate((('r_sb', 'E1'), ('kkn', 'E2'), ('ka', 'E3'), ('k2', 'E3'))):
            P.op('dve', lambda e, ty=ty, a=a, b=b, QTq=QTq: e.tensor_tensor(QTq.t[:, ty, :], f[a].t[:], f[b].t[:], ALU.mult),
                 R=[f[a].d, f[b].d], W=[QTq.d])
        BH, KH = bhat[q4], khat[q4]
        P.op('dve', lambda e, BH=BH: e.tensor_tensor(BH.t[:], f['ka'].t[:], f['E4'].t[:], ALU.mult),
             R=[f['ka'].d, f['E4'].d], W=[BH.d])
        P.op('dve', lambda e, KH=KH: e.tensor_tensor(KH.t[:], f['k2'].t[:], f['E4'].t[:], ALU.mult),
             R=[f['k2'].d, f['E4'].d], W=[KH.d])
        yield
        for ty in range(4):
            for pair in range(2):
                idx = ty * 2 + pair
                P.tr(psT[:, idx * 128:(idx + 1) * 128], QTq.t[:, ty, pair * 128:(pair + 1) * 128], ident,
                     R=[QTq.d, cb.d], W=[psT_d])
        FTq = FT[q4]
        P.op('dve', lambda e, FTq=FTq: e.tensor_copy(FTq.t[:, :, :], psT[:, :].rearrange("p (c n) -> p c n", c=8)),
             R=[psT_d], W=[FTq.d])

        def fm(h, ty):
            pair, hh = h // 2, h % 2
            return FTq.t[hh * 64:(hh + 1) * 64, ty * 2 + pair, :]
        yield
        Nk, Mk, Pk = NM['N'][q][0], NM['M'][q][0], NM['Pm'][q4][0]
        AkT, RbT, RkT = Cm['AkT'][q4], Cm['RbT'][q4], Cm['RkT'][q4]

        def hv(ap, hh):
            return ap.rearrange("p (a b n) -> p a b n", a=2, b=2)[:, :, hh, :]

        def bc2(k):
            return cb.t[:, k, :].unsqueeze(1).broadcast_to([128, 2, 128])
        for T_, (la, ra), mi in ((Nk, (2, 1), 1), (Mk, (1, 2), 2), (AkT, (3, 1), 3), (RbT, (2, 0), 4), (RkT, (3, 0), 4)):
            for hh in range(2):
                bk, dk = BK.get()
                for pair in range(2):
                    h = pair * 2 + hh
                    P.mm(bk[:, pair * 128:(pair + 1) * 128], fm(h, la), fm(h, ra), True, True, R=[FTq.d], W=[dk])
                P.op('dve', lambda e: e.tensor_tensor(hv(T_.t[:, :], hh), bk[:, 0:256].rearrange("p (a n) -> p a n", a=2),
                                                      bc2(mi), ALU.mult), R=[dk, cb.d], W=[T_.d])
            yield
        P.op('dve', lambda e: e.tensor_tensor(g5(Pk.t[:, :]), g5(Nk.t[:, :]), bc4(0), ALU.add),
             R=[Nk.d, cb.d], W=[Pk.d])
        yield
        cur = 0
        for lvl in range(6):
            nxt = (cur + 1) % 3
            Nn, Mn = NM['N'][q][nxt], NM['M'][q][nxt]
            bm, dm = BK.get()
            for h in range(4):
                blk = slice(h * 128, (h + 1) * 128)
                P.mm(bm[:, blk], Nk.t[:, blk], Mk.t[:, blk], True, True, R=[Nk.d, Mk.d], W=[dm])
            if lvl < 5:
                bn, dn = BK.get()
                for h in range(4):
                    blk = slice(h * 128, (h + 1) * 128)
                    P.mm(bn[:, blk], Mk.t[:, blk], Nk.t[:, blk], True, True, R=[Nk.d, Mk.d], W=[dn])
            if lvl >= 1:
                Pn = NM['Pm'][q4][lvl % 3]
                bp, dp = BK.get()
                for h in range(4):
                    blk = slice(h * 128, (h + 1) * 128)
                    P.mm(bp[:, blk], Mk.t[:, blk], Pk.t[:, blk], True, True, R=[Mk.d, Pk.d], W=[dp])
            if lvl < 5:
                P.act(Nn.t[:, :], bn[:, :], AF.Copy, R=[dn], W=[Nn.d])
            P.op('dve', lambda e: e.tensor_copy(Mn.t[:, :], bm[:, :]), R=[dm], W=[Mn.d])
            if lvl >= 1:
                P.op('dve', lambda e: e.tensor_tensor(Pn.t[:, :], bp[:, :], Pk.t[:, :], ALU.add), R=[dp, Pk.d], W=[Pn.d])
                Pk = Pn
            Nk, Mk = Nn, Mn
            cur = nxt
            yield
        Pn = NM['Pm'][q4][0]
        bp, dp = BK.get()
        for h in range(4):
            blk = slice(h * 128, (h + 1) * 128)
            P.mm(bp[:, blk], Mk.t[:, blk], Pk.t[:, blk], True, True, R=[Mk.d, Pk.d], W=[dp])
        P.op('dve', lambda e: e.tensor_tensor(Pn.t[:, :], bp[:, :], Pk.t[:, :], ALU.add), R=[dp, Pk.d], W=[Pn.d])
        Pk = Pn

        CTX[tau] = dict(f=f, FTq=FTq, AkT=AkT, RbT=RbT, RkT=RkT, Pk=Pk, VB=VB, BH=BH, KH=KH, GC=GC, fm=fm)


    def back(tau):
        q = tau % 2
        c_ = CTX.pop(tau)
        f, FTq, AkT, RbT, RkT, Pk, VB, BH, KH, GC, fm = (c_[k] for k in ('f', 'FTq', 'AkT', 'RbT', 'RkT', 'Pk', 'VB', 'BH', 'KH', 'GC', 'fm'))
        S4 = s4b[q]
        f = dict(f)
        for n_ in ('y_sb', 'yc', 'ysq', 'bon', 'zt'):
            f[n_] = F[n_][q]

        def A_(out, in_, func, R, **kw):
            P.act(out.t[:] if isinstance(out, Tl) else out, in_, func, R=R, W=[out.d] if isinstance(out, Tl) else [], **kw)

        def TT(out, a, b, op, R):
            P.op('dve', lambda e: e.tensor_tensor(out.t[:], a, b, op), R=R, W=[out.d])

        def STT(out, a, s, b, op0, op1, R):
            P.op('dve', lambda e: e.scalar_tensor_tensor(out.t[:], a, s, b, op0, op1), R=R, W=[out.d])
        WN = Wn[q]
        for hh in range(2):
            bw, dw = BK.get()
            for pair in range(2):
                h = pair * 2 + hh
                c = slice(h * 64, (h + 1) * 64)
                blk = slice(h * 128, (h + 1) * 128)
                P.mm(bw[:, c], fm(h, 1), Hb[pair].t[hh * 64:(hh + 1) * 64, :], True, False, R=[FTq.d, Hb[pair].d], W=[dw])
                P.mm(bw[:, c], AkT.t[:, blk], VB.t[:, c], False, True, R=[AkT.d, VB.d], W=[dw])
            for pair in range(2):
                h = pair * 2 + hh
                c = slice(h * 64, (h + 1) * 64)
                P.act(WN.t[:, c], bw[:, c], AF.Copy, R=[dw], W=[WN.d], scale=-1.0)
        yield
        bu, du = BK.get()
        for h in range(4):
            c = slice(h * 64, (h + 1) * 64)
            blk = slice(h * 128, (h + 1) * 128)
            P.mm(bu[:, c], Pk.t[:, blk], WN.t[:, c], True, True, R=[Pk.d, WN.d], W=[du])
        UB = Ub[q]
        P.op('dve', lambda e, UB=UB, bu=bu: e.tensor_copy(UB.t[:, :], bu[:, 0:256]), R=[du], W=[UB.d])
        yield
        for hh in range(2):
            by, dy = BK.get()
            for pair in range(2):
                h = pair * 2 + hh
                c = slice(h * 64, (h + 1) * 64)
                blk = slice(h * 128, (h + 1) * 128)
                P.mm(by[:, c], fm(h, 0), Hb[pair].t[hh * 64:(hh + 1) * 64, :], True, False, R=[FTq.d, Hb[pair].d], W=[dy])
                P.mm(by[:, c], RbT.t[:, blk], UB.t[:, c], False, False, R=[RbT.d, UB.d], W=[dy])
                P.mm(by[:, c], RkT.t[:, blk], VB.t[:, c], False, True, R=[RkT.d, VB.d], W=[dy])
            for pair in range(2):
                h = pair * 2 + hh
                c = slice(h * 64, (h + 1) * 64)
                P.act(f['y_sb'].t[:, c], by[:, c], AF.Copy, R=[dy], W=[f['y_sb'].d])
        yield
        bh, dh = BK.get()
        for pair in range(2):
            pc = slice(pair * 128, (pair + 1) * 128)
            P.mm(bh[:, pc], BH.t[:, pc], UB.t[:, pc], True, False, R=[BH.d, UB.d], W=[dh])
            P.mm(bh[:, pc], KH.t[:, pc], VB.t[:, pc], False, True, R=[KH.d, VB.d], W=[dh])
        for pair in range(2):
            for hh in range(2):
                rows = slice(hh * 64, (hh + 1) * 64)
                P.op('dve', lambda e, pair=pair, hh=hh, rows=rows, bh=bh, GC=GC: e.scalar_tensor_tensor(
                    H[pair].t[rows, :], H[pair].t[rows, :], GC.t[rows, pair:pair + 1],
                    bh[rows, pair * 128 + hh * 64: pair * 128 + hh * 64 + 64], ALU.mult, ALU.add),
                    R=[GC.d, dh, Hb[pair].d], W=[H[pair].d])
            P.act(Hb[pair].t[:, :], H[pair].t[:, :], AF.Copy, R=[H[pair].d], W=[Hb[pair].d])
        yield
        y = f['y_sb']
        P.op('dve', lambda e, S4=S4: e.tensor_reduce(S4.t[:, 12:16], v4(y.t[:, :]), AX.X, ALU.add), R=[y.d], W=[S4.d])
        P.op('dve', lambda e, S4=S4: e.tensor_scalar(S4.t[:, 12:16], S4.t[:, 12:16], -1.0 / 64, None, ALU.mult), R=[S4.d], W=[S4.d])
        P.op('dve', lambda e, S4=S4: e.tensor_tensor(v4(f['yc'].t[:, :]), v4(y.t[:, :]), b4(S4.t[:, 12:16]), ALU.add),
             R=[y.d, S4.d], W=[f['yc'].d])
        TT(f['ysq'], f['yc'].t[:], f['yc'].t[:], ALU.mult, [f['yc'].d])
        P.op('dve', lambda e, S4=S4: e.tensor_reduce(S4.t[:, 0:4], v4(f['ysq'].t[:, :]), AX.X, ALU.add), R=[f['ysq'].d], W=[S4.d])
        P.act(S4.t[:, 4:8], S4.t[:, 0:4], AF.Sqrt, R=[S4.d, epsc.d], W=[S4.d], scale=1.0 / 64, bias=epsc.t[:, 1:2])
        P.op('dve', lambda e, S4=S4: e.reciprocal(S4.t[:, 8:12], S4.t[:, 4:8]), R=[S4.d], W=[S4.d])
        P.op('dve', lambda e, S4=S4: e.tensor_tensor(v4(f['ysq'].t[:, :]), v4(f['yc'].t[:, :]), b4(S4.t[:, 8:12]), ALU.mult),
             R=[f['yc'].d, S4.d], W=[f['ysq'].d])
        t_, d_ = bc('lnx_g')
        TT(f['yc'], f['ysq'].t[:], t_, ALU.mult, [f['ysq'].d, d_])
        t_, d_ = bc('lnx_b')
        TT(f['ysq'], f['yc'].t[:], t_, ALU.add, [f['yc'].d, d_])
        yield
        t_, d_ = bc('r_k')
        TT(f['bon'], f['r_sb'].t[:], t_, ALU.mult, [f['r_sb'].d, d_])
        TT(f['yc'], f['bon'].t[:], f['k2'].t[:], ALU.mult, [f['bon'].d, f['k2'].d])
        P.op('dve', lambda e, S4=S4: e.tensor_reduce(S4.t[:, 12:16], v4(f['yc'].t[:, :]), AX.X, ALU.add), R=[f['yc'].d], W=[S4.d])
        P.op('dve', lambda e, S4=S4: e.tensor_tensor(v4(f['bon'].t[:, :]), v4(f['v_sb'].t[:, :]), b4(S4.t[:, 12:16]), ALU.mult),
             R=[f['v_sb'].d, S4.d], W=[f['bon'].d])
        TT(f['yc'], f['ysq'].t[:], f['bon'].t[:], ALU.add, [f['ysq'].d, f['bon'].d])
        TT(f['zt'], f['yc'].t[:], f['g_sb'].t[:], ALU.mult, [f['yc'].d, f['g_sb'].d])
        P.dma('act', zout[tau * 128:(tau + 1) * 128, :], f['zt'].t[:], R=[f['zt'].d])


    def backs(a, b):
        yield from back(a)
        yield from back(b)

    def drive(gens):
        gens = list(gens)
        while gens:
            for g_ in list(gens):
                try:
                    next(g_)
                except StopIteration:
                    gens.remove(g_)

    assert NT % 2 == 0
    drive([front(0), front(1)])
    for p_ in range(NT // 2):
        gl_ = []
        if 2 * p_ + 2 < NT:
            gl_ = [front(2 * p_ + 2), front(2 * p_ + 3)]
        drive(gl_ + [backs(2 * p_, 2 * p_ + 1)])


NEG = 32768.0
SLOPES = [2.0 ** (-(h + 1)) for h in range(8)]
GC = 1.5957691216057308


class _AT:
    pass


class Arena:
    def __init__(self, P, nbytes):
        self.cap = nbytes
        self.t = P.sbuf([128, nbytes // 2], BF16, "arena")
        self.off = 0
        self.deps = []
        self.inh_w = {}
        self.inh_r = {}

    def alloc(self, shape, dt, name=None):
        n = 1
        for s_ in shape[1:]:
            n *= s_
        nb = n * (4 if dt == F32 else 2)
        nb = (nb + 3) // 4 * 4
        assert self.off + nb <= self.cap, ("arena overflow", name, self.off, nb)
        ap = self.t[0:shape[0], self.off // 2:(self.off + nb) // 2]
        if dt == F32:
            ap = ap.bitcast(F32)
        ap = ap[:, 0:n]
        if len(shape) == 3:
            ap = ap.rearrange("p (a b) -> p a b", a=shape[1])
        elif len(shape) == 4:
            ap = ap.rearrange("p (a b c) -> p a b c", a=shape[1], b=shape[2])
        self.off += nb
        o = _AT()
        o.t = ap
        o.d = Dep()
        o.d.w = dict(self.inh_w)
        o.d.r = dict(self.inh_r)
        self.deps.append(o.d)
        return o

    def reset(self):
        for d in self.deps:
            for src in (d.w, d.r):
                for k, v in src.items():
                    if self.inh_w.get(k, 0) < v:
                        self.inh_w[k] = v
        self.inh_r = dict(self.inh_w)
        self.deps = []
        self.off = 0


def nsa_host_consts(S, m):
    NJ = S // 512
    NKT = S // 128
    NCT = (S // 16 - 1 + 127) // 128
    iq = np.arange(128)
    sl = np.array(SLOPES, np.float32)
    c = {}
    qaug = np.zeros((NJ, 128, 8, 5), np.float32)
    for j in range(NJ):
        qi = 4 * j + m
        qaug[j, :, :, 0] = -sl[None, :] * iq[:, None]
        qaug[j, :, :, 1] = sl[None, :]
        qaug[j, :, :, 2] = -sl[None, :] * 128 * qi
        qaug[j, :, :, 3] = sl[None, :] * 128
        qaug[j, :, :, 4] = 31 * sl[None, :]
    c['qaug'] = qaug.reshape(NJ * 128, 40)
    ka = np.zeros((128, NKT, 5), np.float32)
    ka[:, :, 0] = 1
    ka[:, :, 1] = iq[:, None]
    ka[:, :, 2] = 1
    ka[:, :, 3] = np.arange(NKT)[None, :]
    c['kaug_slc'] = ka.reshape(128, NKT * 5)
    kw = np.zeros((128, 5, 5), np.float32)
    kw[:, :, 0] = 1
    kw[:, :, 1] = iq[:, None]
    kw[:, :, 3] = (np.arange(5) - 4)[None, :]
    c['kaug_win'] = kw.reshape(128, 25)
    n = np.arange(NCT * 128)
    c['kaug_cmp'] = np.stack([np.ones_like(n), 16 * (n % 128), np.ones_like(n), 16 * (n // 128), np.ones_like(n)], 0).astype(np.float32)
    pw = np.zeros((NCT * 128, 256), np.float32)
    for nn in range(NCT * 128):
        jj = nn // 4
        if jj < 256:
            pw[nn, jj] += 1.0 if nn % 4 < 3 else 0.5
        if nn % 4 == 3 and jj + 1 < 256:
            pw[nn, jj + 1] += 0.5
    c['poolw'] = pw.reshape(NCT, 128, 256).transpose(1, 0, 2).reshape(128, NCT * 256)
    ex = np.zeros((128, 64, 128), np.float32)
    key = np.arange(128)
    for cc in range(64):
        ex[2 * cc + (key >= 64), cc, key] = 1.0
    c['ex'] = ex.reshape(128, 64 * 128)
    cm = np.zeros((NJ, 2, 128, 128), np.float32)
    for j in range(NJ):
        qi = 4 * j + m
        NT = j // 4
        for ab, nt in enumerate((NT, NT - 1)):
            nn = 128 * nt + iq[:, None]
            cm[j, ab] = (16 * nn + 31 <= 128 * qi + iq[None, :])
    c['cmask'] = ((cm - 1.0) * NEG).reshape(NJ * 2 * 128, 128)
    sm = np.zeros((4, 128, 128), np.float32)
    for s in range(4):
        sm[s] = 1.0 if s < m else ((iq[:, None] <= iq[None, :]) if s == m else 0.0)
    c['smask'] = ((sm - 1.0) * NEG).transpose(1, 0, 2).reshape(128, 4 * 128)
    wm = np.zeros((2, 5, 128, 128), np.float32)
    for st, qi in enumerate((m, m + 4)):
        for slot in range(5):
            kt = qi - 4 + slot
            d = 128 * (qi - kt) + iq[None, :] - iq[:, None]
            wm[st, slot] = ((d >= 0) & (d < 512)) if kt >= 0 else 0.0
    c['wmask'] = ((wm - 1.0) * NEG).transpose(2, 0, 1, 3).reshape(128, 10 * 128)
    sb = np.zeros((NJ, 128, 256), np.float32)
    jj = np.arange(256)[None, :]
    for j in range(NJ):
        qi = 4 * j + m
        blk = (128 * qi + iq[:, None]) // 64
        forced = (jj == 0) | (jj == blk) | (jj == blk - 1)
        sb[j] = np.where(forced, 1e4, 0.0)
        sb[j] = np.where(jj <= blk, sb[j], -1e30)
    c['selbias'] = sb.reshape(NJ * 128, 256)
    c['ident'] = np.eye(128, dtype=np.float32)
    return c


def nsa_host_data(proj_b, m, S):
    NJ = S // 512
    d = {}
    tiles = [4 * j + m for j in range(NJ)]
    d['qg'] = np.concatenate([proj_b[t * 128:(t + 1) * 128, 0:536] for t in tiles], 0)
    kv = proj_b[:, 536:1304]
    d['kv'] = np.concatenate([kv, np.zeros((128, 768), np.float32)], 0)
    kwv = np.concatenate([np.zeros((512, 256), np.float32), proj_b[:, 1048:1304]], 0)
    d['kwin'] = np.concatenate([kwv[(t - 4) * 128 + 512:(t + 1) * 128 + 512] for t in tiles], 0)
    return {k: np.ascontiguousarray(v, dtype=np.float32) for k, v in d.items()}


def nsa_stage(P, S, dr, w):
    NJ = S // 512
    NKT = S // 128
    NCT = (S // 16 - 1 + 127) // 128
    bank = [P.psum([128, 512], F32, "bank%d" % i) for i in range(7)]
    bd = [Dep(True) for _ in range(7)]
    psT = P.psum([128, 1024], BF16, "psT")
    psT_d = Dep(True)
    rot = [0]

    def rbank(lo=0, hi=7):
        i = lo + rot[0] % (hi - lo)
        rot[0] += 1
        return bank[i], bd[i]
    AR = Arena(P, 54 * 1024)

    def AT(P_, shape, dt, name):
        return AR.alloc(shape, dt, name)
    ident_f = Tl(P, [128, 128], F32, "ident_f")
    ident = Tl(P, [128, 128], BF16, "identb")
    P.dma('sp', ident_f.t[:], dr['ident'][:, :], W=[ident_f.d])
    P.op('dve', lambda e: e.tensor_copy(ident.t[:], ident_f.t[:]), R=[ident_f.d], W=[ident.d])
    stage = AT(P, [128, 2048], F32, "cstage")
    exb = Tl(P, [128, 64, 128], BF16, "exb")
    for pc in range(4):
        P.dma('sp', stage.t[:, :], dr['ex'][:, pc * 2048:(pc + 1) * 2048], W=[stage.d])
        P.op('dve', lambda e: e.tensor_copy(exb.t[:, pc * 16:(pc + 1) * 16, :], stage.t[:, :].rearrange("p (c k) -> p c k", c=16)),
             R=[stage.d], W=[exb.d])
    kaug_slc = Tl(P, [128, NKT, 5], F32, "kaug_slc")
    P.dma('sp', kaug_slc.t[:], dr['kaug_slc'].rearrange("p (k c) -> p k c", c=5), W=[kaug_slc.d])
    kaug_win = Tl(P, [128, 5, 5], F32, "kaug_win")
    P.dma('sp', kaug_win.t[:], dr['kaug_win'].rearrange("p (k c) -> p k c", c=5), W=[kaug_win.d])
    smaskb = Tl(P, [128, 4, 128], BF16, "smaskb")
    wmaskb = Tl(P, [128, 10, 128], BF16, "wmaskb")
    P.dma('sp', stage.t[:, 0:512], dr['smask'][:, :], W=[stage.d])
    P.op('dve', lambda e: e.tensor_copy(smaskb.t[:], stage.t[:, 0:512].rearrange("p (k c) -> p k c", c=128)), R=[stage.d], W=[smaskb.d])
    P.dma('sp', stage.t[:, 0:1280], dr['wmask'][:, :], W=[stage.d])
    P.op('dve', lambda e: e.tensor_copy(wmaskb.t[:], stage.t[:, 0:1280].rearrange("p (k c) -> p k c", c=128)), R=[stage.d], W=[wmaskb.d])
    KTs = [Tl(P, [69, S], BF16, "KTs%d" % g) for g in range(2)]
    Vs = Tl(P, [128, NKT, 2, 65], BF16, "Vs")
    KTc = [Tl(P, [69, NCT * 128], BF16, "KTc%d" % g) for g in range(2)]
    Vc = Tl(P, [128, NCT, 2, 321], BF16, "Vc")
    P.op('dve', lambda e: e.memset(Vs.t[:, :, :, 64:65], 1.0), W=[Vs.d])
    P.op('dve', lambda e: e.memset(Vc.t[:, :, :, 64:65], 1.0), W=[Vc.d])
    P.dma('sp', stage.t[:, 0:NCT * 256], dr['poolw'][:, :], R=[], W=[stage.d])
    for g in range(2):
        P.op('dve', lambda e: e.tensor_copy(Vc.t[:, :, g, 65:321], stage.t[:, 0:NCT * 256].rearrange("p (t c) -> p t c", c=256)),
             R=[stage.d], W=[Vc.d])
    P.dma('sp', stage.t[64:69, 0:NCT * 128], dr['kaug_cmp'][:, :], W=[stage.d])
    for g in range(2):
        P.op('dve', lambda e: e.tensor_copy(KTc[g].t[64:69, :], stage.t[64:69, 0:NCT * 128]), R=[stage.d], W=[KTc[g].d])
    kmx = Tl(P, [128, 4], F32, "kmx")
    P.op('dve', lambda e: e.memset(kmx.t[:, :], 0.0), W=[kmx.d])
    kvt = [AT(P, [128, 256], F32, "kvt%d" % i) for i in range(2)]
    Ktm = [AT(P, [128, 2, 69], BF16, "Ktm%d" % i) for i in range(2)]
    sq = AT(P, [128, 512], F32, "sq")
    ss = [AT(P, [128, 8], F32, "ss%d" % i) for i in range(2)]
    def kprepA(kt):
        kv_, km, s_ = kvt[kt % 2], Ktm[kt % 2], ss[kt % 2]
        P.dma('sp', kv_.t[:, :], dr['kv'][kt * 128:(kt + 1) * 128, 256:512], W=[kv_.d])
        P.act(km.t[:, :, 0:64], kv_.t[:, 0:128].rearrange("p (g d) -> p g d", g=2), AF.Copy, R=[kv_.d], W=[km.d])
        for g in range(2):
            P.op('dve', lambda e: e.tensor_copy(km.t[:, g, 64:69], kaug_slc.t[:, kt, :]), R=[kaug_slc.d], W=[km.d])
        P.op('dve', lambda e: e.tensor_tensor(sq.t[:, 0:128], kv_.t[:, 0:128], kv_.t[:, 0:128], ALU.mult), R=[kv_.d], W=[sq.d])
        P.op('dve', lambda e: e.tensor_reduce(s_.t[:, 0:2], sq.t[:, 0:128].rearrange("p (g d) -> p g d", g=2), AX.X, ALU.add),
             R=[sq.d], W=[s_.d])
        P.op('dve', lambda e: e.tensor_tensor(kmx.t[:, 0:2], kmx.t[:, 0:2], s_.t[:, 0:2], ALU.max), R=[s_.d], W=[kmx.d])
        P.op('dve', lambda e: e.tensor_copy(Vs.t[:, kt, :, 0:64], kv_.t[:, 128:256].rearrange("p (g d) -> p g d", g=2)),
             R=[kv_.d], W=[Vs.d])

    def kprepB(kt):
        km = Ktm[kt % 2]
        for g in range(2):
            P.tr(psT[0:69, g * 128:(g + 1) * 128], km.t[:, g, :], ident.t[:], R=[km.d, ident.d], W=[psT_d])
        for g in range(2):
            P.act(KTs[g].t[0:69, kt * 128:(kt + 1) * 128], psT[0:69, g * 128:(g + 1) * 128], AF.Copy, R=[psT_d], W=[KTs[g].d])

    kprepA(0)
    for kt in range(NKT):
        if kt + 1 < NKT:
            kprepA(kt + 1)
        kprepB(kt)
    W1 = [AT(P, [128, 32, 128], BF16, "W1%s" % n) for n in "kv"]
    W2 = [AT(P, [128, 64], BF16, "W2%s" % n) for n in "kv"]
    peT = [AT(P, [128, 32, 2], BF16, "peT%s" % n) for n in "kv"]
    b1 = [AT(P, [128, 2], F32, "b1%s" % n) for n in "kv"]
    for i, n in enumerate("kv"):
        for half in range(2):
            for pc in range(2):
                P.dma('sp', stage.t[half * 64:(half + 1) * 64, 0:2048].rearrange("p (a h) -> p a h", a=16),
                      w['w1_' + n].rearrange("(a d) h -> d a h", d=64)[:, pc * 16:(pc + 1) * 16, :], W=[stage.d])
                P.op('dve', lambda e: e.tensor_copy(W1[i].t[half * 64:(half + 1) * 64, pc * 16:(pc + 1) * 16, :],
                                                    stage.t[half * 64:(half + 1) * 64, 0:2048].rearrange("p (a h) -> p a h", a=16)),
                     R=[stage.d], W=[W1[i].d])
        P.dma('sp', stage.t[:, 0:64], w['w2_' + n][:, :], W=[stage.d])
        P.op('dve', lambda e: e.tensor_copy(W2[i].t[:, :], stage.t[:, 0:64]), R=[stage.d], W=[W2[i].d])
        P.dma('sp', stage.t[0:32, 0:64], w['pe_' + n][:, :], W=[stage.d])
        petm = AT(P, [32, 64], BF16, "petm%s" % n)
        P.op('dve', lambda e: e.tensor_copy(petm.t[:, :], stage.t[0:32, 0:64]), R=[stage.d], W=[petm.d])
        P.tr(psT[0:64, 0:32], petm.t[:, :], ident.t[0:32, 0:32], R=[petm.d, ident.d], W=[psT_d])
        for dup in range(2):
            P.op('dve', lambda e: e.tensor_copy(peT[i].t[0:64, :, dup], psT[0:64, 0:32]), R=[psT_d], W=[peT[i].d])
        bk, bkd = rbank()
        for p_ in range(32):
            P.mm(bk[:, 0:2], W1[i].t[0:64, p_, :], peT[i].t[0:64, p_, :], p_ == 0, p_ == 31, R=[W1[i].d, peT[i].d], W=[bkd])
        P.act(b1[i].t[:, 0:2], bk[:, 0:2], AF.Copy, R=[bkd], W=[b1[i].d])
    kct = AT(P, [128, 2, 2304], BF16, "kct")
    P.op('dve', lambda e: e.memset(kct.t[:, :, :], 0.0), W=[kct.d])
    ctm = [AT(P, [128, 256], F32, "ctm%d" % i) for i in range(2)]
    ctb = [AT(P, [128, 256], BF16, "ctb%d" % i) for i in range(2)]
    gl = {n: [AT(P, [128, 128], F32, "gl_%s%d" % (n, i)) for i in range(2)] for n in ('xb', 'x2', 'in1', 'sg')}
    a1 = [AT(P, [128, 128], BF16, "a1_%d" % i) for i in range(2)]
    cnt = 0
    for nt in range(NCT):
        ntile = min(17, (S + 128 - nt * 2048) // 128)
        def cA(tt):
            c_, cb_ = ctm[tt % 2], ctb[tt % 2]
            r0 = nt * 2048 + tt * 128
            P.dma('sp', c_.t[:, :], dr['kv'][r0:r0 + 128, 0:256], W=[c_.d])
            P.op('dve', lambda e: e.tensor_copy(cb_.t[:, :], c_.t[:, :]), R=[c_.d], W=[cb_.d])

        def cB(tt):
            cb_ = ctb[tt % 2]
            for kvi in range(2):
                P.tr(psT[:, kvi * 128:(kvi + 1) * 128], cb_.t[:, kvi * 128:(kvi + 1) * 128], ident.t[:], R=[cb_.d, ident.d], W=[psT_d])
            P.act(kct.t[:, :, tt * 128:(tt + 1) * 128], psT[:, 0:256].rearrange("p (k n) -> p k n", k=2), AF.Copy, R=[psT_d], W=[kct.d])
        cA(0)
        for tt in range(ntile):
            if tt + 1 < ntile:
                cA(tt + 1)
            cB(tt)
        for kvi in range(2):
            for g in range(2):
                bk, bkd = rbank()
                rows = slice(g * 64, (g + 1) * 64)
                for p_ in range(32):
                    P.mm(bk[:, 0:128], W1[kvi].t[rows, p_, :], kct.t[rows, kvi, p_:p_ + 2033:16], p_ == 0, p_ == 31,
                         R=[W1[kvi].d, kct.d], W=[bkd])
                i2 = cnt % 2
                cnt += 1
                xb, x2, in1, sg, A1 = gl['xb'][i2], gl['x2'][i2], gl['in1'][i2], gl['sg'][i2], a1[i2]
                P.act(xb.t[:, :], bk[:, 0:128], AF.Identity, R=[bkd, b1[kvi].d], W=[xb.d], bias=b1[kvi].t[:, 0:1])
                P.op('dve', lambda e: e.tensor_tensor(x2.t[:, :], xb.t[:, :], xb.t[:, :], ALU.mult), R=[xb.d], W=[x2.d])
                P.op('dve', lambda e: e.tensor_scalar(in1.t[:, :], x2.t[:, :], 0.044715, 1.0, ALU.mult, ALU.add), R=[x2.d], W=[in1.d])
                P.op('dve', lambda e: e.tensor_tensor(x2.t[:, :], in1.t[:, :], xb.t[:, :], ALU.mult), R=[in1.d, xb.d], W=[x2.d])
                P.act(sg.t[:, :], x2.t[:, :], AF.Sigmoid, R=[x2.d], W=[sg.d], scale=GC)
                P.op('dve', lambda e: e.tensor_tensor(A1.t[:, :], xb.t[:, :], sg.t[:, :], ALU.mult), R=[xb.d, sg.d], W=[A1.d])
                bo, bod = rbank()
                if kvi == 0:
                    P.mm(bo[0:64, 0:128], W2[0].t[:, :], A1.t[:, :], True, True, R=[W2[0].d, A1.d], W=[bod])
                    P.act(KTc[g].t[0:64, nt * 128:(nt + 1) * 128], bo[0:64, 0:128], AF.Copy, R=[bod], W=[KTc[g].d])
                else:
                    P.mm(bo[:, 0:64], A1.t[:, :], W2[1].t[:, :], True, True, R=[W2[1].d, A1.d], W=[bod])
                    P.act(Vc.t[:, nt, g, 0:64], bo[:, 0:64], AF.Copy, R=[bod], W=[Vc.d])
    onesb = AT(P, [128, 2], BF16, "onesb")
    P.op('dve', lambda e: e.memset(onesb.t[:, :], 1.0), W=[onesb.d])
    kc2 = AT(P, [64, NCT * 128], BF16, "kc2")
    for g in range(2):
        P.op('dve', lambda e: e.tensor_tensor(kc2.t[:, :], KTc[g].t[0:64, :], KTc[g].t[0:64, :], ALU.mult), R=[KTc[g].d], W=[kc2.d])
        for nt in range(NCT):
            bk, bkd = rbank()
            P.mm(bk[:, 0:2], kc2.t[:, nt * 128:(nt + 1) * 128], onesb.t[0:64, :], True, True, R=[kc2.d, onesb.d], W=[bkd])
            P.op('dve', lambda e: e.tensor_scalar(kmx.t[:, 2:3], bk[:, 0:1], 1.02, None, ALU.mult), R=[bkd], W=[kmx.d])
            P.op('dve', lambda e: e.tensor_tensor(kmx.t[:, 0:1], kmx.t[:, 0:1], kmx.t[:, 2:3], ALU.max), R=[kmx.d], W=[kmx.d])
    P.op('dve', lambda e: e.tensor_tensor(kmx.t[:, 0:1], kmx.t[:, 0:1], kmx.t[:, 1:2], ALU.max), R=[kmx.d], W=[kmx.d])
    kmb = AT(P, [128, 128], F32, "kmb")
    P.op('dve', lambda e: e.tensor_copy(kmb.t[:, :], kmx.t[:, 0:1].broadcast_to([128, 128])), R=[kmx.d], W=[kmb.d])
    bk, bkd = rbank()
    P.op('pe', lambda e: e.transpose(bk[:, 0:128], kmb.t[:, :], ident_f.t[:, :]), R=[kmb.d, ident_f.d], W=[bkd])
    Kmax2 = Tl(P, [128, 1], F32, "Kmax2")
    P.op('dve', lambda e: e.tensor_reduce(Kmax2.t[:, 0:1], bk[:, 0:128], AX.X, ALU.max), R=[bkd], W=[Kmax2.d])
    AR.reset()
    sq = AT(P, [128, 512], F32, "sq2")
    qg = [AT(P, [128, 536], F32, "qg%d" % i) for i in range(2)]
    qa = [AT(P, [128, 8, 5], F32, "qa%d" % i) for i in range(2)]
    gates = [AT(P, [128, 24], F32, "gates%d" % i) for i in range(2)]
    Qtm = [AT(P, [128, 8, 69], BF16, "Qtm%d" % i) for i in range(2)]
    Qaug = [AT(P, [69, 8, 128], BF16, "Qaug%d" % i) for i in range(2)]
    msq = [AT(P, [128, 16], F32, "msq%d" % i) for i in range(2)]
    kwt = [AT(P, [128, 5, 256], F32, "kwt0")] * 2
    Kwm = [AT(P, [128, 5, 2, 69], BF16, "Kwm%d" % i) for i in range(2)]
    KTw = [AT(P, [69, 10, 128], BF16, "KTw%d" % i) for i in range(2)]
    Vw = [AT(P, [128, 5, 2, 65], BF16, "Vw%d" % i) for i in range(2)]
    for i in range(2):
        P.op('dve', lambda e: e.memset(Vw[i].t[:, :, :, 64:65], 1.0), W=[Vw[i].d])
    cmk = [AT(P, [128, 2, 128], F32, "cmk%d" % i) for i in range(2)]
    cmkb = [AT(P, [128, 2, 128], BF16, "cmkb%d" % i) for i in range(2)]
    sbias = [AT(P, [128, 256], F32, "sbias%d" % i) for i in range(2)]
    E = [AT(P, [128, 512], BF16, "E%d" % i) for i in range(3)]
    ecnt = [0]
    imp = AT(P, [128, 256], F32, "imp")
    imp2 = AT(P, [128, 256], F32, "imp2")
    m8 = AT(P, [128, 16], F32, "m8")
    negsel = AT(P, [128, 256], BF16, "negsel")
    NST = [AT(P, [128, 2, 128], BF16, "NST%d" % i) for i in range(2)]
    wts = AT(P, [128, 3, 4], F32, "wts")
    den = AT(P, [128, 3, 4], F32, "den")
    onsa = [AT(P, [128, 512], F32, "onsa%d" % i) for i in range(2)]

    def attn_tile(KT_ap, KT_d, Qg, V_ap, V_d, Ob, Obd, ncolsV, first, last, sel=None, mask=None, per_r_banks=None):
        sb_, sbd = rbank(0, 3)
        sv = sb_[:, :].rearrange("p (r q) -> p r q", r=4)
        extra = []
        if sel is not None:
            ex_ap, nst_ap, nst_d = sel
            extra.append((ex_ap, nst_ap, [exb.d, nst_d]))
        if mask is not None:
            mk_ap, mk_d = mask
            extra.append((ident.t[:, :], mk_ap.unsqueeze(1).broadcast_to([128, 4, 128]), [ident.d, mk_d]))
        P.mm(sv, KT_ap, Qg, True, len(extra) == 0, R=[KT_d, Qaug[jq].d], W=[sbd])
        for i_, (l_, r_, d_) in enumerate(extra):
            P.mm(sv, l_, r_, False, i_ == len(extra) - 1, R=d_, W=[sbd])
        e_ = E[ecnt[0] % 3]
        ecnt[0] += 1
        P.act(e_.t[:, :], sb_[:, :], AF.Exp, R=[sbd], W=[e_.d])
        def pv():
            for r in range(4):
                if per_r_banks is not None:
                    ob, obd = per_r_banks[r]
                    P.mm(ob[:, 0:ncolsV], e_.t[:, r * 128:(r + 1) * 128], V_ap, first, last, R=[e_.d, V_d], W=[obd])
                else:
                    P.mm(Ob[:, r * ncolsV:(r + 1) * ncolsV], e_.t[:, r * 128:(r + 1) * 128], V_ap, first and r == 0, last, R=[e_.d, V_d], W=[Obd],
                         skip_group_check=True)
        pending.append(pv)
        while len(pending) > 2:
            pending.pop(0)()

    pending = []

    def flush():
        while pending:
            pending.pop(0)()

    for j in range(NJ):
        jq = j % 2
        Q_, QA, GT, QT_, QG_, MS = qg[jq], qa[jq], gates[jq], Qtm[jq], Qaug[jq], msq[jq]
        P.dma('sp', Q_.t[:, :], dr['qg'][j * 128:(j + 1) * 128, :], W=[Q_.d])
        P.dma('sp', QA.t[:, :, :], dr['qaug'][j * 128:(j + 1) * 128, :].rearrange("p (h c) -> p h c", c=5), W=[QA.d])
        P.act(GT.t[:, :], Q_.t[:, 512:536], AF.Sigmoid, R=[Q_.d], W=[GT.d])
        P.act(QT_.t[:, :, 0:64], Q_.t[:, 0:512].rearrange("p (h d) -> p h d", h=8), AF.Copy, R=[Q_.d], W=[QT_.d], scale=0.125)
        P.op('dve', lambda e: e.tensor_tensor(sq.t[:, :], Q_.t[:, 0:512], Q_.t[:, 0:512], ALU.mult), R=[Q_.d], W=[sq.d])
        P.op('dve', lambda e: e.tensor_reduce(MS.t[:, 0:8], sq.t[:, :].rearrange("p (h d) -> p h d", h=8), AX.X, ALU.add), R=[sq.d], W=[MS.d])
        P.act(MS.t[:, 8:16], MS.t[:, 0:8], AF.Sqrt, R=[MS.d, Kmax2.d], W=[MS.d], scale=Kmax2.t[:, 0:1])
        P.op('dve', lambda e: e.scalar_tensor_tensor(QT_.t[:, :, 64], MS.t[:, 8:16], -0.125, QA.t[:, :, 0], ALU.mult, ALU.add),
             R=[MS.d, QA.d], W=[QT_.d])
        P.op('dve', lambda e: e.tensor_copy(QT_.t[:, :, 65:69], QA.t[:, :, 1:5]), R=[QA.d], W=[QT_.d])
        for h in range(8):
            P.tr(psT[0:69, h * 128:(h + 1) * 128], QT_.t[:, h, :], ident.t[:], R=[QT_.d, ident.d], W=[psT_d])
        P.act(QG_.t[0:69, :, :], psT[0:69, :].rearrange("p (h q) -> p h q", h=8), AF.Copy, R=[psT_d], W=[QG_.d])
        KW, KM, KTW, VW = kwt[jq], Kwm[jq], KTw[jq], Vw[jq]
        P.dma('sp', KW.t[:, :, :], dr['kwin'][j * 640:(j + 1) * 640, :].rearrange("(s p) c -> p s c", p=128), W=[KW.d])
        P.act(KM.t[:, :, :, 0:64], KW.t[:, :, 0:128].rearrange("p s (g d) -> p s g d", g=2), AF.Copy, R=[KW.d], W=[KM.d])
        for g in range(2):
            P.op('dve', lambda e: e.tensor_copy(KM.t[:, :, g, 64:69], kaug_win.t[:, :, :]), R=[kaug_win.d], W=[KM.d])
        P.op('dve', lambda e: e.tensor_copy(VW.t[:, :, :, 0:64], KW.t[:, :, 128:256].rearrange("p s (g d) -> p s g d", g=2)),
             R=[KW.d], W=[VW.d])
        for half in range(2):
            idxs = list(range(half * 5, half * 5 + 5))
            for ii, sg_ in enumerate(idxs):
                s_, g_ = sg_ // 2, sg_ % 2
                P.tr(psT[0:69, ii * 128:(ii + 1) * 128], KM.t[:, s_, g_, :], ident.t[:], R=[KM.d, ident.d], W=[psT_d])
            P.act(KTW.t[0:69, half * 5:half * 5 + 5, :], psT[0:69, 0:640].rearrange("p (a q) -> p a q", a=5), AF.Copy, R=[psT_d], W=[KTW.d])
        CM, SB_ = cmk[jq], sbias[jq]
        P.dma('sp', CM.t[:, :, :], dr['cmask'][j * 256:(j + 1) * 256, :].rearrange("(a p) c -> p a c", p=128), W=[CM.d])
        P.dma('sp', SB_.t[:, :], dr['selbias'][j * 128:(j + 1) * 128, :], W=[SB_.d])
        CMB = cmkb[jq]
        P.op('dve', lambda e: e.tensor_copy(CMB.t[:], CM.t[:]), R=[CM.d], W=[CMB.d])
        ON = onsa[jq]

        def combine(g, xi, ob, obd):
            flush()
            P.op('dve', lambda e: e.tensor_scalar(den.t[:, xi, :], ob[:, 0:260].rearrange("p (r c) -> p r c", r=4)[:, :, 64], 1e-30, None, ALU.max),
                 R=[obd], W=[den.d])
            P.op('dve', lambda e: e.reciprocal(den.t[:, xi, :], den.t[:, xi, :]), R=[den.d], W=[den.d])
            P.op('dve', lambda e: e.tensor_tensor(wts.t[:, xi, :], den.t[:, xi, :],
                                                  GT.t[:, :].rearrange("p (h x) -> p h x", x=3)[:, 4 * g:4 * g + 4, xi], ALU.mult),
                 R=[den.d, GT.d], W=[wts.d])
            for r in range(4):
                col = (4 * g + r) * 64
                P.op('dve', lambda e: e.scalar_tensor_tensor(ON.t[:, col:col + 64], ob[:, r * 65:r * 65 + 64], wts.t[:, xi, r:r + 1],
                                                             ON.t[:, col:col + 64], ALU.mult, ALU.add), R=[obd, wts.d, ON.d], W=[ON.d])

        def pre(g):
            Qg = QG_.t[0:69, 4 * g:4 * g + 4, :]
            NT = j // 4
            crb = [(bank[3 + r], bd[3 + r]) for r in range(4)]
            for nt in range(NT + 1):
                mk = None
                if nt == NT:
                    mk = (CMB.t[:, 0, :], CMB.d)
                elif nt == NT - 1:
                    mk = (CMB.t[:, 1, :], CMB.d)
                attn_tile(KTc[g].t[0:69, nt * 128:(nt + 1) * 128], KTc[g].d, Qg, Vc.t[:, nt, g, :], Vc.d, None, None, 321,
                          nt == 0, nt == NT, mask=mk, per_r_banks=crb)
            flush()
            for r in range(4):
                ob, obd = crb[r]
                P.op('dve', lambda e: e.tensor_scalar(den.t[:, 0, r:r + 1], ob[:, 64:65], 1e-30, None, ALU.max), R=[obd], W=[den.d])
            P.op('dve', lambda e: e.reciprocal(den.t[:, 0, :], den.t[:, 0, :]), R=[den.d], W=[den.d])
            P.op('dve', lambda e: e.tensor_tensor(wts.t[:, 0, :], den.t[:, 0, :], GT.t[:, :].rearrange("p (h x) -> p h x", x=3)[:, 4 * g:4 * g + 4, 0],
                                                  ALU.mult), R=[den.d, GT.d], W=[wts.d])
            for r in range(4):
                ob, obd = crb[r]
                col = (4 * g + r) * 64
                P.op('dve', lambda e: e.tensor_scalar(ON.t[:, col:col + 64], ob[:, 0:64], wts.t[:, 0, r:r + 1], None, ALU.mult),
                     R=[obd, wts.d], W=[ON.d])
                if r == 0:
                    P.op('dve', lambda e: e.tensor_scalar(imp.t[:, :], ob[:, 65:321], den.t[:, 0, r:r + 1], None, ALU.mult),
                         R=[obd, den.d], W=[imp.d])
                else:
                    P.op('dve', lambda e: e.scalar_tensor_tensor(imp.t[:, :], ob[:, 65:321], den.t[:, 0, r:r + 1], imp.t[:, :], ALU.mult, ALU.add),
                         R=[obd, den.d, imp.d], W=[imp.d])
            wb, wbd = bank[4], bd[4]
            wset = 0 if j == 0 else 1
            for slot in range(5):
                mk = None
                if j == 0 or slot in (0, 4):
                    mk = (wmaskb.t[:, wset * 5 + slot, :], wmaskb.d)
                attn_tile(KTW.t[0:69, slot * 2 + g, :], KTW.d, Qg, VW.t[:, slot, g, :], VW.d, wb, wbd, 65, slot == 0, slot == 4, mask=mk)
            combine(g, 2, wb, wbd)
            P.op('dve', lambda e: e.tensor_tensor(imp2.t[:, :], imp.t[:, :], SB_.t[:, :], ALU.add), R=[imp.d, SB_.d], W=[imp2.d])
            P.op('dve', lambda e: e.max(m8.t[:, 0:8], imp2.t[:, :]), R=[imp2.d], W=[m8.d])
            P.op('dve', lambda e: e.match_replace(imp.t[:, :], m8.t[:, 0:8], imp2.t[:, :], -3.0e38), R=[imp2.d, m8.d], W=[imp.d])
            P.op('dve', lambda e: e.max(m8.t[:, 8:16], imp.t[:, :]), R=[imp.d], W=[m8.d])
            P.op('dve', lambda e: e.tensor_scalar(imp.t[:, :], imp2.t[:, :], m8.t[:, 15:16], None, ALU.is_ge), R=[imp2.d, m8.d], W=[imp.d])
            P.op('dve', lambda e: e.tensor_scalar(negsel.t[:, :], imp.t[:, :], -1.0, NEG, ALU.add, ALU.mult), R=[imp.d], W=[negsel.d])
            NS = NST[g]
            for jt in range(2):
                P.tr(psT[:, jt * 128:(jt + 1) * 128], negsel.t[:, jt * 128:(jt + 1) * 128], ident.t[:], R=[negsel.d, ident.d], W=[psT_d])
            P.op('dve', lambda e: e.tensor_copy(NS.t[:, :, :], psT[:, 0:256].rearrange("p (a q) -> p a q", a=2)), R=[psT_d], W=[NS.d])

        def selb(g):
            Qg = QG_.t[0:69, 4 * g:4 * g + 4, :]
            NS = NST[g]
            sbk, sbkd = bank[3], bd[3]
            nk = 4 * j + 4
            for kt in range(nk):
                mk = None
                if kt >= 4 * j:
                    mk = (smaskb.t[:, kt - 4 * j, :], smaskb.d)
                attn_tile(KTs[g].t[0:69, kt * 128:(kt + 1) * 128], KTs[g].d, Qg, Vs.t[:, kt, g, :], Vs.d, sbk, sbkd, 65,
                          kt == 0, kt == nk - 1, sel=(exb.t[:, kt % 64, :], NS.t[:, kt // 64, :].unsqueeze(1).broadcast_to([128, 4, 128]), NS.d), mask=mk)
            combine(g, 1, sbk, sbkd)

        pre(0)
        pre(1)
        selb(0)
        selb(1)
        P.dma('act', dr['onsa'][j * 128:(j + 1) * 128, :], ON.t[:, :], R=[ON.d])


_PROGS = {}
T_CORE = 4096
SEQ = 16384


def _gcol(v):
    return np.ascontiguousarray(np.asarray(v, np.float32).reshape(8, 128).T)


def _f32(a):
    return np.ascontiguousarray(np.asarray(a, dtype=np.float32))


def _run(nc, in_maps):
    res = run_bass_kernel_spmd(nc, in_maps, core_ids=list(range(8)))
    return res.results


FFN_W = (("wg", [D, DFF]), ("wu", [D, DFF]), ("wd", [DFF, D]), ("pre", [128, 8]), ("post", [D]))


def _ffn_drams(P, tag):
    return {k: P.dram("%s_%s" % (k, tag), shp, F32, "ExternalInput") for k, shp in FFN_W}


def _ffn_inputs(inp, prefix, tag):
    return {"wg_" + tag: _f32(inp[prefix + "_w_gate"]), "wu_" + tag: _f32(inp[prefix + "_w_up"]),
            "wd_" + tag: _f32(inp[prefix + "_w_down"]), "pre_" + tag: _gcol(inp[prefix + "_pre_g"]),
            "post_" + tag: _f32(inp[prefix + "_post_g"])}


def build_L1(T):
    P = Prog()
    x = P.dram("x", [T, D], F32, "ExternalInput")
    x1 = P.dram("x1", [T, D], F32, "ExternalOutput")
    proj = P.dram("proj", [T, 2840], F32, "ExternalOutput")
    ident = P.dram("ident", [128, 128], F32, "ExternalInput")
    w_in = P.dram("w_in", [D, 2840], F32, "ExternalInput")
    gin = P.dram("gcol_in", [128, 8], F32, "ExternalInput")
    fw = _ffn_drams(P, "a")
    C = Consts(P, {'ident': ident})
    B = FFNBufs(P)
    ffn_load_weights(P, B, fw['wg'], fw['wu'], fw['wd'], fw['pre'], fw['post'])
    X = DT(x, T)
    Y = DT(x1, T)
    ffn_stage(P, B, C, X, Y, T)
    inproj_stage(P, B, C, Y, T, w_in, gin, proj)
    return P.build()


def build_PF(T, conv, n_ffn):
    P = Prog()
    ident = P.dram("ident", [128, 128], F32, "ExternalInput")
    a = P.dram("a", [T, 512 if conv else D], F32, "ExternalInput")
    xres = P.dram("xres", [T, D], F32, "ExternalInput")
    Wm = P.dram("w_mix", [D, D], F32, "ExternalInput")
    pg = P.dram("mix_post", [D], F32, "ExternalInput")
    cv = None
    if conv:
        cv = dict(cbcu=P.dram("cbcu", [T + 2, 1536], F32, "ExternalInput"), w=P.dram("conv_w", [3, 512], F32, "ExternalInput"),
                  b=P.dram("conv_b", [512], F32, "ExternalInput"))
    fws = [_ffn_drams(P, "f%d" % i) for i in range(n_ffn)]
    out = P.dram("out", [T, D], F32, "ExternalOutput")
    C = Consts(P, {'ident': ident})
    B = FFNBufs(P)
    cur = DT(P.dram("scr0", [T, D], F32, "Internal"), T)
    projres_stage(P, B, C, T, a, Wm, pg, DT(xres, T), cur, conv=cv)
    for i in range(n_ffn):
        nxt = DT(out if i == n_ffn - 1 else P.dram("scr%d" % (i + 1), [T, D], F32, "Internal"), T)
        fw = fws[i]
        ffn_load_weights(P, B, fw['wg'], fw['wu'], fw['wd'], fw['pre'], fw['post'])
        ffn_stage(P, B, C, cur, nxt, T)
        cur = nxt
    return P.build()


NSA_WSH = dict(pe_k=[32, 64], w1_k=[2048, 128], w2_k=[128, 64], pe_v=[32, 64], w1_v=[2048, 128], w2_v=[128, 64])


def build_NSA(S):
    P = Prog()
    NJ = S // 512
    c0 = nsa_host_consts(S, 0)
    dr = {k: P.dram(k, list(v.shape), F32, "ExternalInput") for k, v in c0.items()}
    for k, v in dict(qg=[NJ * 128, 536], kv=[S + 128, 768], kwin=[NJ * 640, 256]).items():
        dr[k] = P.dram(k, v, F32, "ExternalInput")
    dr['onsa'] = P.dram("onsa", [NJ * 128, 512], F32, "ExternalOutput")
    w = {k: P.dram(k, v, F32, "ExternalInput") for k, v in NSA_WSH.items()}
    nsa_stage(P, S, dr, w)
    return P.build()


RW_SH = dict(gmu=[128, 7, 8], w_r=[D, 256], w_k=[D, 256], w_v=[D, 256], w_dec1=[D, 64], w_a1=[D, 64], w_g1=[D, 128],
             w_dec2=[64, 256], w_a2=[64, 256], w_g2=[128, 256], w0=[256], a0=[256], k_k=[256], k_a=[256], r_k=[256],
             lnx_g=[256], lnx_b=[256])


def build_RWKV(S):
    P = Prog()
    xpad = P.dram("xpad", [S + 1, D], F32, "ExternalInput")
    zout = P.dram("z", [S, 256], F32, "ExternalOutput")
    cmat = P.dram("cmat", [7, 128, 128], F32, "ExternalInput")
    w = {k: P.dram(k, v, F32, "ExternalInput") for k, v in RW_SH.items()}
    rwkv_stage(P, S, xpad, zout, w, cmat)
    return P.build()


def _prog(name, fn):
    if name not in _PROGS:
        _PROGS[name] = fn()
    return _PROGS[name]


def kernel(**inp):
    x = _f32(inp["x"])
    NB, S, _ = x.shape
    T = NB * S // 8
    CPB = 8 // NB
    eye = np.eye(128, dtype=np.float32)
    xs = x.reshape(8, T, D)
    nc = _prog("L1", lambda: build_L1(T))
    shared = dict(ident=eye, w_in=_f32(inp["l0_w_in"]), gcol_in=_gcol(inp["l0_mix_pre_g"]))
    shared.update(_ffn_inputs(inp, "l0_ffn1", "a"))
    r = _run(nc, [dict(shared, x=xs[c]) for c in range(8)])
    x1 = np.stack([r[c]["x1"] for c in range(8)], 0)
    proj = np.stack([r[c]["proj"] for c in range(8)], 0).reshape(NB, S, 2840)
    nc = _prog("NSA", lambda: build_NSA(S))
    cw = dict(pe_k=_f32(inp["l0_cmp_pe_k"]), w1_k=_f32(inp["l0_cmp_w1_k"]), w2_k=_f32(inp["l0_cmp_w2_k"]),
              pe_v=_f32(inp["l0_cmp_pe_v"]), w1_v=_f32(inp["l0_cmp_w1_v"]), w2_v=_f32(inp["l0_cmp_w2_v"]))
    consts = [nsa_host_consts(S, m) for m in range(CPB)]
    maps = []
    for c in range(8):
        b, m = c // CPB, c % CPB
        d = nsa_host_data(proj[b], m, S)
        d.update(consts[m])
        d.update(cw)
        maps.append({k: _f32(v) for k, v in d.items()})
    r = _run(nc, maps)
    onsa = np.zeros((NB, S, 512), np.float32)
    NJ = S // 512
    for c in range(8):
        b, m = c // CPB, c % CPB
        o = r[c]["onsa"]
        for j in range(NJ):
            qi = 4 * j + m
            onsa[b, qi * 128:(qi + 1) * 128] = o[j * 128:(j + 1) * 128]
    onsa = onsa.reshape(8, T, 512)
    nc = _prog("PF2", lambda: build_PF(T, True, 2))
    cbcu_full = np.concatenate([np.zeros((NB, 2, 1536), np.float32), proj[:, :, 1304:2840]], 1)
    shared = dict(ident=eye, w_mix=_f32(inp["l0_w_out"]), mix_post=_f32(inp["l0_mix_post_g"]),
                  conv_w=_f32(inp["l0_conv_w"]), conv_b=_f32(inp["l0_conv_b"]))
    shared.update(_ffn_inputs(inp, "l0_ffn2", "f0"))
    shared.update(_ffn_inputs(inp, "l1_ffn1", "f1"))
    maps = []
    for c in range(8):
        b, m = c // CPB, c % CPB
        t0 = m * T
        maps.append(dict(shared, a=onsa[c], xres=x1[c], cbcu=_f32(cbcu_full[b, t0:t0 + T + 2])))
    r = _run(nc, maps)
    x4 = np.stack([r[c]["out"] for c in range(8)], 0)
    nc = _prog("RWKV", lambda: build_RWKV(S))
    x4b = x4.reshape(NB, S, D)
    cm = rwkv_consts_np()
    gmu = np.concatenate([np.asarray(inp["l1_mix_pre_g"], np.float32)[None], np.asarray(inp["l1_mu"], np.float32)], 0)
    gmu = np.ascontiguousarray(gmu.reshape(7, 8, 128).transpose(2, 0, 1))
    maps = []
    for c in range(8):
        b, hg = c // CPB, c % CPB
        cs = slice(hg * 256, (hg + 1) * 256)
        d = dict(xpad=np.concatenate([np.zeros((1, D), np.float32), x4b[b]], 0), cmat=cm, gmu=gmu,
                 w_r=inp["l1_w_r"][:, cs], w_k=inp["l1_w_k"][:, cs], w_v=inp["l1_w_v"][:, cs],
                 w_dec1=inp["l1_w_dec1"], w_a1=inp["l1_w_a1"], w_g1=inp["l1_w_g1"],
                 w_dec2=inp["l1_w_dec2"][:, cs], w_a2=inp["l1_w_a2"][:, cs], w_g2=inp["l1_w_g2"][:, cs],
                 w0=inp["l1_w0"][cs], a0=inp["l1_a0"][cs], k_k=inp["l1_k_k"][cs], k_a=inp["l1_k_a"][cs],
                 r_k=np.asarray(inp["l1_r_k"]).reshape(-1)[cs], lnx_g=inp["l1_lnx_g"][cs], lnx_b=inp["l1_lnx_b"][cs])
        maps.append({k: _f32(v) for k, v in d.items()})
    r = _run(nc, maps)
    z = np.zeros((NB, S, D), np.float32)
    for c in range(8):
        b, hg = c // CPB, c % CPB
        z[b, :, hg * 256:(hg + 1) * 256] = r[c]["z"]
    z = z.reshape(8, T, D)
    nc = _prog("PF1", lambda: build_PF(T, False, 1))
    shared = dict(ident=eye, w_mix=_f32(inp["l1_w_o"]), mix_post=_f32(inp["l1_mix_post_g"]))
    shared.update(_ffn_inputs(inp, "l1_ffn2", "f0"))
    r = _run(nc, [dict(shared, a=z[c], xres=x4[c]) for c in range(8)])
    out = np.stack([r[c]["out"] for c in range(8)], 0).reshape(NB, S, D)
    return out.astype(np.float32)
```
